# Optimizing a Trainium2 kernel written in Bass

```python
import math
import jax, jax.numpy as jnp
from jax import lax
import numpy as np

D_MODEL = 1024
BATCH = 8
SEQ = 4096
DEPTH = 2

N_MIXERS = 2
N_MLA_LAYERS = (DEPTH + N_MIXERS - 1) // N_MIXERS
N_HYENA_LAYERS = DEPTH // N_MIXERS

MLA_HEADS = 8
Q_LORA = 256
KV_LORA = 128
QK_NOPE = 128
QK_ROPE = 64
QK_HEAD = QK_NOPE + QK_ROPE
V_HEAD = 128
ROPE_HALF = QK_ROPE // 2
ROPE_THETA = 10000.0
Q_BLOCK = 128

HYENA_ORDER = 2
SHORT_CONV = 3
FILTER_EMB = 33
FILTER_BANDS = (FILTER_EMB - 1) // 2
FILTER_WIDTH = 64
FAST_DECAY = 0.3
SLOW_DECAY = 1.5
DECAY_TARGET = 1e-2

N_GROUPS = 4
EXPERTS_PER_GROUP = 4
N_EXPERTS = N_GROUPS * EXPERTS_PER_GROUP
TOP_K = 2
D_EXPERT = 256

EPS = 1e-6

kernel_name = "hybrid_mla_hyena_hmoe_encoder"


def rms_norm(x, g):
    xf = x.astype(jnp.float32)
    y = xf * lax.rsqrt(jnp.mean(xf * xf, axis=-1, keepdims=True) + EPS)
    return (y * g.astype(jnp.float32)).astype(x.dtype)


def modulate(h, shift, scale):
    return h * (1 + scale[:, None, :]) + shift[:, None, :]


def rope_tables(positions):
    inv_freq = 1.0 / (ROPE_THETA ** (jnp.arange(0, QK_ROPE, 2, dtype=jnp.float32) / QK_ROPE))
    ang = positions.astype(jnp.float32)[..., None] * inv_freq
    return jnp.cos(ang), jnp.sin(ang)


def apply_rope(x, cos, sin):
    xf = x.astype(jnp.float32)
    x1, x2 = xf[..., :ROPE_HALF], xf[..., ROPE_HALF:]
    return jnp.concatenate([x1 * cos - x2 * sin, x1 * sin + x2 * cos], axis=-1).astype(x.dtype)


def mla_mixer(h, cos, sin, w_down, q_a_g, kv_a_g, w_uq, w_ukv, q_norm_g, k_norm_g, w_o):
    B, S, _ = h.shape
    lat = h @ w_down
    c_q = lat[..., :Q_LORA]
    c_kv = lat[..., Q_LORA:Q_LORA + KV_LORA]
    k_pe = lat[..., Q_LORA + KV_LORA:]
    q = (rms_norm(c_q, q_a_g) @ w_uq).reshape(B, S, MLA_HEADS, QK_HEAD)
    kv = (rms_norm(c_kv, kv_a_g) @ w_ukv).reshape(B, S, MLA_HEADS, QK_NOPE + V_HEAD)
    cos_h, sin_h = cos[:, :, None, :], sin[:, :, None, :]
    q_nope = rms_norm(q[..., :QK_NOPE], q_norm_g[:QK_NOPE])
    q_pe = apply_rope(rms_norm(q[..., QK_NOPE:], q_norm_g[QK_NOPE:]), cos_h, sin_h)
    k_nope = rms_norm(kv[..., :QK_NOPE], k_norm_g[:QK_NOPE])
    k_pe = apply_rope(rms_norm(k_pe, k_norm_g[QK_NOPE:]), cos, sin)
    v = kv[..., QK_NOPE:]
    q = jnp.concatenate([q_nope, q_pe], axis=-1)
    k = jnp.concatenate([k_nope, jnp.broadcast_to(k_pe[:, :, None, :], (B, S, MLA_HEADS, QK_ROPE))], axis=-1)
    scale = QK_HEAD ** -0.5
    n_blk = S // Q_BLOCK
    q_blocks = q.reshape(B, n_blk, Q_BLOCK, MLA_HEADS, QK_HEAD).transpose(1, 0, 2, 3, 4)

    def attend(q_blk):
        s = jnp.einsum('bqhd,bkhd->bhqk', q_blk, k, preferred_element_type=jnp.float32) * scale
        p = jax.nn.softmax(s, axis=-1).astype(v.dtype)
        return jnp.einsum('bhqk,bkhd->bqhd', p, v)

    o = lax.map(attend, q_blocks)
    o = o.transpose(1, 0, 2, 3, 4).reshape(B, S, MLA_HEADS * V_HEAD)
    return o @ w_o


def hyena_filters(L, w1, b1, f1, w2, b2, f2, w3, b3, f3, w4):
    f32 = jnp.float32
    t = jnp.linspace(0.0, 1.0, L, dtype=f32)[:, None]
    w = 2.0 * math.pi * jnp.arange(L, dtype=f32)[:, None] / L
    fr = jnp.linspace(1e-4, FILTER_BANDS - 1, FILTER_BANDS, dtype=f32)[None, :]
    z = jnp.concatenate([t, jnp.cos(fr * w), -jnp.sin(fr * w)], axis=-1)
    hdn = jnp.sin(f1.astype(f32) * (z @ w1.astype(f32) + b1.astype(f32)))
    hdn = jnp.sin(f2.astype(f32) * (hdn @ w2.astype(f32) + b2.astype(f32)))
    hdn = jnp.sin(f3.astype(f32) * (hdn @ w3.astype(f32) + b3.astype(f32)))
    h = (hdn @ w4.astype(f32)).reshape(L, HYENA_ORDER, 2, D_MODEL)
    deltas = jnp.abs(jnp.linspace(math.log(FAST_DECAY) / DECAY_TARGET,
                                  math.log(SLOW_DECAY) / DECAY_TARGET, D_MODEL, dtype=f32))
    decay = jnp.exp(-t * deltas)
    h = h * decay[:, None, None, :]
    return h / (jnp.sum(jnp.abs(h), axis=0, keepdims=True) + EPS)


def bidir_long_conv(u, h_fwd, h_bwd, bias):
    B, L, D = u.shape
    k2 = jnp.concatenate([h_fwd, jnp.zeros((1, D), jnp.float32), h_bwd[:0:-1]], axis=0)
    k_f = jnp.fft.rfft(k2, axis=0)
    u_f = jnp.fft.rfft(u.astype(jnp.float32), n=2 * L, axis=1)
    y = jnp.fft.irfft(u_f * k_f[None], n=2 * L, axis=1)[:, :L]
    return (y + u.astype(jnp.float32) * bias.astype(jnp.float32)).astype(u.dtype)


def hyena_mixer(h, w_in, b_in, conv_w, conv_b, f_w1, f_b1, f_freq1, f_w2, f_b2, f_freq2,
                f_w3, f_b3, f_freq3, f_w4, filt_bias, w_out):
    B, L, _ = h.shape
    C = 3 * D_MODEL
    u = h @ w_in + b_in
    u = lax.conv_general_dilated(u, conv_w[:, None, :].astype(u.dtype), window_strides=(1,),
                                 padding=((SHORT_CONV // 2, SHORT_CONV // 2),),
                                 dimension_numbers=('NWC', 'WIO', 'NWC'),
                                 feature_group_count=C) + conv_b
    x1, x2, v = jnp.split(u, 3, axis=-1)
    filt = hyena_filters(L, f_w1, f_b1, f_freq1, f_w2, f_b2, f_freq2, f_w3, f_b3, f_freq3, f_w4)
    z = v
    for o, gate in enumerate((x1, x2)):
        z = gate * bidir_long_conv(z, filt[:, o, 0], filt[:, o, 1], filt_bias[o])
    return z @ w_out


def hier_moe(h, wg, bg, we, be, w_gate, w_up, w_down):
    B, S, D = h.shape
    t = h.reshape(B * S, D)
    T = t.shape[0]
    g_prob = jax.nn.softmax((t @ wg).astype(jnp.float32) + bg.astype(jnp.float32), axis=-1)
    g_w, g_idx = lax.top_k(g_prob, 1)
    e_logits = ((t @ we).astype(jnp.float32) + be.astype(jnp.float32)).reshape(T, N_GROUPS, EXPERTS_PER_GROUP)
    e_logits = jnp.take_along_axis(e_logits, g_idx[:, :, None], axis=1)[:, 0]
    e_w, e_idx = lax.top_k(jax.nn.softmax(e_logits, axis=-1), TOP_K)
    weights = g_w * (e_w / jnp.sum(e_w, axis=-1, keepdims=True))
    expert = g_idx * EXPERTS_PER_GROUP + e_idx
    combine = jnp.sum(jax.nn.one_hot(expert, N_EXPERTS, dtype=jnp.float32) * weights[..., None], axis=1)
    combine = combine.astype(t.dtype)
    out = jnp.zeros_like(t)
    for e in range(N_EXPERTS):
        a = jax.nn.silu(t @ w_gate[e]) * (t @ w_up[e])
        out = out + combine[:, e:e + 1] * (a @ w_down[e])
    return out.reshape(B, S, D)


def setup_inputs(seed: int = 0) -> dict:
    key = jax.random.key(seed)
    ks = iter(jax.random.split(key, 64))
    D = D_MODEL
    NA, NH = N_MLA_LAYERS, N_HYENA_LAYERS

    def nrm(shape, scale):
        return jax.random.normal(next(ks), shape, jnp.float32) * scale

    def gain(shape):
        return 1.0 + nrm(shape, 0.02)

    x = nrm((BATCH, SEQ, D), 1.0)
    c = nrm((BATCH, D), 1.0)
    offs = jax.random.randint(next(ks), (BATCH, 1), 0, 1024, dtype=jnp.int32)
    positions = offs + jnp.arange(SEQ, dtype=jnp.int32)[None, :]
    return {
        "x": x, "c": c, "positions": positions,
        "ada_w": nrm((DEPTH, D, 6 * D), 0.5 * D ** -0.5),
        "ada_b": nrm((DEPTH, 6 * D), 0.02),
        "norm_mix_g": gain((DEPTH, D)),
        "norm_ffn_g": gain((DEPTH, D)),
        "mla_w_down": nrm((NA, D, Q_LORA + KV_LORA + QK_ROPE), D ** -0.5),
        "mla_q_a_g": gain((NA, Q_LORA)),
        "mla_kv_a_g": gain((NA, KV_LORA)),
        "mla_w_uq": nrm((NA, Q_LORA, MLA_HEADS * QK_HEAD), Q_LORA ** -0.5),
        "mla_w_ukv": nrm((NA, KV_LORA, MLA_HEADS * (QK_NOPE + V_HEAD)), KV_LORA ** -0.5),
        "mla_q_norm_g": gain((NA, QK_HEAD)),
        "mla_k_norm_g": gain((NA, QK_HEAD)),
        "mla_w_o": nrm((NA, MLA_HEADS * V_HEAD, D), (MLA_HEADS * V_HEAD) ** -0.5),
        "hy_w_in": nrm((NH, D, 3 * D), D ** -0.5),
        "hy_b_in": nrm((NH, 3 * D), 0.02),
        "hy_conv_w": nrm((NH, SHORT_CONV, 3 * D), SHORT_CONV ** -0.5),
        "hy_conv_b": nrm((NH, 3 * D), 0.02),
        "hy_f_w1": nrm((NH, FILTER_EMB, FILTER_WIDTH), FILTER_EMB ** -0.5),
        "hy_f_b1": nrm((NH, FILTER_WIDTH), 0.1),
        "hy_f_freq1": gain((NH, FILTER_WIDTH)),
        "hy_f_w2": nrm((NH, FILTER_WIDTH, FILTER_WIDTH), FILTER_WIDTH ** -0.5),
        "hy_f_b2": nrm((NH, FILTER_WIDTH), 0.1),
        "hy_f_freq2": gain((NH, FILTER_WIDTH)),
        "hy_f_w3": nrm((NH, FILTER_WIDTH, FILTER_WIDTH), FILTER_WIDTH ** -0.5),
        "hy_f_b3": nrm((NH, FILTER_WIDTH), 0.1),
        "hy_f_freq3": gain((NH, FILTER_WIDTH)),
        "hy_f_w4": nrm((NH, FILTER_WIDTH, HYENA_ORDER * 2 * D), FILTER_WIDTH ** -0.5),
        "hy_filt_bias": nrm((NH, HYENA_ORDER, D), 1.0),
        "hy_w_out": nrm((NH, D, D), D ** -0.5),
        "moe_wg": nrm((DEPTH, D, N_GROUPS), D ** -0.5),
        "moe_bg": nrm((DEPTH, N_GROUPS), 0.01),
        "moe_we": nrm((DEPTH, D, N_EXPERTS), D ** -0.5),
        "moe_be": nrm((DEPTH, N_EXPERTS), 0.01),
        "moe_w_gate": nrm((DEPTH, N_EXPERTS, D, D_EXPERT), D ** -0.5),
        "moe_w_up": nrm((DEPTH, N_EXPERTS, D, D_EXPERT), D ** -0.5),
        "moe_w_down": nrm((DEPTH, N_EXPERTS, D_EXPERT, D), D_EXPERT ** -0.5),
    }


def reference(x, c, positions, ada_w, ada_b, norm_mix_g, norm_ffn_g,
              mla_w_down, mla_q_a_g, mla_kv_a_g, mla_w_uq, mla_w_ukv, mla_q_norm_g, mla_k_norm_g, mla_w_o,
              hy_w_in, hy_b_in, hy_conv_w, hy_conv_b,
              hy_f_w1, hy_f_b1, hy_f_freq1, hy_f_w2, hy_f_b2, hy_f_freq2,
              hy_f_w3, hy_f_b3, hy_f_freq3, hy_f_w4, hy_filt_bias, hy_w_out,
              moe_wg, moe_bg, moe_we, moe_be, moe_w_gate, moe_w_up, moe_w_down):
    cos, sin = rope_tables(positions)
    c_act = jax.nn.silu(c.astype(jnp.float32))
    for i in range(DEPTH):
        mod = (c_act @ ada_w[i].astype(jnp.float32) + ada_b[i].astype(jnp.float32)).astype(x.dtype)
        sh1, sc1, g1, sh2, sc2, g2 = jnp.split(mod, 6, axis=-1)
        h = modulate(rms_norm(x, norm_mix_g[i]), sh1, sc1)
        j = i // N_MIXERS
        if i % N_MIXERS == 0:
            y = mla_mixer(h, cos, sin, mla_w_down[j], mla_q_a_g[j], mla_kv_a_g[j], mla_w_uq[j],
                          mla_w_ukv[j], mla_q_norm_g[j], mla_k_norm_g[j], mla_w_o[j])
        else:
            y = hyena_mixer(h, hy_w_in[j], hy_b_in[j], hy_conv_w[j], hy_conv_b[j],
                            hy_f_w1[j], hy_f_b1[j], hy_f_freq1[j], hy_f_w2[j], hy_f_b2[j], hy_f_freq2[j],
                            hy_f_w3[j], hy_f_b3[j], hy_f_freq3[j], hy_f_w4[j], hy_filt_bias[j], hy_w_out[j])
        x = x + g1[:, None, :] * y
        h = modulate(rms_norm(x, norm_ffn_g[i]), sh2, sc2)
        x = x + g2[:, None, :] * hier_moe(h, moe_wg[i], moe_bg[i], moe_we[i], moe_be[i],
                                          moe_w_gate[i], moe_w_up[i], moe_w_down[i])
    return x
```

```python
import math
from contextlib import ExitStack
import numpy as np
import ml_dtypes
import concourse.bass as bass
import concourse.mybir as mybir
from concourse.bass_utils import run_bass_kernel_spmd

F32 = mybir.dt.float32
BF16 = mybir.dt.bfloat16
I32 = mybir.dt.int32
AF = mybir.ActivationFunctionType
ALU = mybir.AluOpType
AX = mybir.AxisListType

D = 1024
S = 4096
NT = S // 128
EPS = 1e-6
NE = 16
DE = 256
NFFT = 8192

ENGS = ("pe", "act", "dve", "pool", "sp")
DMA_RING = {"sp": 12, "act": 4, "pool": 12}
SEM_CAP = 30000


_UID = [0]


def _uid():
    _UID[0] += 1
    return _UID[0]


class Buf:
    __slots__ = ("last_w", "readers", "excl")

    def __init__(self):
        self.last_w = None
        self.readers = []
        self.excl = True


class Op:
    __slots__ = ("eng", "fn", "is_dma", "lidx", "deps", "signal", "sig_idx", "waits", "dma_slot",
                 "dma_val", "ring_wait")

    def __init__(self, eng, fn, is_dma):
        self.eng = eng
        self.fn = fn
        self.is_dma = is_dma
        self.deps = []
        self.signal = False
        self.sig_idx = None
        self.waits = []
        self.dma_slot = None
        self.dma_val = None
        self.ring_wait = None


class Prog:
    def __init__(self):
        self.ops = []
        self.per_eng = {e: [] for e in ENGS}
        self.dma_count = {e: 0 for e in DMA_RING}

    def buf(self):
        return Buf()

    def bufs(self, n):
        return [Buf() for _ in range(n)]

    def op(self, eng, fn, reads=(), writes=(), dma=False):
        o = Op(eng, fn, dma)
        o.lidx = len(self.per_eng[eng])
        deps = set()
        for b in reads:
            if b.last_w is not None:
                deps.add(b.last_w)
            if b.excl:
                for r in b.readers:
                    if r.eng != eng:
                        deps.add(r)
        for b in writes:
            if b.last_w is not None:
                deps.add(b.last_w)
            for r in b.readers:
                deps.add(r)
        deps.discard(o)
        o.deps = list(deps)
        for b in reads:
            b.readers.append(o)
        for b in writes:
            b.last_w = o
            b.readers = []
        if dma:
            n = self.dma_count[eng]
            self.dma_count[eng] = n + 1
            R = DMA_RING[eng]
            o.dma_slot = (eng, n % R)
            o.dma_val = 16 * (n // R + 1)
            if n >= R:
                o.ring_wait = 16 * (n // R)
        self.ops.append(o)
        self.per_eng[eng].append(o)
        return o

    def dma(self, q, out, in_, reads=(), writes=(), **kw):
        if q != "act":
            is_store = "DRAM" in str(out.space)
            cast = out.dtype != in_.dtype
            q = "pool" if (is_store or cast) else "sp"
        return self.op(q, lambda e: e.dma_start(out=out, in_=in_, **kw), reads, writes, dma=True)

    def resolve(self):
        known = {c: {p: -1 for p in ENGS} for c in ENGS}
        known_dma = {c: {} for c in ENGS}
        for o in self.ops:
            c = o.eng
            latest = {}
            latest_dma = {}
            for d in o.deps:
                if d.is_dma:
                    if d.dma_slot not in latest_dma or d.dma_val > latest_dma[d.dma_slot].dma_val:
                        latest_dma[d.dma_slot] = d
                else:
                    if d.eng not in latest or d.lidx > latest[d.eng].lidx:
                        latest[d.eng] = d
            for slot in sorted(latest_dma):
                d = latest_dma[slot]
                k = known_dma[c].get(d.dma_slot, 0)
                if d.dma_val > k:
                    known_dma[c][d.dma_slot] = d.dma_val
                    o.waits.append(("dma", d.dma_slot, d.dma_val))
            for p in ENGS:
                if p not in latest:
                    continue
                d = latest[p]
                if p == "pe" and c == "pe":
                    continue
                if d.lidx > known[c][p]:
                    known[c][p] = d.lidx
                    d.signal = True
                    o.waits.append(("cmp", d))
            if o.is_dma and o.ring_wait is not None:
                k = known_dma[c].get(o.dma_slot, 0)
                if o.ring_wait > k:
                    known_dma[c][o.dma_slot] = o.ring_wait
                    o.waits.append(("dma", o.dma_slot, o.ring_wait))
        sig_count = {}
        for e in ENGS:
            n = 0
            for o in self.per_eng[e]:
                if o.signal and not o.is_dma:
                    o.sig_idx = n
                    n += 1
            sig_count[e] = n
        return sig_count

    def emit(self, nc):
        sig_count = self.resolve()
        handles = []

        def newsem(pfx):
            h = nc.alloc_semaphore(name=f"{pfx}{_uid()}")
            handles.append(h)
            return h
        with ExitStack() as es:
            csem = {}
            for e in ENGS:
                ns = max(1, (sig_count[e] + SEM_CAP - 1) // SEM_CAP)
                csem[e] = [newsem("cs") for i in range(ns)]
            dsem = {}
            for q, R in DMA_RING.items():
                for i in range(min(R, self.dma_count[q])):
                    dsem[(q, i)] = newsem("ds")
            block = es.enter_context(nc.Block())

            def run(engname):
                def body(eng):
                    for o in self.per_eng[engname]:
                        for w in o.waits:
                            if w[0] == "dma":
                                eng.wait_ge(dsem[w[1]], w[2])
                            else:
                                d = w[1]
                                eng.wait_ge(csem[d.eng][d.sig_idx // SEM_CAP], d.sig_idx % SEM_CAP + 1)
                        ins = o.fn(eng)
                        if o.is_dma:
                            ins.then_inc(dsem[o.dma_slot], 16)
                        elif o.signal:
                            ins.then_inc(csem[engname][o.sig_idx // SEM_CAP], 1)
                    if engname in DMA_RING:
                        n = self.dma_count[engname]
                        R = DMA_RING[engname]
                        for s in range(min(R, n)):
                            cnt = (n - 1 - s) // R + 1
                            eng.wait_ge(dsem[(engname, s)], 16 * cnt)
                return body

            block.tensor(run("pe"))
            block.scalar(run("act"))
            block.vector(run("dve"))
            block.gpsimd(run("pool"))
            block.sync(run("sp"))
        nc.clear_and_free_semaphores(handles)


_CONST = {}


def _bf(a):
    return np.ascontiguousarray(a.astype(np.float32)).astype(ml_dtypes.bfloat16)


def host_constants():
    if _CONST:
        return _CONST
    c = {}
    c["ident32"] = np.eye(128, dtype=np.float32)
    c["identb"] = _bf(np.eye(128))
    c["inv_freq"] = (1.0 / (10000.0 ** (np.arange(0, 64, 2, dtype=np.float32) / 64))).astype(np.float32)[None, :]
    sel = np.zeros((16, 16, 128), np.float32)
    for e in range(16):
        sel[e, e, :] = 1.0
    c["sel"] = _bf(sel.reshape(16, 16 * 128))
    L = S
    t = np.linspace(0.0, 1.0, L, dtype=np.float32)[:, None]
    w = (2.0 * math.pi * np.arange(L, dtype=np.float32)[:, None] / L).astype(np.float32)
    fr = np.linspace(1e-4, 15, 16, dtype=np.float32)[None, :]
    z = np.concatenate([t, np.cos(fr * w), -np.sin(fr * w)], axis=-1).astype(np.float32)
    c["zT"] = np.ascontiguousarray(z.T)
    tt = (32 * np.arange(128)[:, None] + np.arange(32)[None, :])
    c["ntl"] = (-t[:, 0][tt]).astype(np.float32)
    deltas = np.abs(np.linspace(math.log(0.3) / 1e-2, math.log(1.5) / 1e-2, D, dtype=np.float32))
    c["deltas"] = deltas.astype(np.float32)[None, :]
    p = np.arange(128)[:, None].astype(np.float64)
    k1 = np.arange(128)[None, :].astype(np.float64)
    F1 = np.exp(-2j * np.pi * p * (k1 + 0.5) / 256.0)
    c["F1"] = _bf(np.stack([F1.real, F1.imag], axis=1))
    sc = 2.0 / NFFT
    c["F1T"] = _bf(np.stack([F1.real.T * sc, F1.imag.T * sc], axis=1))
    a = np.arange(32)[:, None].astype(np.float64)
    k2 = np.arange(32)[None, :].astype(np.float64)
    GB = np.zeros((128, 128, 7, 128), np.float32)
    for kk in range(128):
        G = np.exp(-2j * np.pi * a * (kk + 256.0 * k2 + 0.5) / NFFT)
        mats = [G.real, G.imag, -G.imag, -G.real, G.real.T, G.imag.T, -G.imag.T]
        for mi, M in enumerate(mats):
            for c4 in range(4):
                GB[c4::4, kk, mi, c4::4] = M
    c["GB"] = _bf(GB)
    m4 = np.zeros((128, 4, 128), np.float32)
    for c4 in range(4):
        m4[:, c4, c4::4] = 1.0
    c["mask4"] = m4
    _CONST.update(c)
    return _CONST


class Ctx:
    pass


def build_program(debug=None, stop=None, dump=()):
    nc = bass.Bass("TRN2", target_bir_lowering=False)
    K = Ctx()
    K.nc = nc

    def din(name, shape, dt=F32):
        return nc.dram_tensor(name, list(shape), dt, kind="ExternalInput").ap()

    def dscr(name, shape, dt=F32):
        return nc.dram_tensor(name, list(shape), dt, kind="Internal").ap()

    I = {}
    I["x"] = din("x", [S, D])
    I["c_t"] = din("c_t", [128, 8])
    I["pos_t"] = din("pos_t", [128, NT], I32)
    I["ada_w"] = din("ada_w", [2, D, 6 * D])
    I["ada_b"] = din("ada_b", [2, 6 * D])
    I["norm_mix_g"] = din("norm_mix_g", [2, D])
    I["norm_ffn_g"] = din("norm_ffn_g", [2, D])
    I["mla_w_down"] = din("mla_w_down", [D, 448])
    I["mla_q_a_g"] = din("mla_q_a_g", [1, 256])
    I["mla_kv_a_g"] = din("mla_kv_a_g", [1, 128])
    I["mla_w_uq"] = din("mla_w_uq", [256, 1536])
    I["mla_w_ukv"] = din("mla_w_ukv", [128, 2048])
    I["mla_q_norm_g"] = din("mla_q_norm_g", [1, 192])
    I["mla_k_norm_g"] = din("mla_k_norm_g", [1, 192])
    I["mla_w_o"] = din("mla_w_o", [D, D])
    I["hy_w_in"] = din("hy_w_in", [D, 3 * D])
    I["hy_b_in_t"] = din("hy_b_in_t", [128, 24])
    I["hy_conv_w_t"] = din("hy_conv_w_t", [128, 3, 24])
    I["hy_conv_b_t"] = din("hy_conv_b_t", [128, 24])
    I["hy_f_w1"] = din("hy_f_w1", [33, 64])
    I["hy_f_w2"] = din("hy_f_w2", [64, 64])
    I["hy_f_w3"] = din("hy_f_w3", [64, 64])
    I["hy_f_bf_t"] = din("hy_f_bf_t", [64, 6])
    I["hy_f_w4"] = din("hy_f_w4", [64, 4 * D])
    I["hy_filt_bias"] = din("hy_filt_bias", [2, D])
    I["hy_w_out"] = din("hy_w_out", [D, D])
    I["moe_wr"] = din("moe_wr", [2, D, 20])
    I["moe_br"] = din("moe_br", [2, 20])
    I["moe_w_gate"] = din("moe_w_gate", [2, NE, D, DE])
    I["moe_w_up"] = din("moe_w_up", [2, NE, D, DE])
    I["moe_w_down"] = din("moe_w_down", [2, NE, DE, D])
    I["ident32"] = din("ident32", [128, 128])
    I["identb"] = din("identb", [128, 128], BF16)
    I["inv_freq"] = din("inv_freq", [1, 32])
    I["sel"] = din("sel", [16, 16 * 128], BF16)
    I["zT"] = din("zT", [33, S])
    I["ntl"] = din("ntl", [128, 32])
    I["deltas"] = din("deltas", [1, D])
    I["F1"] = din("F1", [128, 2, 128], BF16)
    I["F1T"] = din("F1T", [128, 2, 128], BF16)
    I["GB"] = din("GB", [128, 128, 7, 128], BF16)
    I["mask4"] = din("mask4", [128, 4, 128])
    K.I = I
    out = nc.dram_tensor("out", [S, D], F32, kind="ExternalOutput").ap()
    K.out = out

    K.xs = dscr("xs", [S, D])
    K.QN = dscr("QN", [8, 128, S], BF16)
    K.KN = dscr("KN", [8, 128, S], BF16)
    K.QP = dscr("QP", [4, 128, S], BF16)
    K.KP = dscr("KP", [64, S], BF16)
    K.V = dscr("V", [8, S, 130], BF16)
    K.OT = dscr("OT", [8, 128, S], BF16)
    K.AT = dscr("AT", [NE, 2, 128, S], BF16)
    K.UC = dscr("UC", [S, 3 * D], BF16)
    K.Ad = dscr("Ad", [2, 2, 128, 32 * D], BF16)
    K.Bd = dscr("Bd", [2, 128, 32 * D], BF16)
    K.Kd = dscr("Kd", [2, 128, 128, 2, 256], BF16)
    K.Z = dscr("Z", [2, S, D], BF16)
    K.HT = dscr("HT", [128, 8, S], BF16)
    K.CT = dscr("CT", [16, S], BF16)
    K.dbg = {}
    if debug:
        for name, (shape, dt) in debug.items():
            K.dbg[name] = nc.dram_tensor("dbg_" + name, list(shape), dt, kind="ExternalOutput").ap()

    with ExitStack() as pes:
        def psb(name, shape, dt):
            return pes.enter_context(nc.sbuf_tensor(name, list(shape), dt))
        K.ident32 = psb("ident32_sb", [128, 128], F32)
        K.identb = psb("identb_sb", [128, 128], BF16)
        K.modb = psb("modb", [128, 6 * D], F32)
        K.cbc = psb("cbc", [128, 8, 128], F32)

        seq = [("setup", lambda: phase_setup(K))]
        for layer in range(2):
            seq.append((f"adaln{layer}", lambda layer=layer: phase_adaln(K, layer)))
            if layer == 0:
                seq.append(("mla_proj", lambda: phase_mla_proj(K)))
                seq.append(("attn", lambda: phase_attention(K)))
                seq.append(("outproj0", lambda: phase_outproj(K, None, I["mla_w_o"], 0, src_fm=K.OT)))
            else:
                seq.append(("hy_norm", lambda: phase_norm_T(K, 0)))
                seq.append(("hy_in", lambda: phase_hyena_in(K)))
                seq.append(("hy_filter", lambda: phase_hyena_filter(K)))
                seq.append(("hy_conv", lambda: phase_hyena_conv(K)))
                seq.append(("outproj1", lambda: phase_outproj(K, K.Z[1], I["hy_w_out"], 1)))
            seq.append((f"moe_norm{layer}", lambda layer=layer: phase_norm_T(K, 3, router_layer=layer)))
            seq.append((f"moe_a{layer}", lambda layer=layer: phase_moe_a(K, layer)))
            seq.append((f"moe_b{layer}", lambda layer=layer: phase_moe_b(K, layer)))
        K.skip = set()
        for name, fn in seq:
            if name not in getattr(build_program, "SKIP", set()):
                fn()
            if stop == name:
                break
        if dump:
            def body(P, sb, ps):
                for nm in dump:
                    src = getattr(K, nm) if hasattr(K, nm) else None
                    if nm == "modb":
                        P.dma("sp", K.dbg[nm], K.modb[:])
                    else:
                        P.dma("sp", K.dbg[nm], src)
            run_phase(K, body)
    return nc


def run_phase(K, body):
    nc = K.nc
    with ExitStack() as es:
        P = Prog()

        pre = f"ph{_uid()}_"

        def sb(name, shape, dt):
            return es.enter_context(nc.sbuf_tensor(pre + name, list(shape), dt))

        def ps(name, shape, dt):
            return es.enter_context(nc.psum_tensor(pre + name, list(shape), dt))
        body(P, sb, ps)
        P.emit(nc)
    nc.all_engine_barrier()


def phase_setup(K):
    I = K.I

    def body(P, sb, ps):
        ct = sb("ct", [128, 8], F32)
        ca = sb("ca", [128, 8], F32)
        b1, b2, b3, b4 = P.bufs(4)
        P.dma("sp", K.ident32[:], I["ident32"], writes=[b1])
        P.dma("sp", K.identb[:], I["identb"], writes=[b2])
        P.dma("sp", ct[:], I["c_t"], writes=[b3])
        P.op("act", lambda e: e.activation(out=ca[:], in_=ct[:], func=AF.Silu), [b3], [b4])
        for kc in range(8):
            P.op("dve", lambda e, kc=kc: e.tensor_copy(out=K.cbc[:, kc, :], in_=ca[:, kc:kc + 1].to_broadcast([128, 128])), [b4], [b1])
        xt = [sb(f"xcp{i}", [128, 4, D], F32) for i in range(2)]
        xb = P.bufs(2)
        for i in range(8):
            P.dma("sp", xt[i % 2][:], I["x"][i * 512:(i + 1) * 512, :].rearrange("(j p) d -> p j d", p=128), writes=[xb[i % 2]])
            P.dma("pool", K.xs[i * 512:(i + 1) * 512, :].rearrange("(j p) d -> p j d", p=128), xt[i % 2][:], reads=[xb[i % 2]])
    run_phase(K, body)


def phase_adaln(K, layer):
    I = K.I

    def body(P, sb, ps):
        wt = [sb(f"adaw{i}", [128, 8, 512], F32) for i in range(2)]
        wb = P.bufs(2)
        bt = sb("adab", [128, 6 * D], F32)
        bb = P.buf()
        gm = sb("gm", [128, 2, D], F32)
        gb_ = P.buf()
        pp = [ps(f"adaps{i}", [128, 512], F32) for i in range(2)]
        pb = P.bufs(2)
        mb = P.bufs(12)
        P.dma("sp", bt[:], I["ada_b"][layer:layer + 1, :].partition_broadcast(128), writes=[bb])
        P.dma("sp", gm[:, 0, :], I["norm_mix_g"][layer:layer + 1, :].partition_broadcast(128), writes=[gb_])
        P.dma("sp", gm[:, 1, :], I["norm_ffn_g"][layer:layer + 1, :].partition_broadcast(128), writes=[gb_])
        wv = I["ada_w"][layer].rearrange("(kc p) n -> p kc n", p=128)
        for j in range(12):
            q = "sp" if j % 2 == 0 else "pool"
            P.dma(q, wt[j % 2][:], wv[:, :, j * 512:(j + 1) * 512], writes=[wb[j % 2]])
            for kc in range(8):
                P.op("pe", lambda e, j=j, kc=kc: e.matmul(pp[j % 2][:], lhsT=K.cbc[:, kc, :], rhs=wt[j % 2][:, kc, :], start=(kc == 0), stop=(kc == 7)),
                     [wb[j % 2]], [pb[j % 2]])
            P.op("dve", lambda e, j=j: e.tensor_tensor(out=K.modb[:, j * 512:(j + 1) * 512], in0=pp[j % 2][:], in1=bt[:, j * 512:(j + 1) * 512], op=ALU.add),
                 [pb[j % 2], bb], [mb[j]])
        for si, gi in ((1, 0), (4, 1)):
            sec = K.modb[:, si * D:(si + 1) * D]
            P.op("dve", lambda e, sec=sec, gi=gi: e.scalar_tensor_tensor(out=sec, in0=sec, scalar=1.0, in1=gm[:, gi, :], op0=ALU.add, op1=ALU.mult),
                 [mb[2 * si], mb[2 * si + 1], gb_], [mb[2 * si], mb[2 * si + 1]])
    run_phase(K, body)


class NormCtx:
    def __init__(self, P, sb, ps, K, want_f32=False, tag="n"):
        self.P, self.K = P, K
        self.want_f32 = want_f32
        self.xt = [sb(f"{tag}_xt{i}", [128, D], F32) for i in range(2)]
        self.xb = P.bufs(2)
        self.junk = sb(f"{tag}_junk", [128, D], F32)
        self.jb = P.buf()
        self.ss = [sb(f"{tag}_ss{i}", [128, 2], F32) for i in range(2)]
        self.sb_ = P.bufs(2)
        self.hn = [sb(f"{tag}_hn{i}", [128, D], F32 if want_f32 else BF16) for i in range(2)]
        self.hb = P.bufs(2)
        self.h32 = [sb(f"{tag}_h32{i}", [128, D], F32) for i in range(2)]
        self.h32b = P.bufs(2)
        if want_f32:
            self.pt = [ps(f"{tag}_pt{i}", [128, 512], F32) for i in range(2)]
            self.ptb = P.bufs(2)
        else:
            self.pt = [ps(f"{tag}_pt", [128, D], BF16)]
            self.ptb = P.bufs(1)
        self.n = 0

    def run(self, x_ap, sec, out_bf, out_bf_buf, out_f32=None, out_f32_buf=None, q="sp"):
        st = self.run_a1(x_ap, sec, q)
        self.run_a2(st, out_bf, out_bf_buf, out_f32, out_f32_buf)

    def run_a1(self, x_ap, sec, q="sp"):
        P, K = self.P, self.K
        i = self.n % 2
        self.n += 1
        xt, ss, hn, h32 = self.xt[i], self.ss[i], self.hn[i], self.h32[i]
        SH = K.modb[:, sec * D:(sec + 1) * D]
        G = K.modb[:, (sec + 1) * D:(sec + 2) * D]
        P.dma(q, xt[:], x_ap, writes=[self.xb[i]])
        P.op("act", lambda e: e.activation(out=self.junk[:], in_=xt[:], func=AF.Square, accum_out=ss[:, 0:1]), [self.xb[i]], [self.jb, self.sb_[i]])
        P.op("act", lambda e: e.activation(out=ss[:, 1:2], in_=ss[:, 0:1], func=AF.Sqrt, scale=1.0 / D, bias=EPS), [self.sb_[i]], [self.sb_[i]])
        P.op("dve", lambda e: e.reciprocal(out=ss[:, 1:2], in_=ss[:, 1:2]), [self.sb_[i]], [self.sb_[i]])
        P.op("dve", lambda e: e.scalar_tensor_tensor(out=h32[:], in0=xt[:], scalar=ss[:, 1:2], in1=G, op0=ALU.mult, op1=ALU.mult),
             [self.xb[i], self.sb_[i]], [self.h32b[i]])
        P.op("pool", lambda e: e.tensor_tensor(out=hn[:], in0=h32[:], in1=SH, op=ALU.add), [self.h32b[i]], [self.hb[i]])
        return i

    def run_a2(self, i, out_bf, out_bf_buf, out_f32=None, out_f32_buf=None):
        P, K = self.P, self.K
        hn = self.hn[i]
        if self.want_f32:
            for half in range(2):
                for k in range(4):
                    kc = half * 4 + k
                    P.op("pe", lambda e, kc=kc, k=k, half=half: e.transpose(self.pt[half][:, k * 128:(k + 1) * 128], hn[:, kc * 128:(kc + 1) * 128], K.ident32[:]),
                         [self.hb[i]], [self.ptb[half]])
                P.op("act", lambda e, half=half: e.copy(out=out_bf[:, half * 4:(half + 1) * 4, :], in_=self.pt[half][:].rearrange("p (k t) -> p k t", k=4)),
                     [self.ptb[half]], [out_bf_buf])
                P.op("dve", lambda e, half=half: e.tensor_copy(out=out_f32[:, half * 4:(half + 1) * 4, :], in_=self.pt[half][:].rearrange("p (k t) -> p k t", k=4)),
                     [self.ptb[half]], [out_f32_buf])
        else:
            for kc in range(8):
                P.op("pe", lambda e, kc=kc: e.transpose(self.pt[0][:, kc * 128:(kc + 1) * 128], hn[:, kc * 128:(kc + 1) * 128], K.identb[:]),
                     [self.hb[i]], [self.ptb[0]])
            P.op("act", lambda e: e.copy(out=out_bf, in_=self.pt[0][:].rearrange("p (k t) -> p k t", k=8)), [self.ptb[0]], [out_bf_buf])


def load_bcast(P, sb, name, src_row_ap, n, q="sp", dt=F32):
    t = sb(name, [128, n], dt)
    b = P.buf()
    P.dma(q, t[:], src_row_ap.partition_broadcast(128), writes=[b])
    return t, b


def emit_sincos_reduce(P, t_ap, tmp_i, tmp_f, bufs_rw, shift):
    b = bufs_rw
    P.op("dve", lambda e: e.tensor_scalar(out=t_ap, in0=t_ap, scalar1=float(1.0 / (2 * math.pi)), scalar2=float(shift / (2 * math.pi)), op0=ALU.mult, op1=ALU.add), b, b)
    P.op("dve", lambda e: e.tensor_copy(out=tmp_i, in_=t_ap), b, b)
    P.op("dve", lambda e: e.tensor_copy(out=tmp_f, in_=tmp_i), b, b)
    P.op("dve", lambda e: e.tensor_tensor(out=t_ap, in0=t_ap, in1=tmp_f, op=ALU.subtract), b, b)
    P.op("dve", lambda e: e.tensor_scalar(out=tmp_f, in0=t_ap, scalar1=0.0, scalar2=None, op0=ALU.is_lt), b, b)
    P.op("dve", lambda e: e.tensor_tensor(out=t_ap, in0=t_ap, in1=tmp_f, op=ALU.add), b, b)
    P.op("dve", lambda e: e.tensor_scalar(out=t_ap, in0=t_ap, scalar1=float(2 * math.pi), scalar2=float(-math.pi), op0=ALU.mult, op1=ALU.add), b, b)
    P.op("dve", lambda e: e.tensor_scalar(out=t_ap, in0=t_ap, scalar1=3.1415925, scalar2=-3.1415925, op0=ALU.min, op1=ALU.max), b, b)


import os
CUT = float(os.environ.get("CUT", "9999"))
NTT = int(os.environ.get("NTT", "32"))


class _Cut(Exception):
    pass


def cut(n):
    if n >= CUT:
        raise _Cut()


def phase_mla_proj(K):
    I = K.I

    def body(P, sb, ps):
        try:
            body2(P, sb, ps)
        except _Cut:
            pass

    def body2(P, sb, ps):
        nctx = NormCtx(P, sb, ps, K, want_f32=False, tag="mn")
        wdn = sb("wdn", [128, 8, 448], BF16); wdn_b = P.buf()
        P.dma("pool", wdn[:], I["mla_w_down"].rearrange("(kc p) n -> p kc n", p=128), writes=[wdn_b])
        wuq = sb("wuq", [128, 2, 1536], BF16); wuq_b = P.buf()
        P.dma("pool", wuq[:], I["mla_w_uq"].rearrange("(kc p) n -> p kc n", p=128), writes=[wuq_b])
        wukv = sb("wukv", [128, 2048], BF16); wukv_b = P.buf()
        P.dma("pool", wukv[:], I["mla_w_ukv"], writes=[wukv_b])
        gqa, gqa_b = load_bcast(P, sb, "gqa", I["mla_q_a_g"], 256)
        gkva, gkva_b = load_bcast(P, sb, "gkva", I["mla_kv_a_g"], 128)
        gq, gq_b = load_bcast(P, sb, "gq", I["mla_q_norm_g"], 192)
        gk, gk_b = load_bcast(P, sb, "gk", I["mla_k_norm_g"], 192)
        invf, invf_b = load_bcast(P, sb, "invf", I["inv_freq"], 32)
        gqk = sb("gqk", [128, 128], F32); gqk_b = P.buf()
        P.op("dve", lambda e: e.tensor_tensor(out=gqk[:], in0=gq[:, 0:128], in1=gk[:, 0:128], op=ALU.mult), [gq_b, gk_b], [gqk_b])
        dv3 = sb("dv3", [128, 3], F32); dv_b = P.buf()
        for j, v in enumerate((1.0 / 256, 1.0 / 128, 1.0 / 64)):
            P.op("dve", lambda e, j=j, v=v: e.memset(dv3[:, j:j + 1], v), [], [dv_b])
        dv16 = sb("dv16", [128, 16], F32)
        P.op("dve", lambda e: e.memset(dv16[:, 0:8], 1.0 / 128), [], [dv_b])
        P.op("dve", lambda e: e.memset(dv16[:, 8:16], 1.0 / 64), [], [dv_b])
        posi = sb("posi", [128, NT], I32); posf = sb("posf", [128, NT], F32); pos_b = P.buf()
        P.dma("sp", posi[:], I["pos_t"], writes=[pos_b])
        P.op("dve", lambda e: e.tensor_copy(out=posf[:], in_=posi[:]), [pos_b], [pos_b])
        cs = sb("cs", [128, NT, 2, 32], F32); cs_b = P.buf()
        tmpi = sb("rtmpi", [128, NT, 2, 32], I32); tmpf = sb("rtmpf", [128, NT, 2, 32], F32)
        for tt in range(NT):
            for j in range(2):
                P.op("dve", lambda e, tt=tt, j=j: e.tensor_scalar(out=cs[:, tt, j, :], in0=invf[:], scalar1=posf[:, tt:tt + 1], scalar2=None, op0=ALU.mult),
                     [pos_b, invf_b], [cs_b])
        emit_sincos_reduce(P, cs[:, :, 0, :], tmpi[:, :, 0, :], tmpf[:, :, 0, :], [cs_b], 1.5 * math.pi)
        emit_sincos_reduce(P, cs[:, :, 1, :], tmpi[:, :, 1, :], tmpf[:, :, 1, :], [cs_b], math.pi)
        P.op("act", lambda e: e.activation(out=cs[:], in_=cs[:], func=AF.Sin), [cs_b], [cs_b])

        hT = [sb(f"hT{i}", [128, 8, 128], BF16) for i in range(2)]; hT_b = P.bufs(2)
        p_lat = ps("p_lat", [128, 512], F32); lat_b = P.buf()
        p_tr = ps("p_tr", [128, 1024], BF16); tr_b = P.buf()
        p_tr2 = ps("p_tr2", [128, 1024], BF16); tr2_b = P.buf()
        p_q = [ps(f"p_q{i}", [128, 512], F32) for i in range(3)]; q_b = P.bufs(3)
        p_kv = [ps("p_kv0", [128, 512], F32), p_lat]; kv_b = [P.buf(), lat_b]; kvser_b = P.buf()
        ss3_l = [sb(f"ss3{i}", [128, 3], F32) for i in range(2)]; ss3_bl = P.bufs(2)
        junk_l = [sb("junk448", [128, 1536], F32)] * 2; junk_bl = [P.buf()] * 2
        cqn_l = [sb(f"cqn{i}", [128, 384], BF16) for i in range(2)]; cqn_bl = P.bufs(2)
        kpn_l = [sb(f"kpn{i}", [128, 64], F32) for i in range(2)]; kpn_bl = P.bufs(2)
        cT_l = [sb(f"cT{i}", [128, 3, 128], BF16) for i in range(2)]; cT_bl = P.bufs(2)
        sqq_l = [sb(f"sqq{i}", [128, 1536], F32) for i in range(2)]; sqq_bl = P.bufs(2)
        rq_l = [sb(f"rq{i}", [128, 16], F32) for i in range(2)]; rq_bl = P.bufs(2)
        qn_l = [sb(f"qn{i}", [128, 8, 128], BF16) for i in range(2)]; qn_bl = P.bufs(2)
        qr_l = [sb(f"qr{i}", [128, 8, 64], F32) for i in range(2)]; qr_bl = P.bufs(2)
        qr2_l = [sb(f"qr2{i}", [128, 8, 64], F32) for i in range(2)]; qr2_bl = P.bufs(2)
        qpe_l = [sb(f"qpe{i}", [128, 8, 64], BF16) for i in range(2)]; qpe_bl = P.bufs(2)
        kraw_l = [sb(f"kraw{i}", [128, 8, 128], F32) for i in range(2)]; kraw_bl = P.bufs(2)
        sqk_l = [sb("sqk", [128, 8, 128], F32)] * 2; sqk_bl = [P.buf()] * 2
        rk_l = [sb(f"rk{i}", [128, 8], F32) for i in range(2)]; rk_bl = P.bufs(2)
        kn_l = [sb(f"kn{i}", [128, 8, 128], BF16) for i in range(2)]; kn_bl = P.bufs(2)
        kpe_l = [sb(f"kpe{i}", [128, 64], BF16) for i in range(2)]; kpe_bl = P.bufs(2)
        kp2_l = [sb(f"kp2{i}", [128, 64], F32) for i in range(2)]; kp2_bl = P.bufs(2)
        vext = [sb(f"vext{i}", [128, 8, 130], BF16) for i in range(2)]; vext_b = P.bufs(2)
        for i in range(2):
            P.op("pool", lambda e, i=i: e.memset(vext[i][:], 1.0), [], [vext_b[i]])
        sQN = [sb(f"sQN{i}", [128, 8, 512], BF16) for i in range(2)]; sQN_b = P.bufs(2)
        sKN = [sb(f"sKN{i}", [128, 8, 512], BF16) for i in range(2)]; sKN_b = P.bufs(2)
        sQP = [sb(f"sQP{i}", [128, 4, 512], BF16) for i in range(2)]; sQP_b = P.bufs(2)
        sKP = [sb(f"sKP{i}", [64, 512], BF16) for i in range(2)]; sKP_b = P.bufs(2)

        def rope(src, dst, cosv, sinv, nh, tmp, rb, wb):
            x1 = src[:, :, 0:32] if nh else src[:, 0:32]
            x2 = src[:, :, 32:64] if nh else src[:, 32:64]
            t1 = tmp[:, :, 0:32] if nh else tmp[:, 0:32]
            t2 = tmp[:, :, 32:64] if nh else tmp[:, 32:64]
            d1 = dst[:, :, 0:32] if nh else dst[:, 0:32]
            d2 = dst[:, :, 32:64] if nh else dst[:, 32:64]
            if nh:
                cb = cosv.unsqueeze(1).to_broadcast([128, nh, 32])
                sn = sinv.unsqueeze(1).to_broadcast([128, nh, 32])
            else:
                cb, sn = cosv, sinv
            P.op("dve", lambda e: e.tensor_tensor(out=t1, in0=x2, in1=sn, op=ALU.mult), rb, wb)
            P.op("dve", lambda e: e.tensor_tensor(out=t2, in0=x1, in1=sn, op=ALU.mult), rb, wb)
            P.op("dve", lambda e: e.tensor_tensor(out=x1, in0=x1, in1=cb, op=ALU.mult), rb, rb)
            P.op("dve", lambda e: e.tensor_tensor(out=x2, in0=x2, in1=cb, op=ALU.mult), rb, rb)
            P.op("dve", lambda e: e.tensor_tensor(out=d1, in0=x1, in1=t1, op=ALU.subtract), rb + wb, wb + [P.buf()] if False else wb)
            P.op("dve", lambda e: e.tensor_tensor(out=d2, in0=x2, in1=t2, op=ALU.add), rb + wb, wb)

        cut(1)

        def do_tile(tt):
            ss3, ss3_b = ss3_l[tt % 2], ss3_bl[tt % 2]
            junk, junk_b = junk_l[tt % 2], junk_bl[tt % 2]
            cqn, cqn_b = cqn_l[tt % 2], cqn_bl[tt % 2]
            kpn, kpn_b = kpn_l[tt % 2], kpn_bl[tt % 2]
            cT, cT_b = cT_l[tt % 2], cT_bl[tt % 2]
            sqq, sqq_b = sqq_l[tt % 2], sqq_bl[tt % 2]
            rq, rq_b = rq_l[tt % 2], rq_bl[tt % 2]
            qn, qn_b = qn_l[tt % 2], qn_bl[tt % 2]
            qr, qr_b = qr_l[tt % 2], qr_bl[tt % 2]
            qr2, qr2_b = qr2_l[tt % 2], qr2_bl[tt % 2]
            qpe, qpe_b = qpe_l[tt % 2], qpe_bl[tt % 2]
            kraw, kraw_b = kraw_l[tt % 2], kraw_bl[tt % 2]
            sqk, sqk_b = sqk_l[tt % 2], sqk_bl[tt % 2]
            rk, rk_b = rk_l[tt % 2], rk_bl[tt % 2]
            kn, kn_b = kn_l[tt % 2], kn_bl[tt % 2]
            kpe, kpe_b = kpe_l[tt % 2], kpe_bl[tt % 2]
            kp2, kp2_b = kp2_l[tt % 2], kp2_bl[tt % 2]
            hi = tt % 2
            g = (tt // 4) % 2
            j4 = tt % 4
            nctx.run(K.xs[tt * 128:(tt + 1) * 128, :], 0, hT[hi][:], hT_b[hi])
            yield
            for kc in range(8):
                P.op("pe", lambda e, kc=kc, hi=hi: e.matmul(p_lat[:, 0:448], lhsT=hT[hi][:, kc, :], rhs=wdn[:, kc, :], start=(kc == 0), stop=(kc == 7)),
                     [hT_b[hi], wdn_b], [lat_b])
            for j, (a, b) in enumerate(((0, 256), (256, 384), (384, 448))):
                P.op("act", lambda e, j=j, a=a, b=b: e.activation(out=junk[:, a:b], in_=p_lat[:, a:b], func=AF.Square, accum_out=ss3[:, j:j + 1]),
                     [lat_b], [junk_b, ss3_b])
            P.op("dve", lambda e: e.tensor_tensor(out=ss3[:], in0=ss3[:], in1=dv3[:], op=ALU.mult), [ss3_b, dv_b], [ss3_b])
            P.op("act", lambda e: e.activation(out=ss3[:], in_=ss3[:], func=AF.Sqrt, bias=EPS), [ss3_b], [ss3_b])
            P.op("dve", lambda e: e.reciprocal(out=ss3[:], in_=ss3[:]), [ss3_b], [ss3_b])
            P.op("dve", lambda e: e.scalar_tensor_tensor(out=cqn[:, 0:256], in0=p_lat[:, 0:256], scalar=ss3[:, 0:1], in1=gqa[:], op0=ALU.mult, op1=ALU.mult),
                 [lat_b, ss3_b, gqa_b], [cqn_b])
            P.op("dve", lambda e: e.scalar_tensor_tensor(out=cqn[:, 256:384], in0=p_lat[:, 256:384], scalar=ss3[:, 1:2], in1=gkva[:], op0=ALU.mult, op1=ALU.mult),
                 [lat_b, ss3_b, gkva_b], [cqn_b])
            P.op("dve", lambda e: e.scalar_tensor_tensor(out=kpn[:], in0=p_lat[:, 384:448], scalar=ss3[:, 2:3], in1=gk[:, 128:192], op0=ALU.mult, op1=ALU.mult),
                 [lat_b, ss3_b, gk_b], [kpn_b])
            cut(3)
            rope(kpn, kpe, cs[:, tt, 0, :], cs[:, tt, 1, :], 0, kp2, [kpn_b, cs_b], [kpe_b, kp2_b])
            cut(4)
            for j in range(3):
                P.op("pe", lambda e, j=j: e.transpose(p_tr2[:, j * 128:(j + 1) * 128], cqn[:, j * 128:(j + 1) * 128], K.identb[:]), [cqn_b], [tr2_b])
            P.op("act", lambda e: e.copy(out=cT[:], in_=p_tr2[:, 0:384].rearrange("p (k t) -> p k t", k=3)), [tr2_b], [cT_b])
            yield
            for n3 in range(3):
                for kc in range(2):
                    P.op("pe", lambda e, n3=n3, kc=kc: e.matmul(p_q[n3][:], lhsT=cT[:, kc, :], rhs=wuq[:, kc, n3 * 512:(n3 + 1) * 512], start=(kc == 0), stop=(kc == 1)),
                         [cT_b, wuq_b], [q_b[n3]])
            cut(5)
            for n3 in range(3):
                P.op("act", lambda e, n3=n3: e.activation(out=sqq[:, n3 * 512:(n3 + 1) * 512], in_=p_q[n3][:], func=AF.Square), [q_b[n3]], [sqq_b])
            sq3 = sqq[:].rearrange("p (h d) -> p h d", h=8)
            P.op("dve", lambda e: e.tensor_reduce(out=rq[:, 0:8], in_=sq3[:, :, 0:128], axis=AX.X, op=ALU.add), [sqq_b], [rq_b])
            P.op("dve", lambda e: e.tensor_reduce(out=rq[:, 8:16], in_=sq3[:, :, 128:192], axis=AX.X, op=ALU.add), [sqq_b], [rq_b])
            P.op("dve", lambda e: e.tensor_tensor(out=rq[:], in0=rq[:], in1=dv16[:], op=ALU.mult), [rq_b, dv_b], [rq_b])
            P.op("act", lambda e: e.activation(out=rq[:], in_=rq[:], func=AF.Sqrt, bias=EPS), [rq_b], [rq_b])
            P.op("dve", lambda e: e.reciprocal(out=rq[:], in_=rq[:]), [rq_b], [rq_b])
            for h in range(8):
                for (lo, hi_, kind) in ((h * 192, h * 192 + 128, 0), (h * 192 + 128, h * 192 + 192, 1)):
                    c0 = lo
                    while c0 < hi_:
                        bnk = c0 // 512
                        c1 = min(hi_, (bnk + 1) * 512)
                        off = c0 - lo
                        w = c1 - c0
                        src = p_q[bnk][:, c0 - bnk * 512:c1 - bnk * 512]
                        if kind == 0:
                            P.op("dve", lambda e, src=src, h=h, off=off, w=w: e.tensor_scalar(out=qn[:, h, off:off + w], in0=src, scalar1=rq[:, h:h + 1], scalar2=None, op0=ALU.mult),
                                 [q_b[bnk], rq_b], [qn_b])
                        else:
                            P.op("dve", lambda e, src=src, h=h, off=off, w=w: e.scalar_tensor_tensor(out=qr[:, h, off:off + w], in0=src, scalar=rq[:, 8 + h:9 + h], in1=gq[:, 128 + off:128 + off + w], op0=ALU.mult, op1=ALU.mult),
                                 [q_b[bnk], rq_b, gq_b], [qr_b])
                        c0 = c1
            cut(6)
            rope(qr, qpe, cs[:, tt, 0, :], cs[:, tt, 1, :], 8, qr2, [qr_b, cs_b], [qpe_b, qr2_b])
            cut(7)
            vi = tt % 2
            for j in range(4):
                pb_ = p_kv[j % 2]
                P.op("pe", lambda e, j=j, pb_=pb_: e.matmul(pb_[:], lhsT=cT[:, 2, :], rhs=wukv[:, j * 512:(j + 1) * 512], start=True, stop=True),
                     [cT_b, wukv_b], [kv_b[j % 2]])
                kvv = pb_[:].rearrange("p (h d) -> p h d", h=2)
                cut(7.1)
                P.op("act", lambda e, j=j, kvv=kvv: e.activation(out=sqk[:, 2 * j:2 * j + 2, :], in_=kvv[:, :, 0:128], func=AF.Square), [kv_b[j % 2]], [sqk_b, kvser_b])
                cut(7.2)
                P.op("dve", lambda e, j=j, kvv=kvv: e.tensor_copy(out=kraw[:, 2 * j:2 * j + 2, :], in_=kvv[:, :, 0:128]), [kv_b[j % 2]], [kraw_b, kvser_b])
                cut(7.3)
                P.op("act", lambda e, j=j, kvv=kvv, vi=vi: e.copy(out=vext[vi][:, 2 * j:2 * j + 2, 0:128], in_=kvv[:, :, 128:256]), [kv_b[j % 2]], [vext_b[vi], kvser_b])
                cut(7.4 + 0.01 * j)
            cut(7.5)
            P.op("dve", lambda e: e.tensor_reduce(out=rk[:], in_=sqk[:], axis=AX.X, op=ALU.add), [sqk_b], [rk_b])
            cut(7.6)
            P.op("act", lambda e: e.activation(out=rk[:], in_=rk[:], func=AF.Sqrt, scale=1.0 / 128, bias=EPS), [rk_b], [rk_b])
            P.op("dve", lambda e: e.reciprocal(out=rk[:], in_=rk[:]), [rk_b], [rk_b])
            cut(7.7)
            P.op("dve", lambda e: e.tensor_tensor(out=kraw[:], in0=kraw[:], in1=rk[:].unsqueeze(2).to_broadcast([128, 8, 128]), op=ALU.mult), [kraw_b, rk_b], [kraw_b])
            cut(7.8)
            P.op("pool", lambda e: e.tensor_tensor(out=kn[:], in0=kraw[:], in1=gqk[:].unsqueeze(1).to_broadcast([128, 8, 128]), op=ALU.mult), [kraw_b, gqk_b], [kn_b])
            cut(8)
            P.dma("sp", K.V[:, tt * 128:(tt + 1) * 128, :].rearrange("h p e -> p h e"), vext[vi][:], reads=[vext_b[vi]])
            yield
            for h in range(8):
                P.op("pe", lambda e, h=h: e.transpose(p_tr[:, h * 128:(h + 1) * 128], qn[:, h, :], K.identb[:]), [qn_b], [tr_b])
            P.op("act", lambda e, g=g, j4=j4: e.copy(out=sQN[g][:, :, j4 * 128:(j4 + 1) * 128], in_=p_tr[:].rearrange("p (h t) -> p h t", h=8)), [tr_b], [sQN_b[g]])
            for h in range(8):
                P.op("pe", lambda e, h=h: e.transpose(p_tr[:, h * 128:(h + 1) * 128], kn[:, h, :], K.identb[:]), [kn_b], [tr_b])
            P.op("dve", lambda e, g=g, j4=j4: e.tensor_copy(out=sKN[g][:, :, j4 * 128:(j4 + 1) * 128], in_=p_tr[:].rearrange("p (h t) -> p h t", h=8)), [tr_b], [sKN_b[g]])
            qpe2 = qpe[:].rearrange("p (a b) d -> p a (b d)", b=2)
            for a in range(4):
                P.op("pe", lambda e, a=a: e.transpose(p_tr[:, a * 128:(a + 1) * 128], qpe2[:, a, :], K.identb[:]), [qpe_b], [tr_b])
            P.op("pe", lambda e: e.transpose(p_tr[0:64, 512:640], kpe[:], K.identb[:]), [kpe_b], [tr_b])
            P.op("act", lambda e, g=g, j4=j4: e.copy(out=sQP[g][:, :, j4 * 128:(j4 + 1) * 128], in_=p_tr[:, 0:512].rearrange("p (h t) -> p h t", h=4)), [tr_b], [sQP_b[g]])
            P.op("dve", lambda e, g=g, j4=j4: e.tensor_copy(out=sKP[g][:, j4 * 128:(j4 + 1) * 128], in_=p_tr[0:64, 512:640]), [tr_b], [sKP_b[g]])
            cut(10)
            if j4 == 3:
                t0 = (tt // 4) * 512
                P.dma("sp", K.QN[:, :, t0:t0 + 512].rearrange("h d t -> d h t"), sQN[g][:], reads=[sQN_b[g]])
                P.dma("sp", K.KN[:, :, t0:t0 + 512].rearrange("h d t -> d h t"), sKN[g][:], reads=[sKN_b[g]])
                P.dma("sp", K.QP[:, :, t0:t0 + 512].rearrange("h d t -> d h t"), sQP[g][:], reads=[sQP_b[g]])
                P.dma("sp", K.KP[:, t0:t0 + 512], sKP[g][:], reads=[sKP_b[g]])
        gens = [do_tile(tt) for tt in range(NTT)]
        NST = 4
        for step in range(NTT + NST - 1):
            for s_ in range(NST - 1, -1, -1):
                tt = step - s_
                if 0 <= tt < NTT:
                    try:
                        next(gens[tt])
                    except StopIteration:
                        pass
    run_phase(K, body)


def phase_attention(K):
    scale = 192 ** -0.5

    def body(P, sb, ps):
        kp = sb("kp", [64, S], BF16); kp_b = P.buf()
        P.dma("sp", kp[:], K.KP, writes=[kp_b])
        ones = sb("ones_b", [128, 128], BF16); ones_b = P.buf()
        P.op("dve", lambda e: e.memset(ones[:], 1.0), [], [ones_b])
        qn = [sb(f"aqn{i}", [128, S], BF16) for i in range(2)]; qn_b = P.bufs(2)
        kn = [sb(f"akn{i}", [128, S], BF16) for i in range(2)]; kn_b = P.bufs(2)
        qp = [sb(f"aqp{i}", [64, S], BF16) for i in range(2)]; qp_b = P.bufs(2)
        vv = [sb(f"av{i}", [128, NT, 130], BF16) for i in range(2)]; vv_b = P.bufs(2)
        NS = 4
        p_s = [ps(f"p_s{i}", [128, 512], F32) for i in range(NS)]; s_b = P.bufs(NS)
        p_o = [ps(f"p_o{i}", [128, 512], F32) for i in range(2)]; o_b = P.bufs(2)
        p_m = [ps(f"p_m{i}", [128, 512], F32) for i in range(2)]; m_b = P.bufs(2)
        NP = 5
        pt = [sb(f"pt{i}", [128, 512], BF16) for i in range(NP)]; pt_b = P.bufs(NP)
        rinv = [sb(f"rinv{i}", [128, 512], F32) for i in range(2)]; rinv_b = P.bufs(2)
        ost = [sb(f"ost{i}", [128, 512], BF16) for i in range(2)]; ost_b = P.bufs(2)

        def load_head(h):
            hi = h % 2
            P.dma("sp", qn[hi][:], K.QN[h], writes=[qn_b[hi]])
            P.dma("pool", kn[hi][:], K.KN[h], writes=[kn_b[hi]])
            P.dma("sp", qp[hi][:], K.QP[h // 2, (h % 2) * 64:(h % 2) * 64 + 64, :], writes=[qp_b[hi]])
            P.dma("pool", vv[hi][:], K.V[h].rearrange("(kt p) e -> p kt e", p=128), writes=[vv_b[hi]])

        iters = [(h, qg, kt) for h in range(8) for qg in range(8) for kt in range(NT)]

        def emit_s(i):
            h, qg, kt = iters[i]
            hi = h % 2
            si = i % NS
            P.op("pe", lambda e: e.matmul(p_s[si][:], lhsT=kn[hi][:, kt * 128:(kt + 1) * 128], rhs=qn[hi][:, qg * 512:(qg + 1) * 512], start=True, stop=False),
                 [kn_b[hi], qn_b[hi]], [s_b[si]])
            P.op("pe", lambda e: e.matmul(p_s[si][:], lhsT=kp[:, kt * 128:(kt + 1) * 128], rhs=qp[hi][:, qg * 512:(qg + 1) * 512], start=False, stop=True),
                 [kp_b, qp_b[hi]], [s_b[si]])

        def emit_rest(i):
            h, qg, kt = iters[i]
            hi = h % 2
            si = i % NS
            pi = i % NP
            oi = (h * 8 + qg) % 2
            P.op("act", lambda e: e.activation(out=pt[pi][:], in_=p_s[si][:], func=AF.Exp, scale=scale), [s_b[si]], [pt_b[pi]])
            P.op("pe", lambda e: e.matmul(p_o[oi][:], lhsT=vv[hi][:, kt, 0:128], rhs=pt[pi][:], start=(kt == 0), stop=(kt == NT - 1)),
                 [pt_b[pi], vv_b[hi]], [o_b[oi]])
            P.op("pe", lambda e: e.matmul(p_m[oi][:], lhsT=ones[:], rhs=pt[pi][:], start=(kt == 0), stop=(kt == NT - 1)),
                 [pt_b[pi], ones_b], [m_b[oi]])
            if kt == NT - 1:
                P.op("dve", lambda e: e.reciprocal(out=rinv[oi][:], in_=p_m[oi][:]), [m_b[oi]], [rinv_b[oi]])
                P.op("dve", lambda e: e.tensor_tensor(out=ost[oi][:], in0=p_o[oi][:], in1=rinv[oi][:], op=ALU.mult), [o_b[oi], rinv_b[oi]], [ost_b[oi]])
                P.dma("sp", K.OT[h, :, qg * 512:(qg + 1) * 512], ost[oi][:], reads=[ost_b[oi]])

        LOOK = 3
        load_head(0)
        load_head(1)
        n = len(iters)
        for i in range(min(LOOK, n)):
            emit_s(i)
        for i in range(n):
            if i + LOOK < n:
                emit_s(i + LOOK)
            emit_rest(i)
            h, qg, kt = iters[i]
            if qg == 0 and kt == 8 and 1 <= h and h + 1 < 8:
                load_head(h + 1)
    run_phase(K, body)


def phase_outproj(K, src_bf, w_ap, layer, src_fm=None):
    def body(P, sb, ps):
        wo = sb("wo", [128, 8, D], BF16); wo_b = P.bufs(8)
        wv = w_ap.rearrange("(kc p) n -> p kc n", p=128)
        for kc in range(8):
            P.dma("pool", wo[:, kc, :], wv[:, kc, :], writes=[wo_b[kc]])
        xt = [sb(f"oxt{i}", [128, D], F32) for i in range(2)]; xt_b = P.bufs(2)
        p_y = [ps(f"p_y{i}", [128, 512], F32) for i in range(4)]; y_b = P.bufs(4)
        tmp = [sb(f"otmp{i}", [128, D], F32) for i in range(2)]; tmp_b = P.bufs(2)
        GATE = K.modb[:, 2 * D:3 * D]
        if src_fm is None:
            ot = [sb(f"ot{i}", [128, D], BF16) for i in range(2)]; ot_b = P.bufs(2)
            oT = [sb(f"oT{i}", [128, 8, 128], BF16) for i in range(2)]; oT_b = P.bufs(2)
            p_tr = ps("p_tr", [128, D], BF16); tr_b = P.buf()
        else:
            og = [sb(f"og{i}", [128, 8, 512], BF16) for i in range(2)]; og_b = P.bufs(2)
        for tt in range(NT):
            i = tt % 2
            P.dma("pool", xt[i][:], K.xs[tt * 128:(tt + 1) * 128, :], writes=[xt_b[i]])
            if src_fm is None:
                P.dma("sp", ot[i][:], src_bf[tt * 128:(tt + 1) * 128, :], writes=[ot_b[i]])
                for kc in range(8):
                    P.op("pe", lambda e, kc=kc, i=i: e.transpose(p_tr[:, kc * 128:(kc + 1) * 128], ot[i][:, kc * 128:(kc + 1) * 128], K.identb[:]), [ot_b[i]], [tr_b])
                P.op("act", lambda e, i=i: e.copy(out=oT[i][:], in_=p_tr[:].rearrange("p (k t) -> p k t", k=8)), [tr_b], [oT_b[i]])
                lhs = lambda kc, i=i: oT[i][:, kc, :]
                lb = oT_b[i]
            else:
                gi = (tt // 4) % 2
                if tt % 4 == 0:
                    t0 = tt * 128
                    P.dma("sp", og[gi][:], src_fm[:, :, t0:t0 + 512].rearrange("h d t -> d h t"), writes=[og_b[gi]])
                lhs = lambda kc, gi=gi, j=tt % 4: og[gi][:, kc, j * 128:(j + 1) * 128]
                lb = og_b[gi]
            for dh in range(2):
                yb = i * 2 + dh
                for kc in range(8):
                    P.op("pe", lambda e, kc=kc, dh=dh, yb=yb, lhs=lhs: e.matmul(p_y[yb][:], lhsT=lhs(kc), rhs=wo[:, kc, dh * 512:(dh + 1) * 512], start=(kc == 0), stop=(kc == 7)),
                         [lb, wo_b[kc]], [y_b[yb]])
                P.op("dve", lambda e, i=i, dh=dh, yb=yb: e.tensor_tensor(out=tmp[i][:, dh * 512:(dh + 1) * 512], in0=p_y[yb][:], in1=GATE[:, dh * 512:(dh + 1) * 512], op=ALU.mult),
                     [y_b[yb]], [tmp_b[i]])
            P.op("dve", lambda e, i=i: e.tensor_tensor(out=xt[i][:], in0=xt[i][:], in1=tmp[i][:], op=ALU.add), [xt_b[i], tmp_b[i]], [xt_b[i]])
            P.dma("sp", K.xs[tt * 128:(tt + 1) * 128, :], xt[i][:], reads=[xt_b[i]])
    run_phase(K, body)


def phase_norm_T(K, sec, router_layer=None):
    I = K.I
    router = router_layer is not None

    def body(P, sb, ps):
        nctx = NormCtx(P, sb, ps, K, want_f32=router, tag="fn")
        hst = [sb(f"hst{i}", [128, 8, 512], BF16) for i in range(2)]; hst_b = P.bufs(2)
        if router:
            layer = router_layer
            h32 = [sb(f"h32T{i}", [128, 8, 128], F32) for i in range(2)]; h32_b = P.bufs(2)
            wr = sb("wr", [128, 8, 20], F32); wr_b = P.buf()
            P.dma("sp", wr[:], I["moe_wr"][layer].rearrange("(kc p) n -> p kc n", p=128), writes=[wr_b])
            br, br_b = load_bcast(P, sb, "br", I["moe_br"][layer:layer + 1, :], 20)
            combT = sb("combT", [16, S], BF16); combT_b = P.buf()
            p_r = ps("p_r", [128, 512], F32); r_b = P.buf()
            lg_l = [sb(f"lg{i}", [128, 20], F32) for i in range(2)]; lg_bl = P.bufs(2)
            m8_l = [sb(f"m8{i}", [128, 8], F32) for i in range(2)]; m8_bl = P.bufs(2)
            gm_l = [sb(f"gmask{i}", [128, 4], F32) for i in range(2)]; gm_bl = P.bufs(2)
            gw_l = [sb(f"gw{i}", [128, 4], F32) for i in range(2)]; gw_bl = P.bufs(2)
            em_l = [sb(f"em{i}", [128, 4, 4], F32) for i in range(2)]; em_bl = P.bufs(2)
            ex_l = [sb(f"ex{i}", [128, 16], F32) for i in range(2)]; ex_bl = P.bufs(2)
            cmb_l = [sb(f"cmb{i}", [128, 16], F32) for i in range(2)]; cmb_bl = P.bufs(2)
            den_l = [sb(f"den{i}", [128, 2], F32) for i in range(2)]; den_bl = P.bufs(2)
            BIG = 1.0e4
        a1 = {}

        def stage_a1(tt):
            a1[tt] = nctx.run_a1(K.xs[tt * 128:(tt + 1) * 128, :], sec)

        def stage_a2(tt):
            i = tt % 2
            g = (tt // 4) % 2
            j4 = tt % 4
            if router:
                nctx.run_a2(a1[tt], hst[g][:, :, j4 * 128:(j4 + 1) * 128], hst_b[g], h32[i][:], h32_b[i])
            else:
                nctx.run_a2(a1[tt], hst[g][:, :, j4 * 128:(j4 + 1) * 128], hst_b[g])
            if j4 == 3:
                t0 = (tt // 4) * 512
                P.dma("sp", K.HT[:, :, t0:t0 + 512], hst[g][:], reads=[hst_b[g]])

        def do_tile(tt):
            if not router:
                return
            lg, lg_b = lg_l[tt % 2], lg_bl[tt % 2]
            m8, m8_b = m8_l[tt % 2], m8_bl[tt % 2]
            gm, gm_b = gm_l[tt % 2], gm_bl[tt % 2]
            gw, gw_b = gw_l[tt % 2], gw_bl[tt % 2]
            em, em_b = em_l[tt % 2], em_bl[tt % 2]
            ex, ex_b = ex_l[tt % 2], ex_bl[tt % 2]
            cmb, cmb_b = cmb_l[tt % 2], cmb_bl[tt % 2]
            den, den_b = den_l[tt % 2], den_bl[tt % 2]
            i = tt % 2
            for kc in range(8):
                P.op("pe", lambda e, kc=kc, i=i: e.matmul(p_r[:, 0:20], lhsT=h32[i][:, kc, :], rhs=wr[:, kc, :], start=(kc == 0), stop=(kc == 7)),
                     [h32_b[i], wr_b], [r_b])
            P.op("dve", lambda e: e.tensor_tensor(out=lg[:], in0=p_r[:, 0:20], in1=br[:], op=ALU.add), [r_b, br_b], [lg_b])
            P.op("dve", lambda e: e.tensor_reduce(out=m8[:, 0:1], in_=lg[:, 0:4], axis=AX.X, op=ALU.max), [lg_b], [m8_b])
            P.op("dve", lambda e: e.tensor_scalar(out=gm[:], in0=lg[:, 0:4], scalar1=m8[:, 0:1], scalar2=None, op0=ALU.is_ge), [lg_b, m8_b], [gm_b])
            P.op("dve", lambda e: e.tensor_scalar(out=gw[:], in0=lg[:, 0:4], scalar1=m8[:, 0:1], scalar2=None, op0=ALU.subtract), [lg_b, m8_b], [gw_b])
            P.op("act", lambda e: e.activation(out=gw[:], in_=gw[:], func=AF.Exp, accum_out=den[:, 0:1]), [gw_b], [gw_b, den_b])
            P.op("dve", lambda e: e.tensor_scalar(out=gm[:], in0=gm[:], scalar1=BIG, scalar2=-BIG, op0=ALU.mult, op1=ALU.add), [gm_b], [gm_b])
            P.op("dve", lambda e: e.tensor_tensor(out=em[:], in0=lg[:, 4:20].rearrange("p (g k) -> p g k", g=4), in1=gm[:].unsqueeze(2).to_broadcast([128, 4, 4]), op=ALU.add),
                 [lg_b, gm_b], [em_b])
            emf = em[:].rearrange("p g k -> p (g k)")
            P.op("dve", lambda e: e.max(out=m8[:], in_=emf), [em_b], [m8_b])
            P.op("dve", lambda e: e.tensor_scalar(out=ex[:], in0=emf, scalar1=m8[:, 0:1], scalar2=None, op0=ALU.subtract), [em_b, m8_b], [ex_b])
            P.op("act", lambda e: e.activation(out=ex[:], in_=ex[:], func=AF.Exp), [ex_b], [ex_b])
            P.op("dve", lambda e: e.tensor_scalar(out=cmb[:], in0=emf, scalar1=m8[:, 1:2], scalar2=None, op0=ALU.is_ge), [em_b, m8_b], [cmb_b])
            P.op("dve", lambda e: e.tensor_tensor(out=cmb[:], in0=cmb[:], in1=ex[:], op=ALU.mult), [cmb_b, ex_b], [cmb_b])
            P.op("dve", lambda e: e.tensor_reduce(out=den[:, 1:2], in_=cmb[:], axis=AX.X, op=ALU.add), [cmb_b], [den_b])
            P.op("dve", lambda e: e.tensor_tensor(out=den[:, 0:1], in0=den[:, 0:1], in1=den[:, 1:2], op=ALU.mult), [den_b], [den_b])
            P.op("dve", lambda e: e.reciprocal(out=den[:, 0:1], in_=den[:, 0:1]), [den_b], [den_b])
            P.op("dve", lambda e: e.tensor_scalar(out=cmb[:], in0=cmb[:], scalar1=den[:, 0:1], scalar2=None, op0=ALU.mult), [cmb_b, den_b], [cmb_b])
            P.op("pe", lambda e: e.transpose(p_r[0:16, 128:256], cmb[:], K.ident32[:]), [cmb_b], [r_b])
            P.op("act", lambda e, tt=tt: e.copy(out=combT[:, tt * 128:(tt + 1) * 128], in_=p_r[0:16, 128:256]), [r_b], [combT_b])
        stage_a1(0)
        stage_a1(1)
        stage_a2(0)
        for tt in range(NT):
            if tt + 2 < NT:
                stage_a1(tt + 2)
            if tt + 1 < NT:
                stage_a2(tt + 1)
            do_tile(tt)
        if router:
            P.dma("sp", K.CT, combT[:], reads=[combT_b])
    run_phase(K, body)


def phase_moe_a(K, layer):
    I = K.I

    def body(P, sb, ps):
        hT = sb("hT_all", [128, 8, S], BF16); hT_b = P.bufs(8)
        for kc in range(8):
            P.dma("sp" if kc % 2 == 0 else "pool", hT[:, kc, :], K.HT[:, kc, :], writes=[hT_b[kc]])
        sel = sb("sel", [16, 16 * 128], BF16); sel_b = P.buf()
        P.dma("sp", sel[:], I["sel"], writes=[sel_b])
        combT = sb("combT", [16, S], BF16); combT_b = P.buf()
        P.dma("sp", combT[:], K.CT, writes=[combT_b])
        wg = [sb(f"wg{i}", [128, 8, DE], BF16) for i in range(2)]; wg_b = P.bufs(2)
        wu = [sb(f"wu{i}", [128, 8, DE], BF16) for i in range(2)]; wu_b = P.bufs(2)
        p_g = [ps(f"p_g{i}", [128, 512], F32) for i in range(2)]; g_b = P.bufs(2)
        p_u = [ps(f"p_u{i}", [128, 512], F32) for i in range(2)]; u_b = P.bufs(2)
        p_c = [ps(f"p_c{i}", [128, 512], F32) for i in range(2)]; c_b = P.bufs(2)
        cb = [sb(f"cb{i}", [128, 512], F32) for i in range(2)]; cb_b = P.bufs(2)
        sg = [sb(f"sg{i}", [128, 512], F32) for i in range(2)]; sg_b = P.bufs(2)
        tu = [sb(f"tu{i}", [128, 512], F32) for i in range(2)]; tu_b = P.bufs(2)
        ast = [sb(f"ast{i}", [128, 2, 512], BF16) for i in range(2)]; ast_b = P.bufs(2)
        it = 0
        ci = 0
        for ex_i in range(NE):
            wi = ex_i % 2
            P.dma("pool", wg[wi][:], I["moe_w_gate"][layer, ex_i].rearrange("(kc p) n -> p kc n", p=128), writes=[wg_b[wi]])
            P.dma("pool", wu[wi][:], I["moe_w_up"][layer, ex_i].rearrange("(kc p) n -> p kc n", p=128), writes=[wu_b[wi]])
            for tq in range(8):
                c_i = ci % 2
                ci += 1
                P.op("pe", lambda e, ex_i=ex_i, tq=tq, c_i=c_i: e.matmul(p_c[c_i][:], lhsT=sel[:, ex_i * 128:(ex_i + 1) * 128], rhs=combT[:, tq * 512:(tq + 1) * 512], start=True, stop=True),
                     [sel_b, combT_b], [c_b[c_i]])
                P.op("act", lambda e, c_i=c_i: e.copy(out=cb[c_i][:], in_=p_c[c_i][:]), [c_b[c_i]], [cb_b[c_i]])
                for hc in range(2):
                    k = it % 2
                    it += 1
                    for kc in range(8):
                        P.op("pe", lambda e, k=k, kc=kc, wi=wi, hc=hc, tq=tq: e.matmul(p_g[k][:], lhsT=wg[wi][:, kc, hc * 128:(hc + 1) * 128], rhs=hT[:, kc, tq * 512:(tq + 1) * 512], start=(kc == 0), stop=(kc == 7)),
                             [wg_b[wi], hT_b[kc]], [g_b[k]])
                    for kc in range(8):
                        P.op("pe", lambda e, k=k, kc=kc, wi=wi, hc=hc, tq=tq: e.matmul(p_u[k][:], lhsT=wu[wi][:, kc, hc * 128:(hc + 1) * 128], rhs=hT[:, kc, tq * 512:(tq + 1) * 512], start=(kc == 0), stop=(kc == 7)),
                             [wu_b[wi], hT_b[kc]], [u_b[k]])
                    P.op("act", lambda e, k=k: e.activation(out=sg[k][:], in_=p_g[k][:], func=AF.Silu), [g_b[k]], [sg_b[k]])
                    P.op("dve", lambda e, k=k: e.tensor_tensor(out=tu[k][:], in0=sg[k][:], in1=p_u[k][:], op=ALU.mult), [sg_b[k], u_b[k]], [tu_b[k]])
                    P.op("pool", lambda e, k=k, c_i=c_i, hc=hc: e.tensor_tensor(out=ast[c_i][:, hc, :], in0=tu[k][:], in1=cb[c_i][:], op=ALU.mult), [tu_b[k], cb_b[c_i]], [ast_b[c_i]])
                P.dma("sp", K.AT[ex_i, :, :, tq * 512:(tq + 1) * 512].rearrange("c p t -> p c t"), ast[c_i][:], reads=[ast_b[c_i]])
    run_phase(K, body)


def phase_moe_b(K, layer):
    I = K.I
    final = (layer == 1)

    def body(P, sb, ps):
        wd = sb("wd", [128, NE, 2, D], BF16); wd_b = P.bufs(NE)
        for e_ in range(NE):
            P.dma("pool", wd[:, e_, :, :], I["moe_w_down"][layer, e_].rearrange("(c p) n -> p c n", p=128), writes=[wd_b[e_]])
        at = [sb(f"at{i}", [128, NE, 2, 512], BF16) for i in range(2)]; at_b = P.bufs(2)
        xt = [sb(f"mxt{i}", [128, D], F32) for i in range(2)]; xt_b = P.bufs(2)
        tmp = [sb(f"mtmp{i}", [128, D], F32) for i in range(2)]; tmp_b = P.bufs(2)
        p_y = [ps(f"p_y{i}", [128, 512], F32) for i in range(4)]; y_b = P.bufs(4)
        GATE = K.modb[:, 5 * D:6 * D]
        dst = K.out if final else K.xs
        for tq in range(8):
            ai = tq % 2
            P.dma("sp", at[ai][:], K.AT[:, :, :, tq * 512:(tq + 1) * 512].rearrange("e c p t -> p e c t"), writes=[at_b[ai]])
            for j in range(4):
                tt = tq * 4 + j
                i = tt % 2
                P.dma("sp", xt[i][:], K.xs[tt * 128:(tt + 1) * 128, :], writes=[xt_b[i]])
                for dh in range(2):
                    yb = i * 2 + dh
                    n = 0
                    for e_ in range(NE):
                        for c in range(2):
                            P.op("pe", lambda e, ai=ai, e_=e_, c=c, j=j, dh=dh, yb=yb, n=n: e.matmul(p_y[yb][:], lhsT=at[ai][:, e_, c, j * 128:(j + 1) * 128], rhs=wd[:, e_, c, dh * 512:(dh + 1) * 512], start=(n == 0), stop=(n == 2 * NE - 1)),
                                 [at_b[ai], wd_b[e_]], [y_b[yb]])
                            n += 1
                    P.op("dve", lambda e, i=i, dh=dh, yb=yb: e.tensor_tensor(out=tmp[i][:, dh * 512:(dh + 1) * 512], in0=p_y[yb][:], in1=GATE[:, dh * 512:(dh + 1) * 512], op=ALU.mult),
                         [y_b[yb]], [tmp_b[i]])
                P.op("pool", lambda e, i=i: e.tensor_tensor(out=xt[i][:], in0=xt[i][:], in1=tmp[i][:], op=ALU.add), [xt_b[i], tmp_b[i]], [xt_b[i]])
                P.dma("sp", dst[tt * 128:(tt + 1) * 128, :], xt[i][:], reads=[xt_b[i]])
    run_phase(K, body)


def phase_hyena_in(K):
    I = K.I

    def body(P, sb, ps):
        hT = sb("hT_all", [128, 8, S], BF16); hT_b = P.bufs(8)
        for kc in range(8):
            P.dma("sp", hT[:, kc, :], K.HT[:, kc, :], writes=[hT_b[kc]])
        bin_ = sb("bin", [128, 24], F32); cw = sb("cw", [128, 3, 24], F32); cbv = sb("cbv", [128, 24], F32); par_b = P.buf()
        P.dma("sp", bin_[:], I["hy_b_in_t"], writes=[par_b])
        P.dma("sp", cw[:], I["hy_conv_w_t"], writes=[par_b])
        P.dma("sp", cbv[:], I["hy_conv_b_t"], writes=[par_b])
        wv = I["hy_w_in"].rearrange("(kc p) n -> p kc n", p=128)
        wi = [sb(f"wi{i}", [128, 8, 128], BF16) for i in range(2)]; wi_b = P.bufs(2)
        p_u = [ps(f"p_u{i}", [128, 512], F32) for i in range(3)]; pu_b = P.bufs(3)
        urow = [sb(f"urow{i}", [128, S + 2], F32) for i in range(2)]; ur_b = P.bufs(2)
        for i in range(2):
            P.op("pool", lambda e, i=i: e.memset(urow[i][:, 0:1], 0.0), [], [ur_b[i]])
            P.op("pool", lambda e, i=i: e.memset(urow[i][:, S + 1:S + 2], 0.0), [], [ur_b[i]])
        ucT = sb("ucT0", [128, S], F32); uc_b = P.buf()
        p_t = [ps(f"p_t{i}", [128, 1024], BF16) for i in range(2)]; pt_b = P.bufs(2)
        stg = [sb(f"ustg{i}", [128, NT, 128], BF16) for i in range(2)]; stg_b = P.bufs(2)
        ucB = [sb(f"ucB{i}", [128, S], BF16) for i in range(2)]; ucB_b = P.bufs(2)
        st = {"it": 0}

        def proj(cc):
            i = cc % 2
            P.dma("pool", wi[i][:], wv[:, :, cc * 128:(cc + 1) * 128], writes=[wi_b[i]])
            for tq in range(8):
                k = st["it"] % 3
                st["it"] += 1
                for kc in range(8):
                    P.op("pe", lambda e, k=k, kc=kc, tq=tq: e.matmul(p_u[k][:], lhsT=wi[i][:, kc, :], rhs=hT[:, kc, tq * 512:(tq + 1) * 512], start=(kc == 0), stop=(kc == 7)),
                         [wi_b[i], hT_b[kc]], [pu_b[k]])
                P.op("act", lambda e, k=k, tq=tq: e.activation(out=urow[i][:, 1 + tq * 512:1 + (tq + 1) * 512], in_=p_u[k][:], func=AF.Identity, bias=bin_[:, cc:cc + 1], scale=1.0),
                     [pu_b[k], par_b], [ur_b[i]])
            P.op("dve", lambda e: e.tensor_scalar(out=ucT[:], in0=urow[i][:, 1:S + 1], scalar1=cw[:, 1, cc:cc + 1], scalar2=cbv[:, cc:cc + 1], op0=ALU.mult, op1=ALU.add),
                 [ur_b[i], par_b], [uc_b])
            P.op("dve", lambda e: e.scalar_tensor_tensor(out=ucT[:], in0=urow[i][:, 0:S], scalar=cw[:, 0, cc:cc + 1], in1=ucT[:], op0=ALU.mult, op1=ALU.add),
                 [ur_b[i], par_b, uc_b], [uc_b])
            P.op("dve", lambda e: e.scalar_tensor_tensor(out=ucB[i][:], in0=urow[i][:, 2:S + 2], scalar=cw[:, 2, cc:cc + 1], in1=ucT[:], op0=ALU.mult, op1=ALU.add),
                 [ur_b[i], par_b, uc_b], [ucB_b[i]])

        def back(cc):
            i = cc % 2
            for t4 in range(8):
                k = t4 % 2
                for j in range(4):
                    tt = t4 * 4 + j
                    P.op("pe", lambda e, k=k, j=j, tt=tt: e.transpose(p_t[k][:, j * 128:(j + 1) * 128], ucB[i][:, tt * 128:(tt + 1) * 128], K.identb[:]), [ucB_b[i]], [pt_b[k]])
                P.op("act", lambda e, k=k, t4=t4: e.copy(out=stg[i][:, t4 * 4:(t4 + 1) * 4, :], in_=p_t[k][:, 0:512].rearrange("p (j c) -> p j c", j=4)), [pt_b[k]], [stg_b[i]])
            P.dma("act", K.UC[:, cc * 128:(cc + 1) * 128].rearrange("(tt p) c -> p tt c", p=128), stg[i][:], reads=[stg_b[i]])

        proj(0)
        for cc in range(24):
            if cc + 1 < 24:
                proj(cc + 1)
            back(cc)
    run_phase(K, body)


def s1_state(P, sb, ps, K, tag):
    st = {"it": 0}
    st["F1"] = sb(tag + "F1", [128, 2, 128], BF16); st["F1_b"] = P.buf()
    P.dma("sp", st["F1"][:], K.I["F1"], writes=[st["F1_b"]])
    st["p"] = [ps(f"{tag}p{i}", [128, 512], F32) for i in range(2)]; st["p_b"] = P.bufs(2)
    return st


def phase_hyena_filter(K):
    I = K.I

    def body(P, sb, ps):
        zT = sb("zT", [33, S], F32); z_b = P.buf()
        P.dma("sp", zT[:], I["zT"], writes=[z_b])
        w1 = sb("fw1", [33, 64], F32); w2 = sb("fw2", [64, 64], F32); w3 = sb("fw3", [64, 64], F32); bf_ = sb("fbf", [64, 6], F32); w_b = P.buf()
        P.dma("sp", w1[:], I["hy_f_w1"], writes=[w_b])
        P.dma("sp", w2[:], I["hy_f_w2"], writes=[w_b])
        P.dma("sp", w3[:], I["hy_f_w3"], writes=[w_b])
        P.dma("sp", bf_[:], I["hy_f_bf_t"], writes=[w_b])
        fb = sb("fb", [64, 3], F32)
        for l in range(3):
            P.op("dve", lambda e, l=l: e.tensor_tensor(out=fb[:, l:l + 1], in0=bf_[:, 2 * l:2 * l + 1], in1=bf_[:, 2 * l + 1:2 * l + 2], op=ALU.mult), [w_b], [w_b])
        hd = [sb(f"hd{i}", [64, S], F32) for i in range(2)]; hd_b = P.bufs(2)
        hdb = sb("hdb", [64, S], BF16); hdb_b = P.buf()
        ti = sb("fti", [64, 512], I32); tf = sb("ftf", [64, 512], F32); tmp_b = P.buf()
        p_h = ps("p_h", [128, 512], F32); ph_b = P.buf()
        srcs = [(zT, z_b, w1, 33), (hd[0], hd_b[0], w2, 64), (hd[1], hd_b[1], w3, 64)]
        for l in range(3):
            src, src_b, wl, kdim = srcs[l]
            dst, dst_b = (hd[l % 2], hd_b[l % 2])
            for ch in range(8):
                sl = slice(ch * 512, (ch + 1) * 512)
                P.op("pe", lambda e, src=src, wl=wl, kdim=kdim, sl=sl: e.matmul(p_h[0:64, :], lhsT=wl[0:kdim, :], rhs=src[0:kdim, sl], start=True, stop=True), [src_b, w_b], [ph_b])
                P.op("dve", lambda e, dst=dst, sl=sl, l=l: e.tensor_scalar(out=dst[:, sl], in0=p_h[0:64, :], scalar1=bf_[:, 2 * l + 1:2 * l + 2], scalar2=fb[:, l:l + 1], op0=ALU.mult, op1=ALU.add),
                     [ph_b, w_b], [dst_b])
                emit_sincos_reduce(P, dst[:, sl], ti[:], tf[:], [dst_b, tmp_b], math.pi + 64 * math.pi)
            P.op("act", lambda e, dst=dst: e.activation(out=dst[:], in_=dst[:], func=AF.Sin), [dst_b], [dst_b])
        P.op("dve", lambda e: e.tensor_copy(out=hdb[:], in_=hd[0][:]), [hd_b[0]], [hdb_b])
        if "hdn" in K.dbg:
            P.dma("sp", K.dbg["hdn"], hd[0][:], reads=[hd_b[0]])
        w4 = sb("fw4", [64, 4 * D], BF16); w4_b = P.buf()
        P.dma("pool", w4[:], I["hy_f_w4"], writes=[w4_b])
        dl, dl_b = load_bcast(P, sb, "deltas", I["deltas"], D)
        ntl = sb("ntl", [128, 32], F32); ntl_b = P.buf()
        P.dma("sp", ntl[:], I["ntl"], writes=[ntl_b])
        msk = sb("msk", [128, 4, 128], F32); msk_b = P.buf()
        P.dma("sp", msk[:], I["mask4"], writes=[msk_b])
        dec = [sb(f"dec{i}", [128, D], F32) for i in range(2)]; dec_b = P.bufs(2)
        asum = sb("asum", [128, 2 * D], F32); as_b = P.buf()
        fa = [sb(f"fa{i}", [128, 2 * D], BF16) for i in range(2)]; fa_b = P.bufs(2)
        absf = [sb("absf0", [128, 2 * D], F32)] * 2; absf_b = [P.buf()] * 2
        p_f = [ps(f"p_f{i}", [128, 512], F32) for i in range(2)]; pf_b = P.bufs(2)
        hdv = hdb[:].rearrange("w (p a) -> w a p", a=32)
        st = s1_state(P, sb, ps, K, "fs1")
        fstg = [sb(f"fstg{i}", [128, 2, 2, D], BF16) for i in range(2)]; fstg_b = P.bufs(2)
        ad_b = P.bufs(32)
        rn2 = sb("rn2", [128, 2, 256], F32); rn2_b = P.buf()
        gbt = [sb(f"gbt{i}", [128, 4, 4, 128], BF16) for i in range(2)]; gbt_b = P.bufs(2)
        ain = [sb(f"ain{i}", [128, 2, 2, 4, 256], BF16) for i in range(2)]; ain_b = [P.bufs(4) for _ in range(2)]
        ains = [sb(f"ains{i}", [128, 2, 2, 4, 256], BF16) for i in range(2)]; ains_b = [P.bufs(2) for _ in range(2)]
        p_k = [ps(f"p_k{i}", [128, 2, 256], F32) for i in range(2)]; pk_b = P.bufs(2)
        p_n = ps("p_n", [128, 2, 256], F32); pn_b = P.buf()
        kst = [sb(f"kst{i}", [128, 4, 2, 256], BF16) for i in range(2)]; kst_b = P.bufs(2)
        it = 0
        for o in range(2):
            P.op("pool", lambda e: e.memset(asum[:], 0.0), [], [as_b])
            sp4 = st["p"] + [p_k[0][:].rearrange("p r g -> p (r g)"), p_k[1][:].rearrange("p r g -> p (r g)")]
            sp4 = [t if not hasattr(t, "ap") or True else t for t in sp4]
            sp4_b = st["p_b"] + pk_b

            def gen(a):
                nonlocal it
                di = a % 2
                P.op("act", lambda e: e.activation(out=dec[di][:], in_=dl[:], func=AF.Exp, scale=ntl[:, a:a + 1]), [dl_b, ntl_b], [dec_b[di]])
                for dr in range(2):
                    for chh in range(2):
                        col = (o * 2 + dr) * D + chh * 512
                        k = it % 2
                        it += 1
                        P.op("pe", lambda e, k=k, col=col: e.matmul(p_f[k][:], lhsT=hdv[:, a, :], rhs=w4[:, col:col + 512], start=True, stop=True), [hdb_b, w4_b], [pf_b[k]])
                        lc = dr * D + chh * 512
                        P.op("dve", lambda e, k=k, lc=lc, chh=chh: e.tensor_tensor(out=fa[di][:, lc:lc + 512], in0=p_f[k][:], in1=dec[di][:, chh * 512:(chh + 1) * 512], op=ALU.mult),
                             [pf_b[k], dec_b[di]], [fa_b[di]])
                P.op("act", lambda e: e.activation(out=absf[di][:], in_=fa[di][:], func=AF.Abs), [fa_b[di]], [absf_b[di]])
                P.op("pool", lambda e: e.tensor_tensor(out=asum[:], in0=asum[:], in1=absf[di][:], op=ALU.add), [absf_b[di], as_b], [as_b])
                if a == 0:
                    P.op("pool", lambda e: e.memset(fa[di][0:1, D:2 * D], 0.0), [as_b, fa_b[di]], [fa_b[di]])

            def s1(a):
                di = a % 2
                g = a % 2
                for dr in range(2):
                    for chh in range(2):
                        lc = dr * D + chh * 512
                        for ri in range(2):
                            k = st["it"] % 4
                            st["it"] += 1
                            pk = sp4[k]
                            pkv = pk if k >= 2 else pk[:]
                            P.op("pe", lambda e, pkv=pkv, ri=ri, lc=lc: e.matmul(pkv, lhsT=st["F1"][:, ri, :], rhs=fa[di][:, lc:lc + 512], start=True, stop=True),
                                 [fa_b[di], st["F1_b"]], [sp4_b[k]])
                            if ri == 0:
                                P.op("act", lambda e, pkv=pkv, dr=dr, ri=ri, chh=chh: e.copy(out=fstg[g][:, dr, ri, chh * 512:(chh + 1) * 512], in_=pkv), [sp4_b[k]], [fstg_b[g]])
                            else:
                                P.op("dve", lambda e, pkv=pkv, dr=dr, ri=ri, chh=chh: e.tensor_copy(out=fstg[g][:, dr, ri, chh * 512:(chh + 1) * 512], in_=pkv), [sp4_b[k]], [fstg_b[g]])
                P.dma("sp", K.Ad[:, :, :, a * D:(a + 1) * D].rearrange("s r k n -> k s r n"), fstg[g][:], reads=[fstg_b[g]], writes=[ad_b[a]])

            gen(0)
            for a in range(32):
                if a + 1 < 32:
                    gen(a + 1)
                s1(a)
            for dr in range(2):
                for c4 in range(4):
                    P.op("pe", lambda e, dr=dr, c4=c4: e.matmul(p_n[:, dr, :], lhsT=msk[:, c4, :], rhs=asum[:, dr * D + c4 * 256:dr * D + (c4 + 1) * 256], start=(c4 == 0), stop=(c4 == 3)),
                         [msk_b, as_b], [pn_b])
            P.op("dve", lambda e: e.tensor_scalar(out=rn2[:], in0=p_n[:], scalar1=EPS, scalar2=None, op0=ALU.add), [pn_b], [rn2_b])
            P.op("dve", lambda e: e.reciprocal(out=rn2[:], in_=rn2[:]), [rn2_b], [rn2_b])
            if "rn2" in K.dbg and o == 0:
                P.dma("sp", K.dbg["rn2"], rn2[:], reads=[rn2_b])
            re_terms = [(0, 0, 0), (2, 0, 1), (0, 1, 0), (2, 1, 1)]
            im_terms = [(1, 0, 0), (0, 0, 1), (2, 1, 0), (3, 1, 1)]
            KB = 4
            for grp in range(128 // KB):
                gi = grp % 2
                k0 = grp * KB
                P.dma("sp", gbt[gi][:], K.I["GB"][:, k0:k0 + KB, 0:4, :], writes=[gbt_b[gi]])
                for s_ in range(2):
                    for r_ in range(2):
                        P.dma("act" if r_ == 1 else "sp", ain[gi][:, s_, r_, :, :], K.Ad[s_, r_, k0:k0 + KB, :].rearrange("k (q g) -> q k g", g=256), reads=ad_b, writes=[ain_b[gi][s_ * 2 + r_]])
                for s_ in range(2):
                    P.op("dve", lambda e, gi=gi, s_=s_: e.tensor_tensor(out=ains[gi][:, s_].rearrange("p r k g -> p (r k) g"), in0=ain[gi][:, s_].rearrange("p r k g -> p (r k) g"),
                                                                       in1=rn2[:, s_:s_ + 1, :].to_broadcast([128, 2 * KB, 256]), op=ALU.mult),
                         ain_b[gi][s_ * 2:s_ * 2 + 2] + [rn2_b], [ains_b[gi][s_]])
                for kl in range(KB):
                    k1 = k0 + kl
                    kk = k1 % 2
                    for ri, terms in ((0, re_terms), (1, im_terms)):
                        for n, (mi, s_, r_) in enumerate(terms):
                            P.op("pe", lambda e, kk=kk, ri=ri, mi=mi, s_=s_, r_=r_, gi=gi, n=n, kl=kl: e.matmul(p_k[kk][:, ri, :], lhsT=gbt[gi][:, kl, mi, :], rhs=ains[gi][:, s_, r_, kl, :], start=(n == 0), stop=(n == 3)),
                                 [gbt_b[gi], ains_b[gi][s_]], [pk_b[kk]])
                    ks = (k1 // 4) % 2
                    P.op("act", lambda e, kk=kk, ks=ks, k1=k1: e.copy(out=kst[ks][:, k1 % 4, :, :], in_=p_k[kk][:]), [pk_b[kk]], [kst_b[ks]])
                    if k1 % 4 == 3:
                        P.dma("sp", K.Kd[o, :, k1 - 3:k1 + 1], kst[ks][:], reads=[kst_b[ks]])
    run_phase(K, body)


def phase_hyena_conv(K):
    I = K.I

    def body(P, sb, ps):
        st = s1_state(P, sb, ps, K, "ds1")
        F1T = sb("F1T", [128, 2, 128], BF16); f1t_b = P.buf()
        P.dma("sp", F1T[:], I["F1T"], writes=[f1t_b])
        fbias, fbias_b = None, None
        fbias = sb("fbias", [128, 2, D], F32); fbias_b = P.buf()
        for o in range(2):
            P.dma("sp", fbias[:, o, :], I["hy_filt_bias"][o:o + 1, :].partition_broadcast(128), writes=[fbias_b])
        xin = [sb(f"xin{i}", [128, 4, D], BF16) for i in range(2)]; xin_b = P.bufs(2)
        stg = [sb(f"s1stg{i}", [128, 2, 2048], BF16) for i in range(2)]; stg_b = P.bufs(2)
        ad_b = P.bufs(16)
        bd_b = P.bufs(16)
        z0_b = P.bufs(8)
        gb = [sb(f"gb{i}", [128, 4, 7, 128], BF16) for i in range(3)]; gb_b = P.bufs(3)
        ain = [sb(f"cain{i}", [128, 2, 4, 256], BF16) for i in range(3)]; ain_b = [P.bufs(2) for _ in range(3)]
        kk_ = [sb(f"ckk{i}", [128, 4, 2, 256], BF16) for i in range(3)]; kk_b = P.bufs(3)
        ksw = [sb(f"cksw{i}", [128, 4, 2, 256], BF16) for i in range(3)]; ksw_b = [P.bufs(2) for _ in range(3)]
        p_y = [ps(f"p_y{i}", [128, 2, 256], F32) for i in range(3)]; py_b = P.bufs(3)
        p_b = [ps(f"p_b{i}", [128, 2, 256], F32) for i in range(3)]; pb_b = P.bufs(3)
        m1 = [sb(f"m1{i}", [128, 2, 256], BF16) for i in range(2)]; m1_b = P.bufs(2)
        m2 = [sb(f"m2{i}", [128, 2, 256], BF16) for i in range(2)]; m2_b = P.bufs(2)
        pp = [sb(f"pp{i}", [128, 2, 256], BF16) for i in range(2)]; pp_b = P.bufs(2)
        bst = [sb(f"bst{i}", [128, 8, 2, 256], BF16) for i in range(2)]; bst_b = P.bufs(2)
        p_i = st["p"]; pi_b = st["p_b"]
        bin_ = stg; bin_b = stg_b
        gt = [sb(f"gt{i}", [128, 2, D], BF16) for i in range(2)]; gt_b = P.bufs(2)
        zi = [sb(f"zi{i}", [128, 2, D], F32) for i in range(2)]; zi_b = P.bufs(2)
        zib = [sb(f"zib{i}", [128, 2, D], BF16) for i in range(2)]; zib_b = P.bufs(2)
        t1 = [sb(f"t1{i}", [128, 512], F32) for i in range(2)]; t1_b = P.bufs(2)
        zo = [sb(f"zo{i}", [128, 2, D], BF16) for i in range(2)]; zo_b = P.bufs(2)
        UCv = K.UC.rearrange("(p a) c -> p a c", a=32)
        Zv = [K.Z[o].rearrange("(p a) c -> p a c", a=32) for o in range(2)]
        for o in range(2):
            for grp in range(8):
                xi = grp % 2
                if o == 0:
                    P.dma("pool", xin[xi][:], UCv[:, grp * 4:(grp + 1) * 4, 2 * D:3 * D], writes=[xin_b[xi]])
                else:
                    P.dma("sp", xin[xi][:], Zv[0][:, grp * 4:(grp + 1) * 4, :], reads=z0_b, writes=[xin_b[xi]])
                for j in range(8):
                    nch = grp * 8 + j
                    rhs = xin[xi][:, j // 2, (j % 2) * 512:(j % 2 + 1) * 512]
                    g = (nch // 4) % 2
                    for ri in range(2):
                        k = st["it"] % 2
                        st["it"] += 1
                        P.op("pe", lambda e, k=k, ri=ri, rhs=rhs: e.matmul(st["p"][k][:], lhsT=st["F1"][:, ri, :], rhs=rhs, start=True, stop=True), [xin_b[xi], st["F1_b"]], [st["p_b"][k]])
                        if ri == 0:
                            P.op("act", lambda e, k=k, g=g, ri=ri, nch=nch: e.copy(out=stg[g][:, ri, (nch % 4) * 512:(nch % 4 + 1) * 512], in_=st["p"][k][:]), [st["p_b"][k]], [stg_b[g]])
                        else:
                            P.op("dve", lambda e, k=k, g=g, ri=ri, nch=nch: e.tensor_copy(out=stg[g][:, ri, (nch % 4) * 512:(nch % 4 + 1) * 512], in_=st["p"][k][:]), [st["p_b"][k]], [stg_b[g]])
                    if nch % 4 == 3:
                        c0 = (nch // 4) * 2048
                        P.dma("sp", K.Ad[0, :, :, c0:c0 + 2048].rearrange("r k n -> k r n"), stg[g][:], reads=[stg_b[g]], writes=[ad_b[nch // 4]])
            KB = 4

            def load_grp(grp):
                bi = grp % 3
                k0 = grp * KB
                P.dma("sp", gb[bi][:], I["GB"][:, k0:k0 + KB], writes=[gb_b[bi]])
                for r_ in range(2):
                    P.dma("act", ain[bi][:, r_, :, :], K.Ad[0, r_, k0:k0 + KB, :].rearrange("k (q g) -> q k g", g=256), reads=ad_b, writes=[ain_b[bi][r_]])
                P.dma("sp", kk_[bi][:], K.Kd[o, :, k0:k0 + KB], writes=[kk_b[bi]])

            def emit_s2(k1):
                bi = (k1 // KB) % 3
                kl = k1 % KB
                yi = k1 % 3
                if kl == 0:
                    load_grp(k1 // KB)
                for ri, terms in ((0, [(0, 0), (2, 1)]), (1, [(1, 0), (0, 1)])):
                    for n, (mi, r_) in enumerate(terms):
                        P.op("pe", lambda e, ri=ri, mi=mi, r_=r_, n=n: e.matmul(p_y[yi][:, ri, :], lhsT=gb[bi][:, kl, mi, :], rhs=ain[bi][:, r_, kl, :], start=(n == 0), stop=(n == 1)),
                             [gb_b[bi], ain_b[bi][r_]], [py_b[yi]])

            def emit_rest(k1):
                bi = (k1 // KB) % 3
                kl = k1 % KB
                yi = k1 % 3
                gi = k1 % 2
                P.op("dve", lambda e: e.tensor_tensor(out=m1[gi][:], in0=p_y[yi][:], in1=kk_[bi][:, kl], op=ALU.mult), [py_b[yi], kk_b[bi]], [m1_b[gi]])
                P.op("dve", lambda e: e.tensor_tensor(out=m2[gi][:, 0, :], in0=p_y[yi][:, 0, :], in1=kk_[bi][:, kl, 1, :], op=ALU.mult), [py_b[yi], kk_b[bi]], [m2_b[gi]])
                P.op("dve", lambda e: e.tensor_tensor(out=m2[gi][:, 1, :], in0=p_y[yi][:, 1, :], in1=kk_[bi][:, kl, 0, :], op=ALU.mult), [py_b[yi], kk_b[bi]], [m2_b[gi]])
                P.op("pool", lambda e: e.tensor_tensor(out=pp[gi][:, 0, :], in0=m1[gi][:, 0, :], in1=m1[gi][:, 1, :], op=ALU.subtract), [m1_b[gi]], [pp_b[gi]])
                P.op("pool", lambda e: e.tensor_tensor(out=pp[gi][:, 1, :], in0=m2[gi][:, 0, :], in1=m2[gi][:, 1, :], op=ALU.add), [m2_b[gi]], [pp_b[gi]])
                for ri, terms in ((0, [(4, 0), (5, 1)]), (1, [(4, 1), (6, 0)])):
                    for n, (mi, r_) in enumerate(terms):
                        P.op("pe", lambda e, ri=ri, mi=mi, r_=r_, n=n: e.matmul(p_b[yi][:, ri, :], lhsT=gb[bi][:, kl, mi, :], rhs=pp[gi][:, r_, :], start=(n == 0), stop=(n == 1)),
                             [gb_b[bi], pp_b[gi]], [pb_b[yi]])
                ks = (k1 // 8) % 2
                P.op("act", lambda e: e.copy(out=bst[ks][:, k1 % 8, :, :], in_=p_b[yi][:]), [pb_b[yi]], [bst_b[ks]])
                if k1 % 8 == 7:
                    P.dma("sp", K.Bd[0, k1 - 7:k1 + 1, :].rearrange("k (q g) -> q k g", g=256), bst[ks][:, :, 0, :], reads=[bst_b[ks]], writes=[bd_b[k1 // 8]])
                    P.dma("sp", K.Bd[1, k1 - 7:k1 + 1, :].rearrange("k (q g) -> q k g", g=256), bst[ks][:, :, 1, :], reads=[bst_b[ks]], writes=[bd_b[k1 // 8]])

            LOOK = 2
            for k1 in range(LOOK):
                emit_s2(k1)
            for k1 in range(128):
                if k1 + LOOK < 128:
                    emit_s2(k1 + LOOK)
                emit_rest(k1)
            for grp in range(16):
                bi = grp % 2
                P.dma("sp", bin_[bi][:], K.Bd[:, :, grp * 2048:(grp + 1) * 2048].rearrange("r k n -> k r n"), reads=bd_b, writes=[bin_b[bi]])
                P.dma("pool", gt[bi][:], UCv[:, grp * 2:(grp + 1) * 2, o * D:(o + 1) * D], writes=[gt_b[bi]])
                if o == 0:
                    P.dma("sp", zib[bi][:], UCv[:, grp * 2:(grp + 1) * 2, 2 * D:3 * D], writes=[zib_b[bi]])
                else:
                    P.dma("sp", zib[bi][:], Zv[0][:, grp * 2:(grp + 1) * 2, :], reads=z0_b, writes=[zib_b[bi]])
                P.op("dve", lambda e, bi=bi, o=o: e.tensor_tensor(out=zi[bi][:], in0=zib[bi][:], in1=fbias[:, o:o + 1, :].to_broadcast([128, 2, D]), op=ALU.mult), [zib_b[bi], fbias_b], [zi_b[bi]])
                for j in range(4):
                    k = (grp * 4 + j) % 2
                    for ri in range(2):
                        P.op("pe", lambda e, k=k, ri=ri, bi=bi, j=j: e.matmul(p_i[k][:], lhsT=F1T[:, ri, :], rhs=bin_[bi][:, ri, j * 512:(j + 1) * 512], start=(ri == 0), stop=(ri == 1)),
                             [f1t_b, bin_b[bi]], [pi_b[k]])
                    aa, hh = j // 2, j % 2
                    P.op("dve", lambda e, k=k, bi=bi, aa=aa, hh=hh: e.tensor_tensor(out=t1[k][:], in0=p_i[k][:], in1=zi[bi][:, aa, hh * 512:(hh + 1) * 512], op=ALU.add), [pi_b[k], zi_b[bi]], [t1_b[k]])
                    P.op("dve", lambda e, k=k, bi=bi, aa=aa, hh=hh: e.tensor_tensor(out=zo[bi][:, aa, hh * 512:(hh + 1) * 512], in0=t1[k][:], in1=gt[bi][:, aa, hh * 512:(hh + 1) * 512], op=ALU.mult), [t1_b[k], gt_b[bi]], [zo_b[bi]])
                P.dma("sp", Zv[o][:, grp * 2:(grp + 1) * 2, :], zo[bi][:], reads=[zo_b[bi]], writes=([z0_b[grp // 2]] if o == 0 else []))
    run_phase(K, body)


_PROG = {}


def _layout_inputs(inp, b):
    f = lambda a: np.ascontiguousarray(np.asarray(a, dtype=np.float32))
    m = {}
    m["x"] = f(inp["x"][b])
    m["c_t"] = f(np.asarray(inp["c"][b]).reshape(8, 128).T)
    m["pos_t"] = np.ascontiguousarray(np.asarray(inp["positions"][b]).astype(np.int32).reshape(NT, 128).T)
    for k in ("ada_w", "ada_b", "norm_mix_g", "norm_ffn_g"):
        m[k] = f(inp[k])
    m["mla_w_down"] = f(inp["mla_w_down"][0])
    m["mla_q_a_g"] = f(inp["mla_q_a_g"])
    m["mla_kv_a_g"] = f(inp["mla_kv_a_g"])
    m["mla_w_uq"] = f(inp["mla_w_uq"][0])
    m["mla_w_ukv"] = f(inp["mla_w_ukv"][0])
    m["mla_q_norm_g"] = f(inp["mla_q_norm_g"])
    m["mla_k_norm_g"] = f(inp["mla_k_norm_g"])
    m["mla_w_o"] = f(inp["mla_w_o"][0])
    m["hy_w_in"] = f(inp["hy_w_in"][0])
    m["hy_b_in_t"] = f(np.asarray(inp["hy_b_in"][0]).reshape(24, 128).T)
    m["hy_conv_w_t"] = f(np.asarray(inp["hy_conv_w"][0]).reshape(3, 24, 128).transpose(2, 0, 1))
    m["hy_conv_b_t"] = f(np.asarray(inp["hy_conv_b"][0]).reshape(24, 128).T)
    m["hy_f_w1"] = f(inp["hy_f_w1"][0])
    m["hy_f_w2"] = f(inp["hy_f_w2"][0])
    m["hy_f_w3"] = f(inp["hy_f_w3"][0])
    m["hy_f_bf_t"] = f(np.stack([np.asarray(inp[k][0]) for k in ("hy_f_b1", "hy_f_freq1", "hy_f_b2", "hy_f_freq2", "hy_f_b3", "hy_f_freq3")], axis=1))
    m["hy_f_w4"] = f(inp["hy_f_w4"][0])
    m["hy_filt_bias"] = f(inp["hy_filt_bias"][0])
    m["hy_w_out"] = f(inp["hy_w_out"][0])
    m["moe_wr"] = f(np.concatenate([np.asarray(inp["moe_wg"]), np.asarray(inp["moe_we"])], axis=-1))
    m["moe_br"] = f(np.concatenate([np.asarray(inp["moe_bg"]), np.asarray(inp["moe_be"])], axis=-1))
    m["moe_w_gate"] = f(inp["moe_w_gate"])
    m["moe_w_up"] = f(inp["moe_w_up"])
    m["moe_w_down"] = f(inp["moe_w_down"])
    return m


def kernel(**inputs):
    if "nc" not in _PROG:
        _PROG["nc"] = build_program()
    nc = _PROG["nc"]
    const = host_constants()
    shared = None
    in_maps = []
    for b in range(8):
        m = _layout_inputs(inputs, b)
        if shared is None:
            shared = {k: v for k, v in m.items() if k not in ("x", "c_t", "pos_t")}
        else:
            for k in shared:
                m[k] = shared[k]
        m.update(const)
        in_maps.append(m)
    res = run_bass_kernel_spmd(nc, in_maps, core_ids=list(range(8)))
    return np.stack([np.asarray(r["out"], dtype=np.float32) for r in res.results], axis=0)
```

```python
import math
from contextlib import ExitStack
import numpy as np
import ml_dtypes
import concourse.bass as bass
import concourse.mybir as mybir
from concourse.bass_utils import run_bass_kernel_spmd

F32 = mybir.dt.float32
BF16 = mybir.dt.bfloat16
I32 = mybir.dt.int32
AF = mybir.ActivationFunctionType
ALU = mybir.AluOpType
AX = mybir.AxisListType

D = 1024
S = 4096
NT = S // 128
EPS = 1e-6
NE = 16
DE = 256
NFFT = 8192

ENGS = ("pe", "act", "dve", "pool", "sp")
DMA_RING = {"sp": 12, "act": 4, "pool": 12}
SEM_CAP = 30000


_UID = [0]


def _uid():
    _UID[0] += 1
    return _UID[0]


class Buf:
    __slots__ = ("last_w", "readers", "excl")

    def __init__(self):
        self.last_w = None
        self.readers = []
        self.excl = True


class Op:
    __slots__ = ("eng", "fn", "is_dma", "lidx", "deps", "signal", "sig_idx", "waits", "dma_slot",
                 "dma_val", "ring_wait")

    def __init__(self, eng, fn, is_dma):
        self.eng = eng
        self.fn = fn
        self.is_dma = is_dma
        self.deps = []
        self.signal = False
        self.sig_idx = None
        self.waits = []
        self.dma_slot = None
        self.dma_val = None
        self.ring_wait = None


class Prog:
    def __init__(self):
        self.ops = []
        self.per_eng = {e: [] for e in ENGS}
        self.dma_count = {e: 0 for e in DMA_RING}

    def buf(self):
        return Buf()

    def bufs(self, n):
        return [Buf() for _ in range(n)]

    def op(self, eng, fn, reads=(), writes=(), dma=False):
        o = Op(eng, fn, dma)
        o.lidx = len(self.per_eng[eng])
        deps = set()
        for b in reads:
            if b.last_w is not None:
                deps.add(b.last_w)
            if b.excl:
                for r in b.readers:
                    if r.eng != eng:
                        deps.add(r)
        for b in writes:
            if b.last_w is not None:
                deps.add(b.last_w)
            for r in b.readers:
                deps.add(r)
        deps.discard(o)
        o.deps = list(deps)
        for b in reads:
            b.readers.append(o)
        for b in writes:
            b.last_w = o
            b.readers = []
        if dma:
            n = self.dma_count[eng]
            self.dma_count[eng] = n + 1
            R = DMA_RING[eng]
            o.dma_slot = (eng, n % R)
            o.dma_val = 16 * (n // R + 1)
            if n >= R:
                o.ring_wait = 16 * (n // R)
        self.ops.append(o)
        self.per_eng[eng].append(o)
        return o

    def dma(self, q, out, in_, reads=(), writes=(), **kw):
        if q != "act":
            is_store = "DRAM" in str(out.space)
            cast = out.dtype != in_.dtype
            q = "pool" if (is_store or cast) else "sp"
        return self.op(q, lambda e: e.dma_start(out=out, in_=in_, **kw), reads, writes, dma=True)

    def resolve(self):
        known = {c: {p: -1 for p in ENGS} for c in ENGS}
        known_dma = {c: {} for c in ENGS}
        for o in self.ops:
            c = o.eng
            latest = {}
            latest_dma = {}
            for d in o.deps:
                if d.is_dma:
                    if d.dma_slot not in latest_dma or d.dma_val > latest_dma[d.dma_slot].dma_val:
                        latest_dma[d.dma_slot] = d
                else:
                    if d.eng not in latest or d.lidx > latest[d.eng].lidx:
                        latest[d.eng] = d
            for slot in sorted(latest_dma):
                d = latest_dma[slot]
                k = known_dma[c].get(d.dma_slot, 0)
                if d.dma_val > k:
                    known_dma[c][d.dma_slot] = d.dma_val
                    o.waits.append(("dma", d.dma_slot, d.dma_val))
            for p in ENGS:
                if p not in latest:
                    continue
                d = latest[p]
                if p == "pe" and c == "pe":
                    continue
                if d.lidx > known[c][p]:
                    known[c][p] = d.lidx
                    d.signal = True
                    o.waits.append(("cmp", d))
            if o.is_dma and o.ring_wait is not None:
                k = known_dma[c].get(o.dma_slot, 0)
                if o.ring_wait > k:
                    known_dma[c][o.dma_slot] = o.ring_wait
                    o.waits.append(("dma", o.dma_slot, o.ring_wait))
        sig_count = {}
        for e in ENGS:
            n = 0
            for o in self.per_eng[e]:
                if o.signal and not o.is_dma:
                    o.sig_idx = n
                    n += 1
            sig_count[e] = n
        return sig_count

    def emit(self, nc):
        sig_count = self.resolve()
        handles = []

        def newsem(pfx):
            h = nc.alloc_semaphore(name=f"{pfx}{_uid()}")
            handles.append(h)
            return h
        with ExitStack() as es:
            csem = {}
            for e in ENGS:
                ns = max(1, (sig_count[e] + SEM_CAP - 1) // SEM_CAP)
                csem[e] = [newsem("cs") for i in range(ns)]
            dsem = {}
            for q, R in DMA_RING.items():
                for i in range(min(R, self.dma_count[q])):
                    dsem[(q, i)] = newsem("ds")
            block = es.enter_context(nc.Block())

            def run(engname):
                def body(eng):
                    for o in self.per_eng[engname]:
                        for w in o.waits:
                            if w[0] == "dma":
                                eng.wait_ge(dsem[w[1]], w[2])
                            else:
                                d = w[1]
                                eng.wait_ge(csem[d.eng][d.sig_idx // SEM_CAP], d.sig_idx % SEM_CAP + 1)
                        ins = o.fn(eng)
                        if o.is_dma:
                            ins.then_inc(dsem[o.dma_slot], 16)
                        elif o.signal:
                            ins.then_inc(csem[engname][o.sig_idx // SEM_CAP], 1)
                    if engname in DMA_RING:
                        n = self.dma_count[engname]
                        R = DMA_RING[engname]
                        for s in range(min(R, n)):
                            cnt = (n - 1 - s) // R + 1
                            eng.wait_ge(dsem[(engname, s)], 16 * cnt)
                return body

            block.tensor(run("pe"))
            block.scalar(run("act"))
            block.vector(run("dve"))
            block.gpsimd(run("pool"))
            block.sync(run("sp"))
        nc.clear_and_free_semaphores(handles)


_CONST = {}


def _bf(a):
    return np.ascontiguousarray(a.astype(np.float32)).astype(ml_dtypes.bfloat16)


def host_constants():
    if _CONST:
        return _CONST
    c = {}
    c["ident32"] = np.eye(128, dtype=np.float32)
    c["identb"] = _bf(np.eye(128))
    c["inv_freq"] = (1.0 / (10000.0 ** (np.arange(0, 64, 2, dtype=np.float32) / 64))).astype(np.float32)[None, :]
    sel = np.zeros((16, 16, 128), np.float32)
    for e in range(16):
        sel[e, e, :] = 1.0
    c["sel"] = _bf(sel.reshape(16, 16 * 128))
    L = S
    t = np.linspace(0.0, 1.0, L, dtype=np.float32)[:, None]
    w = (2.0 * math.pi * np.arange(L, dtype=np.float32)[:, None] / L).astype(np.float32)
    fr = np.linspace(1e-4, 15, 16, dtype=np.float32)[None, :]
    z = np.concatenate([t, np.cos(fr * w), -np.sin(fr * w)], axis=-1).astype(np.float32)
    c["zT"] = np.ascontiguousarray(z.T)
    tt = (32 * np.arange(128)[:, None] + np.arange(32)[None, :])
    c["ntl"] = (-t[:, 0][tt]).astype(np.float32)
    deltas = np.abs(np.linspace(math.log(0.3) / 1e-2, math.log(1.5) / 1e-2, D, dtype=np.float32))
    c["deltas"] = deltas.astype(np.float32)[None, :]
    p = np.arange(128)[:, None].astype(np.float64)
    k1 = np.arange(128)[None, :].astype(np.float64)
    F1 = np.exp(-2j * np.pi * p * (k1 + 0.5) / 256.0)
    c["F1"] = _bf(np.stack([F1.real, F1.imag], axis=1))
    sc = 2.0 / NFFT
    c["F1T"] = _bf(np.stack([F1.real.T * sc, F1.imag.T * sc], axis=1))
    a = np.arange(32)[:, None].astype(np.float64)
    k2 = np.arange(32)[None, :].astype(np.float64)
    GB = np.zeros((128, 128, 7, 128), np.float32)
    for kk in range(128):
        G = np.exp(-2j * np.pi * a * (kk + 256.0 * k2 + 0.5) / NFFT)
        mats = [G.real, G.imag, -G.imag, -G.real, G.real.T, G.imag.T, -G.imag.T]
        for mi, M in enumerate(mats):
            for c4 in range(4):
                GB[c4::4, kk, mi, c4::4] = M
    c["GB"] = _bf(GB)
    m4 = np.zeros((128, 4, 128), np.float32)
    for c4 in range(4):
        m4[:, c4, c4::4] = 1.0
    c["mask4"] = m4
    _CONST.update(c)
    return _CONST


class Ctx:
    pass


def build_program(debug=None, stop=None, dump=()):
    nc = bass.Bass("TRN2", target_bir_lowering=False)
    K = Ctx()
    K.nc = nc

    def din(name, shape, dt=F32):
        return nc.dram_tensor(name, list(shape), dt, kind="ExternalInput").ap()

    def dscr(name, shape, dt=F32):
        return nc.dram_tensor(name, list(shape), dt, kind="Internal").ap()

    I = {}
    I["x"] = din("x", [S, D])
    I["c_t"] = din("c_t", [128, 8])
    I["pos_t"] = din("pos_t", [128, NT], I32)
    I["ada_w"] = din("ada_w", [2, D, 6 * D])
    I["ada_b"] = din("ada_b", [2, 6 * D])
    I["norm_mix_g"] = din("norm_mix_g", [2, D])
    I["norm_ffn_g"] = din("norm_ffn_g", [2, D])
    I["mla_w_down"] = din("mla_w_down", [D, 448])
    I["mla_q_a_g"] = din("mla_q_a_g", [1, 256])
    I["mla_kv_a_g"] = din("mla_kv_a_g", [1, 128])
    I["mla_w_uq"] = din("mla_w_uq", [256, 1536])
    I["mla_w_ukv"] = din("mla_w_ukv", [128, 2048])
    I["mla_q_norm_g"] = din("mla_q_norm_g", [1, 192])
    I["mla_k_norm_g"] = din("mla_k_norm_g", [1, 192])
    I["mla_w_o"] = din("mla_w_o", [D, D])
    I["hy_w_in"] = din("hy_w_in", [D, 3 * D])
    I["hy_b_in_t"] = din("hy_b_in_t", [128, 24])
    I["hy_conv_w_t"] = din("hy_conv_w_t", [128, 3, 24])
    I["hy_conv_b_t"] = din("hy_conv_b_t", [128, 24])
    I["hy_f_w1"] = din("hy_f_w1", [33, 64])
    I["hy_f_w2"] = din("hy_f_w2", [64, 64])
    I["hy_f_w3"] = din("hy_f_w3", [64, 64])
    I["hy_f_bf_t"] = din("hy_f_bf_t", [64, 6])
    I["hy_f_w4"] = din("hy_f_w4", [64, 4 * D])
    I["hy_filt_bias"] = din("hy_filt_bias", [2, D])
    I["hy_w_out"] = din("hy_w_out", [D, D])
    I["moe_wr"] = din("moe_wr", [2, D, 20])
    I["moe_br"] = din("moe_br", [2, 20])
    I["moe_w_gate"] = din("moe_w_gate", [2, NE, D, DE])
    I["moe_w_up"] = din("moe_w_up", [2, NE, D, DE])
    I["moe_w_down"] = din("moe_w_down", [2, NE, DE, D])
    I["ident32"] = din("ident32", [128, 128])
    I["identb"] = din("identb", [128, 128], BF16)
    I["inv_freq"] = din("inv_freq", [1, 32])
    I["sel"] = din("sel", [16, 16 * 128], BF16)
    I["zT"] = din("zT", [33, S])
    I["ntl"] = din("ntl", [128, 32])
    I["deltas"] = din("deltas", [1, D])
    I["F1"] = din("F1", [128, 2, 128], BF16)
    I["F1T"] = din("F1T", [128, 2, 128], BF16)
    I["GB"] = din("GB", [128, 128, 7, 128], BF16)
    I["mask4"] = din("mask4", [128, 4, 128])
    K.I = I
    out = nc.dram_tensor("out", [S, D], F32, kind="ExternalOutput").ap()
    K.out = out

    K.xs = dscr("xs", [S, D])
    K.QN = dscr("QN", [8, 128, S], BF16)
    K.KN = dscr("KN", [8, 128, S], BF16)
    K.QP = dscr("QP", [4, 128, S], BF16)
    K.KP = dscr("KP", [64, S], BF16)
    K.V = dscr("V", [8, S, 130], BF16)
    K.OT = dscr("OT", [8, 128, S], BF16)
    K.AT = dscr("AT", [NE, 2, 128, S], BF16)
    K.UC = dscr("UC", [S, 3 * D], BF16)
    K.Ad = dscr("Ad", [2, 2, 128, 32 * D], BF16)
    K.Bd = dscr("Bd", [2, 128, 32 * D], BF16)
    K.Kd = dscr("Kd", [2, 128, 128, 2, 256], BF16)
    K.Z = dscr("Z", [2, S, D], BF16)
    K.HT = dscr("HT", [128, 8, S], BF16)
    K.CT = dscr("CT", [16, S], BF16)
    K.dbg = {}
    if debug:
        for name, (shape, dt) in debug.items():
            K.dbg[name] = nc.dram_tensor("dbg_" + name, list(shape), dt, kind="ExternalOutput").ap()

    with ExitStack() as pes:
        def psb(name, shape, dt):
            return pes.enter_context(nc.sbuf_tensor(name, list(shape), dt))
        K.ident32 = psb("ident32_sb", [128, 128], F32)
        K.identb = psb("identb_sb", [128, 128], BF16)
        K.modb = psb("modb", [128, 6 * D], F32)
        K.cbc = psb("cbc", [128, 8, 128], F32)

        seq = [("setup", lambda: phase_setup(K))]
        for layer in range(2):
            seq.append((f"adaln{layer}", lambda layer=layer: phase_adaln(K, layer)))
            if layer == 0:
                seq.append(("mla_proj", lambda: phase_mla_proj(K)))
                seq.append(("attn", lambda: phase_attention(K)))
                seq.append(("outproj0", lambda: phase_outproj(K, None, I["mla_w_o"], 0, src_fm=K.OT)))
            else:
                seq.append(("hy_norm", lambda: phase_norm_T(K, 0)))
                seq.append(("hy_in", lambda: phase_hyena_in(K)))
                seq.append(("hy_filter", lambda: phase_hyena_filter(K)))
                seq.append(("hy_conv", lambda: phase_hyena_conv(K)))
                seq.append(("outproj1", lambda: phase_outproj(K, K.Z[1], I["hy_w_out"], 1)))
            seq.append((f"moe_norm{layer}", lambda layer=layer: phase_norm_T(K, 3, router_layer=layer)))
            seq.append((f"moe_a{layer}", lambda layer=layer: phase_moe_a(K, layer)))
            seq.append((f"moe_b{layer}", lambda layer=layer: phase_moe_b(K, layer)))
        K.skip = set()
        for name, fn in seq:
            if name not in getattr(build_program, "SKIP", set()):
                fn()
            if stop == name:
                break
        if dump:
            def body(P, sb, ps):
                for nm in dump:
                    src = getattr(K, nm) if hasattr(K, nm) else None
                    if nm == "modb":
                        P.dma("sp", K.dbg[nm], K.modb[:])
                    else:
                        P.dma("sp", K.dbg[nm], src)
            run_phase(K, body)
    return nc


def run_phase(K, body):
    nc = K.nc
    with ExitStack() as es:
        P = Prog()

        pre = f"ph{_uid()}_"

        def sb(name, shape, dt):
            return es.enter_context(nc.sbuf_tensor(pre + name, list(shape), dt))

        def ps(name, shape, dt):
            return es.enter_context(nc.psum_tensor(pre + name, list(shape), dt))
        body(P, sb, ps)
        P.emit(nc)
    nc.all_engine_barrier()


def phase_setup(K):
    I = K.I

    def body(P, sb, ps):
        ct = sb("ct", [128, 8], F32)
        ca = sb("ca", [128, 8], F32)
        b1, b2, b3, b4 = P.bufs(4)
        P.dma("sp", K.ident32[:], I["ident32"], writes=[b1])
        P.dma("sp", K.identb[:], I["identb"], writes=[b2])
        P.dma("sp", ct[:], I["c_t"], writes=[b3])
        P.op("act", lambda e: e.activation(out=ca[:], in_=ct[:], func=AF.Silu), [b3], [b4])
        for kc in range(8):
            P.op("dve", lambda e, kc=kc: e.tensor_copy(out=K.cbc[:, kc, :], in_=ca[:, kc:kc + 1].to_broadcast([128, 128])), [b4], [b1])
        xt = [sb(f"xcp{i}", [128, 4, D], F32) for i in range(2)]
        xb = P.bufs(2)
        for i in range(8):
            P.dma("sp", xt[i % 2][:], I["x"][i * 512:(i + 1) * 512, :].rearrange("(j p) d -> p j d", p=128), writes=[xb[i % 2]])
            P.dma("pool", K.xs[i * 512:(i + 1) * 512, :].rearrange("(j p) d -> p j d", p=128), xt[i % 2][:], reads=[xb[i % 2]])
    run_phase(K, body)


def phase_adaln(K, layer):
    I = K.I

    def body(P, sb, ps):
        wt = [sb(f"adaw{i}", [128, 8, 512], F32) for i in range(2)]
        wb = P.bufs(2)
        bt = sb("adab", [128, 6 * D], F32)
        bb = P.buf()
        gm = sb("gm", [128, 2, D], F32)
        gb_ = P.buf()
        pp = [ps(f"adaps{i}", [128, 512], F32) for i in range(2)]
        pb = P.bufs(2)
        mb = P.bufs(12)
        P.dma("sp", bt[:], I["ada_b"][layer:layer + 1, :].partition_broadcast(128), writes=[bb])
        P.dma("sp", gm[:, 0, :], I["norm_mix_g"][layer:layer + 1, :].partition_broadcast(128), writes=[gb_])
        P.dma("sp", gm[:, 1, :], I["norm_ffn_g"][layer:layer + 1, :].partition_broadcast(128), writes=[gb_])
        wv = I["ada_w"][layer].rearrange("(kc p) n -> p kc n", p=128)
        for j in range(12):
            q = "sp" if j % 2 == 0 else "pool"
            P.dma(q, wt[j % 2][:], wv[:, :, j * 512:(j + 1) * 512], writes=[wb[j % 2]])
            for kc in range(8):
                P.op("pe", lambda e, j=j, kc=kc: e.matmul(pp[j % 2][:], lhsT=K.cbc[:, kc, :], rhs=wt[j % 2][:, kc, :], start=(kc == 0), stop=(kc == 7)),
                     [wb[j % 2]], [pb[j % 2]])
            P.op("dve", lambda e, j=j: e.tensor_tensor(out=K.modb[:, j * 512:(j + 1) * 512], in0=pp[j % 2][:], in1=bt[:, j * 512:(j + 1) * 512], op=ALU.add),
                 [pb[j % 2], bb], [mb[j]])
        for si, gi in ((1, 0), (4, 1)):
            sec = K.modb[:, si * D:(si + 1) * D]
            P.op("dve", lambda e, sec=sec, gi=gi: e.scalar_tensor_tensor(out=sec, in0=sec, scalar=1.0, in1=gm[:, gi, :], op0=ALU.add, op1=ALU.mult),
                 [mb[2 * si], mb[2 * si + 1], gb_], [mb[2 * si], mb[2 * si + 1]])
    run_phase(K, body)


class NormCtx:
    def __init__(self, P, sb, ps, K, want_f32=False, tag="n"):
        self.P, self.K = P, K
        self.want_f32 = want_f32
        self.xt = [sb(f"{tag}_xt{i}", [128, D], F32) for i in range(2)]
        self.xb = P.bufs(2)
        self.junk = sb(f"{tag}_junk", [128, D], F32)
        self.jb = P.buf()
        self.ss = [sb(f"{tag}_ss{i}", [128, 2], F32) for i in range(2)]
        self.sb_ = P.bufs(2)
        self.hn = [sb(f"{tag}_hn{i}", [128, D], F32 if want_f32 else BF16) for i in range(2)]
        self.hb = P.bufs(2)
        self.h32 = [sb(f"{tag}_h32{i}", [128, D], F32) for i in range(2)]
        self.h32b = P.bufs(2)
        if want_f32:
            self.pt = [ps(f"{tag}_pt{i}", [128, 512], F32) for i in range(2)]
            self.ptb = P.bufs(2)
        else:
            self.pt = [ps(f"{tag}_pt", [128, D], BF16)]
            self.ptb = P.bufs(1)
        self.n = 0

    def run(self, x_ap, sec, out_bf, out_bf_buf, out_f32=None, out_f32_buf=None, q="sp"):
        st = self.run_a1(x_ap, sec, q)
        self.run_a2(st, out_bf, out_bf_buf, out_f32, out_f32_buf)

    def run_a1(self, x_ap, sec, q="sp"):
        P, K = self.P, self.K
        i = self.n % 2
        self.n += 1
        xt, ss, hn, h32 = self.xt[i], self.ss[i], self.hn[i], self.h32[i]
        SH = K.modb[:, sec * D:(sec + 1) * D]
        G = K.modb[:, (sec + 1) * D:(sec + 2) * D]
        P.dma(q, xt[:], x_ap, writes=[self.xb[i]])
        P.op("act", lambda e: e.activation(out=self.junk[:], in_=xt[:], func=AF.Square, accum_out=ss[:, 0:1]), [self.xb[i]], [self.jb, self.sb_[i]])
        P.op("act", lambda e: e.activation(out=ss[:, 1:2], in_=ss[:, 0:1], func=AF.Sqrt, scale=1.0 / D, bias=EPS), [self.sb_[i]], [self.sb_[i]])
        P.op("dve", lambda e: e.reciprocal(out=ss[:, 1:2], in_=ss[:, 1:2]), [self.sb_[i]], [self.sb_[i]])
        P.op("dve", lambda e: e.scalar_tensor_tensor(out=h32[:], in0=xt[:], scalar=ss[:, 1:2], in1=G, op0=ALU.mult, op1=ALU.mult),
             [self.xb[i], self.sb_[i]], [self.h32b[i]])
        P.op("pool", lambda e: e.tensor_tensor(out=hn[:], in0=h32[:], in1=SH, op=ALU.add), [self.h32b[i]], [self.hb[i]])
        return i

    def run_a2(self, i, out_bf, out_bf_buf, out_f32=None, out_f32_buf=None):
        P, K = self.P, self.K
        hn = self.hn[i]
        if self.want_f32:
            for half in range(2):
                for k in range(4):
                    kc = half * 4 + k
                    P.op("pe", lambda e, kc=kc, k=k, half=half: e.transpose(self.pt[half][:, k * 128:(k + 1) * 128], hn[:, kc * 128:(kc + 1) * 128], K.ident32[:]),
                         [self.hb[i]], [self.ptb[half]])
                P.op("act", lambda e, half=half: e.copy(out=out_bf[:, half * 4:(half + 1) * 4, :], in_=self.pt[half][:].rearrange("p (k t) -> p k t", k=4)),
                     [self.ptb[half]], [out_bf_buf])
                P.op("dve", lambda e, half=half: e.tensor_copy(out=out_f32[:, half * 4:(half + 1) * 4, :], in_=self.pt[half][:].rearrange("p (k t) -> p k t", k=4)),
                     [self.ptb[half]], [out_f32_buf])
        else:
            for kc in range(8):
                P.op("pe", lambda e, kc=kc: e.transpose(self.pt[0][:, kc * 128:(kc + 1) * 128], hn[:, kc * 128:(kc + 1) * 128], K.identb[:]),
                     [self.hb[i]], [self.ptb[0]])
            P.op("act", lambda e: e.copy(out=out_bf, in_=self.pt[0][:].rearrange("p (k t) -> p k t", k=8)), [self.ptb[0]], [out_bf_buf])


def load_bcast(P, sb, name, src_row_ap, n, q="sp", dt=F32):
    t = sb(name, [128, n], dt)
    b = P.buf()
    P.dma(q, t[:], src_row_ap.partition_broadcast(128), writes=[b])
    return t, b


def emit_sincos_reduce(P, t_ap, tmp_i, tmp_f, bufs_rw, shift):
    b = bufs_rw
    P.op("dve", lambda e: e.tensor_scalar(out=t_ap, in0=t_ap, scalar1=float(1.0 / (2 * math.pi)), scalar2=float(shift / (2 * math.pi)), op0=ALU.mult, op1=ALU.add), b, b)
    P.op("dve", lambda e: e.tensor_copy(out=tmp_i, in_=t_ap), b, b)
    P.op("dve", lambda e: e.tensor_copy(out=tmp_f, in_=tmp_i), b, b)
    P.op("dve", lambda e: e.tensor_tensor(out=t_ap, in0=t_ap, in1=tmp_f, op=ALU.subtract), b, b)
    P.op("dve", lambda e: e.tensor_scalar(out=tmp_f, in0=t_ap, scalar1=0.0, scalar2=None, op0=ALU.is_lt), b, b)
    P.op("dve", lambda e: e.tensor_tensor(out=t_ap, in0=t_ap, in1=tmp_f, op=ALU.add), b, b)
    P.op("dve", lambda e: e.tensor_scalar(out=t_ap, in0=t_ap, scalar1=float(2 * math.pi), scalar2=float(-math.pi), op0=ALU.mult, op1=ALU.add), b, b)
    P.op("dve", lambda e: e.tensor_scalar(out=t_ap, in0=t_ap, scalar1=3.1415925, scalar2=-3.1415925, op0=ALU.min, op1=ALU.max), b, b)


import os
CUT = float(os.environ.get("CUT", "9999"))
NTT = int(os.environ.get("NTT", "32"))


class _Cut(Exception):
    pass


def cut(n):
    if n >= CUT:
        raise _Cut()


def phase_mla_proj(K):
    I = K.I

    def body(P, sb, ps):
        try:
            body2(P, sb, ps)
        except _Cut:
            pass

    def body2(P, sb, ps):
        nctx = NormCtx(P, sb, ps, K, want_f32=False, tag="mn")
        wdn = sb("wdn", [128, 8, 448], BF16); wdn_b = P.buf()
        P.dma("pool", wdn[:], I["mla_w_down"].rearrange("(kc p) n -> p kc n", p=128), writes=[wdn_b])
        wuq = sb("wuq", [128, 2, 1536], BF16); wuq_b = P.buf()
        P.dma("pool", wuq[:], I["mla_w_uq"].rearrange("(kc p) n -> p kc n", p=128), writes=[wuq_b])
        wukv = sb("wukv", [128, 2048], BF16); wukv_b = P.buf()
        P.dma("pool", wukv[:], I["mla_w_ukv"], writes=[wukv_b])
        gqa, gqa_b = load_bcast(P, sb, "gqa", I["mla_q_a_g"], 256)
        gkva, gkva_b = load_bcast(P, sb, "gkva", I["mla_kv_a_g"], 128)
        gq, gq_b = load_bcast(P, sb, "gq", I["mla_q_norm_g"], 192)
        gk, gk_b = load_bcast(P, sb, "gk", I["mla_k_norm_g"], 192)
        invf, invf_b = load_bcast(P, sb, "invf", I["inv_freq"], 32)
        gqk = sb("gqk", [128, 128], F32); gqk_b = P.buf()
        P.op("dve", lambda e: e.tensor_tensor(out=gqk[:], in0=gq[:, 0:128], in1=gk[:, 0:128], op=ALU.mult), [gq_b, gk_b], [gqk_b])
        dv3 = sb("dv3", [128, 3], F32); dv_b = P.buf()
        for j, v in enumerate((1.0 / 256, 1.0 / 128, 1.0 / 64)):
            P.op("dve", lambda e, j=j, v=v: e.memset(dv3[:, j:j + 1], v), [], [dv_b])
        dv16 = sb("dv16", [128, 16], F32)
        P.op("dve", lambda e: e.memset(dv16[:, 0:8], 1.0 / 128), [], [dv_b])
        P.op("dve", lambda e: e.memset(dv16[:, 8:16], 1.0 / 64), [], [dv_b])
        posi = sb("posi", [128, NT], I32); posf = sb("posf", [128, NT], F32); pos_b = P.buf()
        P.dma("sp", posi[:], I["pos_t"], writes=[pos_b])
        P.op("dve", lambda e: e.tensor_copy(out=posf[:], in_=posi[:]), [pos_b], [pos_b])
        cs = sb("cs", [128, NT, 2, 32], F32); cs_b = P.buf()
        tmpi = sb("rtmpi", [128, NT, 2, 32], I32); tmpf = sb("rtmpf", [128, NT, 2, 32], F32)
        for tt in range(NT):
            for j in range(2):
                P.op("dve", lambda e, tt=tt, j=j: e.tensor_scalar(out=cs[:, tt, j, :], in0=invf[:], scalar1=posf[:, tt:tt + 1], scalar2=None, op0=ALU.mult),
                     [pos_b, invf_b], [cs_b])
        emit_sincos_reduce(P, cs[:, :, 0, :], tmpi[:, :, 0, :], tmpf[:, :, 0, :], [cs_b], 1.5 * math.pi)
        emit_sincos_reduce(P, cs[:, :, 1, :], tmpi[:, :, 1, :], tmpf[:, :, 1, :], [cs_b], math.pi)
        P.op("act", lambda e: e.activation(out=cs[:], in_=cs[:], func=AF.Sin), [cs_b], [cs_b])

        hT = [sb(f"hT{i}", [128, 8, 128], BF16) for i in range(2)]; hT_b = P.bufs(2)
        p_lat = ps("p_lat", [128, 512], F32); lat_b = P.buf()
        p_tr = ps("p_tr", [128, 1024], BF16); tr_b = P.buf()
        p_tr2 = ps("p_tr2", [128, 1024], BF16); tr2_b = P.buf()
        p_q = [ps(f"p_q{i}", [128, 512], F32) for i in range(3)]; q_b = P.bufs(3)
        p_kv = [ps("p_kv0", [128, 512], F32), p_lat]; kv_b = [P.buf(), lat_b]; kvser_b = P.buf()
        ss3_l = [sb(f"ss3{i}", [128, 3], F32) for i in range(2)]; ss3_bl = P.bufs(2)
        junk_l = [sb("junk448", [128, 1536], F32)] * 2; junk_bl = [P.buf()] * 2
        cqn_l = [sb(f"cqn{i}", [128, 384], BF16) for i in range(2)]; cqn_bl = P.bufs(2)
        kpn_l = [sb(f"kpn{i}", [128, 64], F32) for i in range(2)]; kpn_bl = P.bufs(2)
        cT_l = [sb(f"cT{i}", [128, 3, 128], BF16) for i in range(2)]; cT_bl = P.bufs(2)
        sqq_l = [sb(f"sqq{i}", [128, 1536], F32) for i in range(2)]; sqq_bl = P.bufs(2)
        rq_l = [sb(f"rq{i}", [128, 16], F32) for i in range(2)]; rq_bl = P.bufs(2)
        qn_l = [sb(f"qn{i}", [128, 8, 128], BF16) for i in range(2)]; qn_bl = P.bufs(2)
        qr_l = [sb(f"qr{i}", [128, 8, 64], F32) for i in range(2)]; qr_bl = P.bufs(2)
        qr2_l = [sb(f"qr2{i}", [128, 8, 64], F32) for i in range(2)]; qr2_bl = P.bufs(2)
        qpe_l = [sb(f"qpe{i}", [128, 8, 64], BF16) for i in range(2)]; qpe_bl = P.bufs(2)
        kraw_l = [sb(f"kraw{i}", [128, 8, 128], F32) for i in range(2)]; kraw_bl = P.bufs(2)
        sqk_l = [sb("sqk", [128, 8, 128], F32)] * 2; sqk_bl = [P.buf()] * 2
        rk_l = [sb(f"rk{i}", [128, 8], F32) for i in range(2)]; rk_bl = P.bufs(2)
        kn_l = [sb(f"kn{i}", [128, 8, 128], BF16) for i in range(2)]; kn_bl = P.bufs(2)
        kpe_l = [sb(f"kpe{i}", [128, 64], BF16) for i in range(2)]; kpe_bl = P.bufs(2)
        kp2_l = [sb(f"kp2{i}", [128, 64], F32) for i in range(2)]; kp2_bl = P.bufs(2)
        vext = [sb(f"vext{i}", [128, 8, 130], BF16) for i in range(2)]; vext_b = P.bufs(2)
        for i in range(2):
            P.op("pool", lambda e, i=i: e.memset(vext[i][:], 1.0), [], [vext_b[i]])
        sQN = [sb(f"sQN{i}", [128, 8, 512], BF16) for i in range(2)]; sQN_b = P.bufs(2)
        sKN = [sb(f"sKN{i}", [128, 8, 512], BF16) for i in range(2)]; sKN_b = P.bufs(2)
        sQP = [sb(f"sQP{i}", [128, 4, 512], BF16) for i in range(2)]; sQP_b = P.bufs(2)
        sKP = [sb(f"sKP{i}", [64, 512], BF16) for i in range(2)]; sKP_b = P.bufs(2)

        def rope(src, dst, cosv, sinv, nh, tmp, rb, wb):
            x1 = src[:, :, 0:32] if nh else src[:, 0:32]
            x2 = src[:, :, 32:64] if nh else src[:, 32:64]
            t1 = tmp[:, :, 0:32] if nh else tmp[:, 0:32]
            t2 = tmp[:, :, 32:64] if nh else tmp[:, 32:64]
            d1 = dst[:, :, 0:32] if nh else dst[:, 0:32]
            d2 = dst[:, :, 32:64] if nh else dst[:, 32:64]
            if nh:
                cb = cosv.unsqueeze(1).to_broadcast([128, nh, 32])
                sn = sinv.unsqueeze(1).to_broadcast([128, nh, 32])
            else:
                cb, sn = cosv, sinv
            P.op("dve", lambda e: e.tensor_tensor(out=t1, in0=x2, in1=sn, op=ALU.mult), rb, wb)
            P.op("dve", lambda e: e.tensor_tensor(out=t2, in0=x1, in1=sn, op=ALU.mult), rb, wb)
            P.op("dve", lambda e: e.tensor_tensor(out=x1, in0=x1, in1=cb, op=ALU.mult), rb, rb)
            P.op("dve", lambda e: e.tensor_tensor(out=x2, in0=x2, in1=cb, op=ALU.mult), rb, rb)
            P.op("dve", lambda e: e.tensor_tensor(out=d1, in0=x1, in1=t1, op=ALU.subtract), rb + wb, wb + [P.buf()] if False else wb)
            P.op("dve", lambda e: e.tensor_tensor(out=d2, in0=x2, in1=t2, op=ALU.add), rb + wb, wb)

        cut(1)

        def do_tile(tt):
            ss3, ss3_b = ss3_l[tt % 2], ss3_bl[tt % 2]
            junk, junk_b = junk_l[tt % 2], junk_bl[tt % 2]
            cqn, cqn_b = cqn_l[tt % 2], cqn_bl[tt % 2]
            kpn, kpn_b = kpn_l[tt % 2], kpn_bl[tt % 2]
            cT, cT_b = cT_l[tt % 2], cT_bl[tt % 2]
            sqq, sqq_b = sqq_l[tt % 2], sqq_bl[tt % 2]
            rq, rq_b = rq_l[tt % 2], rq_bl[tt % 2]
            qn, qn_b = qn_l[tt % 2], qn_bl[tt % 2]
            qr, qr_b = qr_l[tt % 2], qr_bl[tt % 2]
            qr2, qr2_b = qr2_l[tt % 2], qr2_bl[tt % 2]
            qpe, qpe_b = qpe_l[tt % 2], qpe_bl[tt % 2]
            kraw, kraw_b = kraw_l[tt % 2], kraw_bl[tt % 2]
            sqk, sqk_b = sqk_l[tt % 2], sqk_bl[tt % 2]
            rk, rk_b = rk_l[tt % 2], rk_bl[tt % 2]
            kn, kn_b = kn_l[tt % 2], kn_bl[tt % 2]
            kpe, kpe_b = kpe_l[tt % 2], kpe_bl[tt % 2]
            kp2, kp2_b = kp2_l[tt % 2], kp2_bl[tt % 2]
            hi = tt % 2
            g = (tt // 4) % 2
            j4 = tt % 4
            nctx.run(K.xs[tt * 128:(tt + 1) * 128, :], 0, hT[hi][:], hT_b[hi])
            yield
            for kc in range(8):
                P.op("pe", lambda e, kc=kc, hi=hi: e.matmul(p_lat[:, 0:448], lhsT=hT[hi][:, kc, :], rhs=wdn[:, kc, :], start=(kc == 0), stop=(kc == 7)),
                     [hT_b[hi], wdn_b], [lat_b])
            for j, (a, b) in enumerate(((0, 256), (256, 384), (384, 448))):
                P.op("act", lambda e, j=j, a=a, b=b: e.activation(out=junk[:, a:b], in_=p_lat[:, a:b], func=AF.Square, accum_out=ss3[:, j:j + 1]),
                     [lat_b], [junk_b, ss3_b])
            P.op("dve", lambda e: e.tensor_tensor(out=ss3[:], in0=ss3[:], in1=dv3[:], op=ALU.mult), [ss3_b, dv_b], [ss3_b])
            P.op("act", lambda e: e.activation(out=ss3[:], in_=ss3[:], func=AF.Sqrt, bias=EPS), [ss3_b], [ss3_b])
            P.op("dve", lambda e: e.reciprocal(out=ss3[:], in_=ss3[:]), [ss3_b], [ss3_b])
            P.op("dve", lambda e: e.scalar_tensor_tensor(out=cqn[:, 0:256], in0=p_lat[:, 0:256], scalar=ss3[:, 0:1], in1=gqa[:], op0=ALU.mult, op1=ALU.mult),
                 [lat_b, ss3_b, gqa_b], [cqn_b])
            P.op("dve", lambda e: e.scalar_tensor_tensor(out=cqn[:, 256:384], in0=p_lat[:, 256:384], scalar=ss3[:, 1:2], in1=gkva[:], op0=ALU.mult, op1=ALU.mult),
                 [lat_b, ss3_b, gkva_b], [cqn_b])
            P.op("dve", lambda e: e.scalar_tensor_tensor(out=kpn[:], in0=p_lat[:, 384:448], scalar=ss3[:, 2:3], in1=gk[:, 128:192], op0=ALU.mult, op1=ALU.mult),
                 [lat_b, ss3_b, gk_b], [kpn_b])
            cut(3)
            rope(kpn, kpe, cs[:, tt, 0, :], cs[:, tt, 1, :], 0, kp2, [kpn_b, cs_b], [kpe_b, kp2_b])
            cut(4)
            for j in range(3):
                P.op("pe", lambda e, j=j: e.transpose(p_tr2[:, j * 128:(j + 1) * 128], cqn[:, j * 128:(j + 1) * 128], K.identb[:]), [cqn_b], [tr2_b])
            P.op("act", lambda e: e.copy(out=cT[:], in_=p_tr2[:, 0:384].rearrange("p (k t) -> p k t", k=3)), [tr2_b], [cT_b])
            yield
            for n3 in range(3):
                for kc in range(2):
                    P.op("pe", lambda e, n3=n3, kc=kc: e.matmul(p_q[n3][:], lhsT=cT[:, kc, :], rhs=wuq[:, kc, n3 * 512:(n3 + 1) * 512], start=(kc == 0), stop=(kc == 1)),
                         [cT_b, wuq_b], [q_b[n3]])
            cut(5)
            for n3 in range(3):
                P.op("act", lambda e, n3=n3: e.activation(out=sqq[:, n3 * 512:(n3 + 1) * 512], in_=p_q[n3][:], func=AF.Square), [q_b[n3]], [sqq_b])
            sq3 = sqq[:].rearrange("p (h d) -> p h d", h=8)
            P.op("dve", lambda e: e.tensor_reduce(out=rq[:, 0:8], in_=sq3[:, :, 0:128], axis=AX.X, op=ALU.add), [sqq_b], [rq_b])
            P.op("dve", lambda e: e.tensor_reduce(out=rq[:, 8:16], in_=sq3[:, :, 128:192], axis=AX.X, op=ALU.add), [sqq_b], [rq_b])
            P.op("dve", lambda e: e.tensor_tensor(out=rq[:], in0=rq[:], in1=dv16[:], op=ALU.mult), [rq_b, dv_b], [rq_b])
            P.op("act", lambda e: e.activation(out=rq[:], in_=rq[:], func=AF.Sqrt, bias=EPS), [rq_b], [rq_b])
            P.op("dve", lambda e: e.reciprocal(out=rq[:], in_=rq[:]), [rq_b], [rq_b])
            for h in range(8):
                for (lo, hi_, kind) in ((h * 192, h * 192 + 128, 0), (h * 192 + 128, h * 192 + 192, 1)):
                    c0 = lo
                    while c0 < hi_:
                        bnk = c0 // 512
                        c1 = min(hi_, (bnk + 1) * 512)
                        off = c0 - lo
                        w = c1 - c0
                        src = p_q[bnk][:, c0 - bnk * 512:c1 - bnk * 512]
                        if kind == 0:
                            P.op("dve", lambda e, src=src, h=h, off=off, w=w: e.tensor_scalar(out=qn[:, h, off:off + w], in0=src, scalar1=rq[:, h:h + 1], scalar2=None, op0=ALU.mult),
                                 [q_b[bnk], rq_b], [qn_b])
                        else:
                            P.op("dve", lambda e, src=src, h=h, off=off, w=w: e.scalar_tensor_tensor(out=qr[:, h, off:off + w], in0=src, scalar=rq[:, 8 + h:9 + h], in1=gq[:, 128 + off:128 + off + w], op0=ALU.mult, op1=ALU.mult),
                                 [q_b[bnk], rq_b, gq_b], [qr_b])
                        c0 = c1
            cut(6)
            rope(qr, qpe, cs[:, tt, 0, :], cs[:, tt, 1, :], 8, qr2, [qr_b, cs_b], [qpe_b, qr2_b])
            cut(7)
            vi = tt % 2
            for j in range(4):
                pb_ = p_kv[j % 2]
                P.op("pe", lambda e, j=j, pb_=pb_: e.matmul(pb_[:], lhsT=cT[:, 2, :], rhs=wukv[:, j * 512:(j + 1) * 512], start=True, stop=True),
                     [cT_b, wukv_b], [kv_b[j % 2]])
                kvv = pb_[:].rearrange("p (h d) -> p h d", h=2)
                cut(7.1)
                P.op("act", lambda e, j=j, kvv=kvv: e.activation(out=sqk[:, 2 * j:2 * j + 2, :], in_=kvv[:, :, 0:128], func=AF.Square), [kv_b[j % 2]], [sqk_b, kvser_b])
                cut(7.2)
                P.op("dve", lambda e, j=j, kvv=kvv: e.tensor_copy(out=kraw[:, 2 * j:2 * j + 2, :], in_=kvv[:, :, 0:128]), [kv_b[j % 2]], [kraw_b, kvser_b])
                cut(7.3)
                P.op("act", lambda e, j=j, kvv=kvv, vi=vi: e.copy(out=vext[vi][:, 2 * j:2 * j + 2, 0:128], in_=kvv[:, :, 128:256]), [kv_b[j % 2]], [vext_b[vi], kvser_b])
                cut(7.4 + 0.01 * j)
            cut(7.5)
            P.op("dve", lambda e: e.tensor_reduce(out=rk[:], in_=sqk[:], axis=AX.X, op=ALU.add), [sqk_b], [rk_b])
            cut(7.6)
            P.op("act", lambda e: e.activation(out=rk[:], in_=rk[:], func=AF.Sqrt, scale=1.0 / 128, bias=EPS), [rk_b], [rk_b])
            P.op("dve", lambda e: e.reciprocal(out=rk[:], in_=rk[:]), [rk_b], [rk_b])
            cut(7.7)
            P.op("dve", lambda e: e.tensor_tensor(out=kraw[:], in0=kraw[:], in1=rk[:].unsqueeze(2).to_broadcast([128, 8, 128]), op=ALU.mult), [kraw_b, rk_b], [kraw_b])
            cut(7.8)
            P.op("pool", lambda e: e.tensor_tensor(out=kn[:], in0=kraw[:], in1=gqk[:].unsqueeze(1).to_broadcast([128, 8, 128]), op=ALU.mult), [kraw_b, gqk_b], [kn_b])
            cut(8)
            P.dma("sp", K.V[:, tt * 128:(tt + 1) * 128, :].rearrange("h p e -> p h e"), vext[vi][:], reads=[vext_b[vi]])
            yield
            for h in range(8):
                P.op("pe", lambda e, h=h: e.transpose(p_tr[:, h * 128:(h + 1) * 128], qn[:, h, :], K.identb[:]), [qn_b], [tr_b])
            P.op("act", lambda e, g=g, j4=j4: e.copy(out=sQN[g][:, :, j4 * 128:(j4 + 1) * 128], in_=p_tr[:].rearrange("p (h t) -> p h t", h=8)), [tr_b], [sQN_b[g]])
            for h in range(8):
                P.op("pe", lambda e, h=h: e.transpose(p_tr[:, h * 128:(h + 1) * 128], kn[:, h, :], K.identb[:]), [kn_b], [tr_b])
            P.op("dve", lambda e, g=g, j4=j4: e.tensor_copy(out=sKN[g][:, :, j4 * 128:(j4 + 1) * 128], in_=p_tr[:].rearrange("p (h t) -> p h t", h=8)), [tr_b], [sKN_b[g]])
            qpe2 = qpe[:].rearrange("p (a b) d -> p a (b d)", b=2)
            for a in range(4):
                P.op("pe", lambda e, a=a: e.transpose(p_tr[:, a * 128:(a + 1) * 128], qpe2[:, a, :], K.identb[:]), [qpe_b], [tr_b])
            P.op("pe", lambda e: e.transpose(p_tr[0:64, 512:640], kpe[:], K.identb[:]), [kpe_b], [tr_b])
            P.op("act", lambda e, g=g, j4=j4: e.copy(out=sQP[g][:, :, j4 * 128:(j4 + 1) * 128], in_=p_tr[:, 0:512].rearrange("p (h t) -> p h t", h=4)), [tr_b], [sQP_b[g]])
            P.op("dve", lambda e, g=g, j4=j4: e.tensor_copy(out=sKP[g][:, j4 * 128:(j4 + 1) * 128], in_=p_tr[0:64, 512:640]), [tr_b], [sKP_b[g]])
            cut(10)
            if j4 == 3:
                t0 = (tt // 4) * 512
                P.dma("sp", K.QN[:, :, t0:t0 + 512].rearrange("h d t -> d h t"), sQN[g][:], reads=[sQN_b[g]])
                P.dma("sp", K.KN[:, :, t0:t0 + 512].rearrange("h d t -> d h t"), sKN[g][:], reads=[sKN_b[g]])
                P.dma("sp", K.QP[:, :, t0:t0 + 512].rearrange("h d t -> d h t"), sQP[g][:], reads=[sQP_b[g]])
                P.dma("sp", K.KP[:, t0:t0 + 512], sKP[g][:], reads=[sKP_b[g]])
        gens = [do_tile(tt) for tt in range(NTT)]
        NST = 4
        for step in range(NTT + NST - 1):
            for s_ in range(NST - 1, -1, -1):
                tt = step - s_
                if 0 <= tt < NTT:
                    try:
                        next(gens[tt])
                    except StopIteration:
                        pass
    run_phase(K, body)


def phase_attention(K):
    scale = 192 ** -0.5

    def body(P, sb, ps):
        kp = sb("kp", [64, S], BF16); kp_b = P.buf()
        P.dma("sp", kp[:], K.KP, writes=[kp_b])
        ones = sb("ones_b", [128, 128], BF16); ones_b = P.buf()
        P.op("dve", lambda e: e.memset(ones[:], 1.0), [], [ones_b])
        qn = [sb(f"aqn{i}", [128, S], BF16) for i in range(2)]; qn_b = P.bufs(2)
        kn = [sb(f"akn{i}", [128, S], BF16) for i in range(2)]; kn_b = P.bufs(2)
        qp = [sb(f"aqp{i}", [64, S], BF16) for i in range(2)]; qp_b = P.bufs(2)
        vv = [sb(f"av{i}", [128, NT, 130], BF16) for i in range(2)]; vv_b = P.bufs(2)
        NS = 4
        p_s = [ps(f"p_s{i}", [128, 512], F32) for i in range(NS)]; s_b = P.bufs(NS)
        p_o = [ps(f"p_o{i}", [128, 512], F32) for i in range(2)]; o_b = P.bufs(2)
        p_m = [ps(f"p_m{i}", [128, 512], F32) for i in range(2)]; m_b = P.bufs(2)
        NP = 5
        pt = [sb(f"pt{i}", [128, 512], BF16) for i in range(NP)]; pt_b = P.bufs(NP)
        rinv = [sb(f"rinv{i}", [128, 512], F32) for i in range(2)]; rinv_b = P.bufs(2)
        ost = [sb(f"ost{i}", [128, 512], BF16) for i in range(2)]; ost_b = P.bufs(2)

        def load_head(h):
            hi = h % 2
            P.dma("sp", qn[hi][:], K.QN[h], writes=[qn_b[hi]])
            P.dma("pool", kn[hi][:], K.KN[h], writes=[kn_b[hi]])
            P.dma("sp", qp[hi][:], K.QP[h // 2, (h % 2) * 64:(h % 2) * 64 + 64, :], writes=[qp_b[hi]])
            P.dma("pool", vv[hi][:], K.V[h].rearrange("(kt p) e -> p kt e", p=128), writes=[vv_b[hi]])

        iters = [(h, qg, kt) for h in range(8) for qg in range(8) for kt in range(NT)]

        def emit_s(i):
            h, qg, kt = iters[i]
            hi = h % 2
            si = i % NS
            P.op("pe", lambda e: e.matmul(p_s[si][:], lhsT=kn[hi][:, kt * 128:(kt + 1) * 128], rhs=qn[hi][:, qg * 512:(qg + 1) * 512], start=True, stop=False),
                 [kn_b[hi], qn_b[hi]], [s_b[si]])
            P.op("pe", lambda e: e.matmul(p_s[si][:], lhsT=kp[:, kt * 128:(kt + 1) * 128], rhs=qp[hi][:, qg * 512:(qg + 1) * 512], start=False, stop=True),
                 [kp_b, qp_b[hi]], [s_b[si]])

        def emit_rest(i):
            h, qg, kt = iters[i]
            hi = h % 2
            si = i % NS
            pi = i % NP
            oi = (h * 8 + qg) % 2
            P.op("act", lambda e: e.activation(out=pt[pi][:], in_=p_s[si][:], func=AF.Exp, scale=scale), [s_b[si]], [pt_b[pi]])
            P.op("pe", lambda e: e.matmul(p_o[oi][:], lhsT=vv[hi][:, kt, 0:128], rhs=pt[pi][:], start=(kt == 0), stop=(kt == NT - 1)),
                 [pt_b[pi], vv_b[hi]], [o_b[oi]])
            P.op("pe", lambda e: e.matmul(p_m[oi][:], lhsT=ones[:], rhs=pt[pi][:], start=(kt == 0), stop=(kt == NT - 1)),
                 [pt_b[pi], ones_b], [m_b[oi]])
            if kt == NT - 1:
                P.op("dve", lambda e: e.reciprocal(out=rinv[oi][:], in_=p_m[oi][:]), [m_b[oi]], [rinv_b[oi]])
                P.op("dve", lambda e: e.tensor_tensor(out=ost[oi][:], in0=p_o[oi][:], in1=rinv[oi][:], op=ALU.mult), [o_b[oi], rinv_b[oi]], [ost_b[oi]])
                P.dma("sp", K.OT[h, :, qg * 512:(qg + 1) * 512], ost[oi][:], reads=[ost_b[oi]])

        LOOK = 3
        load_head(0)
        load_head(1)
        n = len(iters)
        for i in range(min(LOOK, n)):
            emit_s(i)
        for i in range(n):
            if i + LOOK < n:
                emit_s(i + LOOK)
            emit_rest(i)
            h, qg, kt = iters[i]
            if qg == 0 and kt == 8 and 1 <= h and h + 1 < 8:
                load_head(h + 1)
    run_phase(K, body)


def phase_outproj(K, src_bf, w_ap, layer, src_fm=None):
    def body(P, sb, ps):
        wo = sb("wo", [128, 8, D], BF16); wo_b = P.bufs(8)
        wv = w_ap.rearrange("(kc p) n -> p kc n", p=128)
        for kc in range(8):
            P.dma("pool", wo[:, kc, :], wv[:, kc, :], writes=[wo_b[kc]])
        xt = [sb(f"oxt{i}", [128, D], F32) for i in range(2)]; xt_b = P.bufs(2)
        p_y = [ps(f"p_y{i}", [128, 512], F32) for i in range(4)]; y_b = P.bufs(4)
        tmp = [sb(f"otmp{i}", [128, D], F32) for i in range(2)]; tmp_b = P.bufs(2)
        GATE = K.modb[:, 2 * D:3 * D]
        if src_fm is None:
            ot = [sb(f"ot{i}", [128, D], BF16) for i in range(2)]; ot_b = P.bufs(2)
            oT = [sb(f"oT{i}", [128, 8, 128], BF16) for i in range(2)]; oT_b = P.bufs(2)
            p_tr = ps("p_tr", [128, D], BF16); tr_b = P.buf()
        else:
            og = [sb(f"og{i}", [128, 8, 512], BF16) for i in range(2)]; og_b = P.bufs(2)
        for tt in range(NT):
            i = tt % 2
            P.dma("pool", xt[i][:], K.xs[tt * 128:(tt + 1) * 128, :], writes=[xt_b[i]])
            if src_fm is None:
                P.dma("sp", ot[i][:], src_bf[tt * 128:(tt + 1) * 128, :], writes=[ot_b[i]])
                for kc in range(8):
                    P.op("pe", lambda e, kc=kc, i=i: e.transpose(p_tr[:, kc * 128:(kc + 1) * 128], ot[i][:, kc * 128:(kc + 1) * 128], K.identb[:]), [ot_b[i]], [tr_b])
                P.op("act", lambda e, i=i: e.copy(out=oT[i][:], in_=p_tr[:].rearrange("p (k t) -> p k t", k=8)), [tr_b], [oT_b[i]])
                lhs = lambda kc, i=i: oT[i][:, kc, :]
                lb = oT_b[i]
            else:
                gi = (tt // 4) % 2
                if tt % 4 == 0:
                    t0 = tt * 128
                    P.dma("sp", og[gi][:], src_fm[:, :, t0:t0 + 512].rearrange("h d t -> d h t"), writes=[og_b[gi]])
                lhs = lambda kc, gi=gi, j=tt % 4: og[gi][:, kc, j * 128:(j + 1) * 128]
                lb = og_b[gi]
            for dh in range(2):
                yb = i * 2 + dh
                for kc in range(8):
                    P.op("pe", lambda e, kc=kc, dh=dh, yb=yb, lhs=lhs: e.matmul(p_y[yb][:], lhsT=lhs(kc), rhs=wo[:, kc, dh * 512:(dh + 1) * 512], start=(kc == 0), stop=(kc == 7)),
                         [lb, wo_b[kc]], [y_b[yb]])
                P.op("dve", lambda e, i=i, dh=dh, yb=yb: e.tensor_tensor(out=tmp[i][:, dh * 512:(dh + 1) * 512], in0=p_y[yb][:], in1=GATE[:, dh * 512:(dh + 1) * 512], op=ALU.mult),
                     [y_b[yb]], [tmp_b[i]])
            P.op("dve", lambda e, i=i: e.tensor_tensor(out=xt[i][:], in0=xt[i][:], in1=tmp[i][:], op=ALU.add), [xt_b[i], tmp_b[i]], [xt_b[i]])
            P.dma("sp", K.xs[tt * 128:(tt + 1) * 128, :], xt[i][:], reads=[xt_b[i]])
    run_phase(K, body)


def phase_norm_T(K, sec, router_layer=None):
    I = K.I
    router = router_layer is not None

    def body(P, sb, ps):
        nctx = NormCtx(P, sb, ps, K, want_f32=router, tag="fn")
        hst = [sb(f"hst{i}", [128, 8, 512], BF16) for i in range(2)]; hst_b = P.bufs(2)
        if router:
            layer = router_layer
            h32 = [sb(f"h32T{i}", [128, 8, 128], F32) for i in range(2)]; h32_b = P.bufs(2)
            wr = sb("wr", [128, 8, 20], F32); wr_b = P.buf()
            P.dma("sp", wr[:], I["moe_wr"][layer].rearrange("(kc p) n -> p kc n", p=128), writes=[wr_b])
            br, br_b = load_bcast(P, sb, "br", I["moe_br"][layer:layer + 1, :], 20)
            combT = sb("combT", [16, S], BF16); combT_b = P.buf()
            p_r = ps("p_r", [128, 512], F32); r_b = P.buf()
            lg_l = [sb(f"lg{i}", [128, 20], F32) for i in range(2)]; lg_bl = P.bufs(2)
            m8_l = [sb(f"m8{i}", [128, 8], F32) for i in range(2)]; m8_bl = P.bufs(2)
            gm_l = [sb(f"gmask{i}", [128, 4], F32) for i in range(2)]; gm_bl = P.bufs(2)
            gw_l = [sb(f"gw{i}", [128, 4], F32) for i in range(2)]; gw_bl = P.bufs(2)
            em_l = [sb(f"em{i}", [128, 4, 4], F32) for i in range(2)]; em_bl = P.bufs(2)
            ex_l = [sb(f"ex{i}", [128, 16], F32) for i in range(2)]; ex_bl = P.bufs(2)
            cmb_l = [sb(f"cmb{i}", [128, 16], F32) for i in range(2)]; cmb_bl = P.bufs(2)
            den_l = [sb(f"den{i}", [128, 2], F32) for i in range(2)]; den_bl = P.bufs(2)
            BIG = 1.0e4
        a1 = {}

        def stage_a1(tt):
            a1[tt] = nctx.run_a1(K.xs[tt * 128:(tt + 1) * 128, :], sec)

        def stage_a2(tt):
            i = tt % 2
            g = (tt // 4) % 2
            j4 = tt % 4
            if router:
                nctx.run_a2(a1[tt], hst[g][:, :, j4 * 128:(j4 + 1) * 128], hst_b[g], h32[i][:], h32_b[i])
            else:
                nctx.run_a2(a1[tt], hst[g][:, :, j4 * 128:(j4 + 1) * 128], hst_b[g])
            if j4 == 3:
                t0 = (tt // 4) * 512
                P.dma("sp", K.HT[:, :, t0:t0 + 512], hst[g][:], reads=[hst_b[g]])

        def do_tile(tt):
            if not router:
                return
            lg, lg_b = lg_l[tt % 2], lg_bl[tt % 2]
            m8, m8_b = m8_l[tt % 2], m8_bl[tt % 2]
            gm, gm_b = gm_l[tt % 2], gm_bl[tt % 2]
            gw, gw_b = gw_l[tt % 2], gw_bl[tt % 2]
            em, em_b = em_l[tt % 2], em_bl[tt % 2]
            ex, ex_b = ex_l[tt % 2], ex_bl[tt % 2]
            cmb, cmb_b = cmb_l[tt % 2], cmb_bl[tt % 2]
            den, den_b = den_l[tt % 2], den_bl[tt % 2]
            i = tt % 2
            for kc in range(8):
                P.op("pe", lambda e, kc=kc, i=i: e.matmul(p_r[:, 0:20], lhsT=h32[i][:, kc, :], rhs=wr[:, kc, :], start=(kc == 0), stop=(kc == 7)),
                     [h32_b[i], wr_b], [r_b])
            P.op("dve", lambda e: e.tensor_tensor(out=lg[:], in0=p_r[:, 0:20], in1=br[:], op=ALU.add), [r_b, br_b], [lg_b])
            P.op("dve", lambda e: e.tensor_reduce(out=m8[:, 0:1], in_=lg[:, 0:4], axis=AX.X, op=ALU.max), [lg_b], [m8_b])
            P.op("dve", lambda e: e.tensor_scalar(out=gm[:], in0=lg[:, 0:4], scalar1=m8[:, 0:1], scalar2=None, op0=ALU.is_ge), [lg_b, m8_b], [gm_b])
            P.op("dve", lambda e: e.tensor_scalar(out=gw[:], in0=lg[:, 0:4], scalar1=m8[:, 0:1], scalar2=None, op0=ALU.subtract), [lg_b, m8_b], [gw_b])
            P.op("act", lambda e: e.activation(out=gw[:], in_=gw[:], func=AF.Exp, accum_out=den[:, 0:1]), [gw_b], [gw_b, den_b])
            P.op("dve", lambda e: e.tensor_scalar(out=gm[:], in0=gm[:], scalar1=BIG, scalar2=-BIG, op0=ALU.mult, op1=ALU.add), [gm_b], [gm_b])
            P.op("dve", lambda e: e.tensor_tensor(out=em[:], in0=lg[:, 4:20].rearrange("p (g k) -> p g k", g=4), in1=gm[:].unsqueeze(2).to_broadcast([128, 4, 4]), op=ALU.add),
                 [lg_b, gm_b], [em_b])
            emf = em[:].rearrange("p g k -> p (g k)")
            P.op("dve", lambda e: e.max(out=m8[:], in_=emf), [em_b], [m8_b])
            P.op("dve", lambda e: e.tensor_scalar(out=ex[:], in0=emf, scalar1=m8[:, 0:1], scalar2=None, op0=ALU.subtract), [em_b, m8_b], [ex_b])
            P.op("act", lambda e: e.activation(out=ex[:], in_=ex[:], func=AF.Exp), [ex_b], [ex_b])
            P.op("dve", lambda e: e.tensor_scalar(out=cmb[:], in0=emf, scalar1=m8[:, 1:2], scalar2=None, op0=ALU.is_ge), [em_b, m8_b], [cmb_b])
            P.op("dve", lambda e: e.tensor_tensor(out=cmb[:], in0=cmb[:], in1=ex[:], op=ALU.mult), [cmb_b, ex_b], [cmb_b])
            P.op("dve", lambda e: e.tensor_reduce(out=den[:, 1:2], in_=cmb[:], axis=AX.X, op=ALU.add), [cmb_b], [den_b])
            P.op("dve", lambda e: e.tensor_tensor(out=den[:, 0:1], in0=den[:, 0:1], in1=den[:, 1:2], op=ALU.mult), [den_b], [den_b])
            P.op("dve", lambda e: e.reciprocal(out=den[:, 0:1], in_=den[:, 0:1]), [den_b], [den_b])
            P.op("dve", lambda e: e.tensor_scalar(out=cmb[:], in0=cmb[:], scalar1=den[:, 0:1], scalar2=None, op0=ALU.mult), [cmb_b, den_b], [cmb_b])
            P.op("pe", lambda e: e.transpose(p_r[0:16, 128:256], cmb[:], K.ident32[:]), [cmb_b], [r_b])
            P.op("act", lambda e, tt=tt: e.copy(out=combT[:, tt * 128:(tt + 1) * 128], in_=p_r[0:16, 128:256]), [r_b], [combT_b])
        stage_a1(0)
        stage_a1(1)
        stage_a2(0)
        for tt in range(NT):
            if tt + 2 < NT:
                stage_a1(tt + 2)
            if tt + 1 < NT:
                stage_a2(tt + 1)
            do_tile(tt)
        if router:
            P.dma("sp", K.CT, combT[:], reads=[combT_b])
    run_phase(K, body)


def phase_moe_a(K, layer):
    I = K.I

    def body(P, sb, ps):
        hT = sb("hT_all", [128, 8, S], BF16); hT_b = P.bufs(8)
        for kc in range(8):
            P.dma("sp" if kc % 2 == 0 else "pool", hT[:, kc, :], K.HT[:, kc, :], writes=[hT_b[kc]])
        sel = sb("sel", [16, 16 * 128], BF16); sel_b = P.buf()
        P.dma("sp", sel[:], I["sel"], writes=[sel_b])
        combT = sb("combT", [16, S], BF16); combT_b = P.buf()
        P.dma("sp", combT[:], K.CT, writes=[combT_b])
        wg = [sb(f"wg{i}", [128, 8, DE], BF16) for i in range(2)]; wg_b = P.bufs(2)
        wu = [sb(f"wu{i}", [128, 8, DE], BF16) for i in range(2)]; wu_b = P.bufs(2)
        p_g = [ps(f"p_g{i}", [128, 512], F32) for i in range(2)]; g_b = P.bufs(2)
        p_u = [ps(f"p_u{i}", [128, 512], F32) for i in range(2)]; u_b = P.bufs(2)
        p_c = [ps(f"p_c{i}", [128, 512], F32) for i in range(2)]; c_b = P.bufs(2)
        cb = [sb(f"cb{i}", [128, 512], F32) for i in range(2)]; cb_b = P.bufs(2)
        sg = [sb(f"sg{i}", [128, 512], F32) for i in range(2)]; sg_b = P.bufs(2)
        tu = [sb(f"tu{i}", [128, 512], F32) for i in range(2)]; tu_b = P.bufs(2)
        ast = [sb(f"ast{i}", [128, 2, 512], BF16) for i in range(2)]; ast_b = P.bufs(2)
        it = 0
        ci = 0
        for ex_i in range(NE):
            wi = ex_i % 2
            P.dma("pool", wg[wi][:], I["moe_w_gate"][layer, ex_i].rearrange("(kc p) n -> p kc n", p=128), writes=[wg_b[wi]])
            P.dma("pool", wu[wi][:], I["moe_w_up"][layer, ex_i].rearrange("(kc p) n -> p kc n", p=128), writes=[wu_b[wi]])
            for tq in range(8):
                c_i = ci % 2
                ci += 1
                P.op("pe", lambda e, ex_i=ex_i, tq=tq, c_i=c_i: e.matmul(p_c[c_i][:], lhsT=sel[:, ex_i * 128:(ex_i + 1) * 128], rhs=combT[:, tq * 512:(tq + 1) * 512], start=True, stop=True),
                     [sel_b, combT_b], [c_b[c_i]])
                P.op("act", lambda e, c_i=c_i: e.copy(out=cb[c_i][:], in_=p_c[c_i][:]), [c_b[c_i]], [cb_b[c_i]])
                for hc in range(2):
                    k = it % 2
                    it += 1
                    for kc in range(8):
                        P.op("pe", lambda e, k=k, kc=kc, wi=wi, hc=hc, tq=tq: e.matmul(p_g[k][:], lhsT=wg[wi][:, kc, hc * 128:(hc + 1) * 128], rhs=hT[:, kc, tq * 512:(tq + 1) * 512], start=(kc == 0), stop=(kc == 7)),
                             [wg_b[wi], hT_b[kc]], [g_b[k]])
                    for kc in range(8):
                        P.op("pe", lambda e, k=k, kc=kc, wi=wi, hc=hc, tq=tq: e.matmul(p_u[k][:], lhsT=wu[wi][:, kc, hc * 128:(hc + 1) * 128], rhs=hT[:, kc, tq * 512:(tq + 1) * 512], start=(kc == 0), stop=(kc == 7)),
                             [wu_b[wi], hT_b[kc]], [u_b[k]])
                    P.op("act", lambda e, k=k: e.activation(out=sg[k][:], in_=p_g[k][:], func=AF.Silu), [g_b[k]], [sg_b[k]])
                    P.op("dve", lambda e, k=k: e.tensor_tensor(out=tu[k][:], in0=sg[k][:], in1=p_u[k][:], op=ALU.mult), [sg_b[k], u_b[k]], [tu_b[k]])
                    P.op("pool", lambda e, k=k, c_i=c_i, hc=hc: e.tensor_tensor(out=ast[c_i][:, hc, :], in0=tu[k][:], in1=cb[c_i][:], op=ALU.mult), [tu_b[k], cb_b[c_i]], [ast_b[c_i]])
                P.dma("sp", K.AT[ex_i, :, :, tq * 512:(tq + 1) * 512].rearrange("c p t -> p c t"), ast[c_i][:], reads=[ast_b[c_i]])
    run_phase(K, body)


def phase_moe_b(K, layer):
    I = K.I
    final = (layer == 1)

    def body(P, sb, ps):
        wd = sb("wd", [128, NE, 2, D], BF16); wd_b = P.bufs(NE)
        for e_ in range(NE):
            P.dma("pool", wd[:, e_, :, :], I["moe_w_down"][layer, e_].rearrange("(c p) n -> p c n", p=128), writes=[wd_b[e_]])
        at = [sb(f"at{i}", [128, NE, 2, 512], BF16) for i in range(2)]; at_b = P.bufs(2)
        xt = [sb(f"mxt{i}", [128, D], F32) for i in range(2)]; xt_b = P.bufs(2)
        tmp = [sb(f"mtmp{i}", [128, D], F32) for i in range(2)]; tmp_b = P.bufs(2)
        p_y = [ps(f"p_y{i}", [128, 512], F32) for i in range(4)]; y_b = P.bufs(4)
        GATE = K.modb[:, 5 * D:6 * D]
        dst = K.out if final else K.xs
        for tq in range(8):
            ai = tq % 2
            P.dma("sp", at[ai][:], K.AT[:, :, :, tq * 512:(tq + 1) * 512].rearrange("e c p t -> p e c t"), writes=[at_b[ai]])
            for j in range(4):
                tt = tq * 4 + j
                i = tt % 2
                P.dma("sp", xt[i][:], K.xs[tt * 128:(tt + 1) * 128, :], writes=[xt_b[i]])
                for dh in range(2):
                    yb = i * 2 + dh
                    n = 0
                    for e_ in range(NE):
                        for c in range(2):
                            P.op("pe", lambda e, ai=ai, e_=e_, c=c, j=j, dh=dh, yb=yb, n=n: e.matmul(p_y[yb][:], lhsT=at[ai][:, e_, c, j * 128:(j + 1) * 128], rhs=wd[:, e_, c, dh * 512:(dh + 1) * 512], start=(n == 0), stop=(n == 2 * NE - 1)),
                                 [at_b[ai], wd_b[e_]], [y_b[yb]])
                            n += 1
                    P.op("dve", lambda e, i=i, dh=dh, yb=yb: e.tensor_tensor(out=tmp[i][:, dh * 512:(dh + 1) * 512], in0=p_y[yb][:], in1=GATE[:, dh * 512:(dh + 1) * 512], op=ALU.mult),
                         [y_b[yb]], [tmp_b[i]])
                P.op("pool", lambda e, i=i: e.tensor_tensor(out=xt[i][:], in0=xt[i][:], in1=tmp[i][:], op=ALU.add), [xt_b[i], tmp_b[i]], [xt_b[i]])
                P.dma("sp", dst[tt * 128:(tt + 1) * 128, :], xt[i][:], reads=[xt_b[i]])
    run_phase(K, body)


def phase_hyena_in(K):
    I = K.I

    def body(P, sb, ps):
        hT = sb("hT_all", [128, 8, S], BF16); hT_b = P.bufs(8)
        for kc in range(8):
            P.dma("sp", hT[:, kc, :], K.HT[:, kc, :], writes=[hT_b[kc]])
        bin_ = sb("bin", [128, 24], F32); cw = sb("cw", [128, 3, 24], F32); cbv = sb("cbv", [128, 24], F32); par_b = P.buf()
        P.dma("sp", bin_[:], I["hy_b_in_t"], writes=[par_b])
        P.dma("sp", cw[:], I["hy_conv_w_t"], writes=[par_b])
        P.dma("sp", cbv[:], I["hy_conv_b_t"], writes=[par_b])
        wv = I["hy_w_in"].rearrange("(kc p) n -> p kc n", p=128)
        wi = [sb(f"wi{i}", [128, 8, 128], BF16) for i in range(2)]; wi_b = P.bufs(2)
        p_u = [ps(f"p_u{i}", [128, 512], F32) for i in range(3)]; pu_b = P.bufs(3)
        urow = [sb(f"urow{i}", [128, S + 2], F32) for i in range(2)]; ur_b = P.bufs(2)
        for i in range(2):
            P.op("pool", lambda e, i=i: e.memset(urow[i][:, 0:1], 0.0), [], [ur_b[i]])
            P.op("pool", lambda e, i=i: e.memset(urow[i][:, S + 1:S + 2], 0.0), [], [ur_b[i]])
        ucT = sb("ucT0", [128, S], F32); uc_b = P.buf()
        p_t = [ps(f"p_t{i}", [128, 1024], BF16) for i in range(2)]; pt_b = P.bufs(2)
        stg = [sb(f"ustg{i}", [128, NT, 128], BF16) for i in range(2)]; stg_b = P.bufs(2)
        ucB = [sb(f"ucB{i}", [128, S], BF16) for i in range(2)]; ucB_b = P.bufs(2)
        st = {"it": 0}

        def proj(cc):
            i = cc % 2
            P.dma("pool", wi[i][:], wv[:, :, cc * 128:(cc + 1) * 128], writes=[wi_b[i]])
            for tq in range(8):
                k = st["it"] % 3
                st["it"] += 1
                for kc in range(8):
                    P.op("pe", lambda e, k=k, kc=kc, tq=tq: e.matmul(p_u[k][:], lhsT=wi[i][:, kc, :], rhs=hT[:, kc, tq * 512:(tq + 1) * 512], start=(kc == 0), stop=(kc == 7)),
                         [wi_b[i], hT_b[kc]], [pu_b[k]])
                P.op("act", lambda e, k=k, tq=tq: e.activation(out=urow[i][:, 1 + tq * 512:1 + (tq + 1) * 512], in_=p_u[k][:], func=AF.Identity, bias=bin_[:, cc:cc + 1], scale=1.0),
                     [pu_b[k], par_b], [ur_b[i]])
            P.op("dve", lambda e: e.tensor_scalar(out=ucT[:], in0=urow[i][:, 1:S + 1], scalar1=cw[:, 1, cc:cc + 1], scalar2=cbv[:, cc:cc + 1], op0=ALU.mult, op1=ALU.add),
                 [ur_b[i], par_b], [uc_b])
            P.op("dve", lambda e: e.scalar_tensor_tensor(out=ucT[:], in0=urow[i][:, 0:S], scalar=cw[:, 0, cc:cc + 1], in1=ucT[:], op0=ALU.mult, op1=ALU.add),
                 [ur_b[i], par_b, uc_b], [uc_b])
            P.op("dve", lambda e: e.scalar_tensor_tensor(out=ucB[i][:], in0=urow[i][:, 2:S + 2], scalar=cw[:, 2, cc:cc + 1], in1=ucT[:], op0=ALU.mult, op1=ALU.add),
                 [ur_b[i], par_b, uc_b], [ucB_b[i]])

        def back(cc):
            i = cc % 2
            for t4 in range(8):
                k = t4 % 2
                for j in range(4):
                    tt = t4 * 4 + j
                    P.op("pe", lambda e, k=k, j=j, tt=tt: e.transpose(p_t[k][:, j * 128:(j + 1) * 128], ucB[i][:, tt * 128:(tt + 1) * 128], K.identb[:]), [ucB_b[i]], [pt_b[k]])
                P.op("act", lambda e, k=k, t4=t4: e.copy(out=stg[i][:, t4 * 4:(t4 + 1) * 4, :], in_=p_t[k][:, 0:512].rearrange("p (j c) -> p j c", j=4)), [pt_b[k]], [stg_b[i]])
            P.dma("act", K.UC[:, cc * 128:(cc + 1) * 128].rearrange("(tt p) c -> p tt c", p=128), stg[i][:], reads=[stg_b[i]])

        proj(0)
        for cc in range(24):
            if cc + 1 < 24:
                proj(cc + 1)
            back(cc)
    run_phase(K, body)


def s1_state(P, sb, ps, K, tag):
    st = {"it": 0}
    st["F1"] = sb(tag + "F1", [128, 2, 128], BF16); st["F1_b"] = P.buf()
    P.dma("sp", st["F1"][:], K.I["F1"], writes=[st["F1_b"]])
    st["p"] = [ps(f"{tag}p{i}", [128, 512], F32) for i in range(2)]; st["p_b"] = P.bufs(2)
    return st


def phase_hyena_filter(K):
    I = K.I

    def body(P, sb, ps):
        zT = sb("zT", [33, S], F32); z_b = P.buf()
        P.dma("sp", zT[:], I["zT"], writes=[z_b])
        w1 = sb("fw1", [33, 64], F32); w2 = sb("fw2", [64, 64], F32); w3 = sb("fw3", [64, 64], F32); bf_ = sb("fbf", [64, 6], F32); w_b = P.buf()
        P.dma("sp", w1[:], I["hy_f_w1"], writes=[w_b])
        P.dma("sp", w2[:], I["hy_f_w2"], writes=[w_b])
        P.dma("sp", w3[:], I["hy_f_w3"], writes=[w_b])
        P.dma("sp", bf_[:], I["hy_f_bf_t"], writes=[w_b])
        fb = sb("fb", [64, 3], F32)
        for l in range(3):
            P.op("dve", lambda e, l=l: e.tensor_tensor(out=fb[:, l:l + 1], in0=bf_[:, 2 * l:2 * l + 1], in1=bf_[:, 2 * l + 1:2 * l + 2], op=ALU.mult), [w_b], [w_b])
        hd = [sb(f"hd{i}", [64, S], F32) for i in range(2)]; hd_b = P.bufs(2)
        hdb = sb("hdb", [64, S], BF16); hdb_b = P.buf()
        ti = sb("fti", [64, 512], I32); tf = sb("ftf", [64, 512], F32); tmp_b = P.buf()
        p_h = ps("p_h", [128, 512], F32); ph_b = P.buf()
        srcs = [(zT, z_b, w1, 33), (hd[0], hd_b[0], w2, 64), (hd[1], hd_b[1], w3, 64)]
        for l in range(3):
            src, src_b, wl, kdim = srcs[l]
            dst, dst_b = (hd[l % 2], hd_b[l % 2])
            for ch in range(8):
                sl = slice(ch * 512, (ch + 1) * 512)
                P.op("pe", lambda e, src=src, wl=wl, kdim=kdim, sl=sl: e.matmul(p_h[0:64, :], lhsT=wl[0:kdim, :], rhs=src[0:kdim, sl], start=True, stop=True), [src_b, w_b], [ph_b])
                P.op("dve", lambda e, dst=dst, sl=sl, l=l: e.tensor_scalar(out=dst[:, sl], in0=p_h[0:64, :], scalar1=bf_[:, 2 * l + 1:2 * l + 2], scalar2=fb[:, l:l + 1], op0=ALU.mult, op1=ALU.add),
                     [ph_b, w_b], [dst_b])
                emit_sincos_reduce(P, dst[:, sl], ti[:], tf[:], [dst_b, tmp_b], math.pi + 64 * math.pi)
            P.op("act", lambda e, dst=dst: e.activation(out=dst[:], in_=dst[:], func=AF.Sin), [dst_b], [dst_b])
        P.op("dve", lambda e: e.tensor_copy(out=hdb[:], in_=hd[0][:]), [hd_b[0]], [hdb_b])
        if "hdn" in K.dbg:
            P.dma("sp", K.dbg["hdn"], hd[0][:], reads=[hd_b[0]])
        w4 = sb("fw4", [64, 4 * D], BF16); w4_b = P.buf()
        P.dma("pool", w4[:], I["hy_f_w4"], writes=[w4_b])
        dl, dl_b = load_bcast(P, sb, "deltas", I["deltas"], D)
        ntl = sb("ntl", [128, 32], F32); ntl_b = P.buf()
        P.dma("sp", ntl[:], I["ntl"], writes=[ntl_b])
        msk = sb("msk", [128, 4, 128], F32); msk_b = P.buf()
        P.dma("sp", msk[:], I["mask4"], writes=[msk_b])
        dec = [sb(f"dec{i}", [128, D], F32) for i in range(2)]; dec_b = P.bufs(2)
        asum = sb("asum", [128, 2 * D], F32); as_b = P.buf()
        fa = [sb(f"fa{i}", [128, 2 * D], BF16) for i in range(2)]; fa_b = P.bufs(2)
        absf = [sb("absf0", [128, 2 * D], F32)] * 2; absf_b = [P.buf()] * 2
        p_f = [ps(f"p_f{i}", [128, 512], F32) for i in range(2)]; pf_b = P.bufs(2)
        hdv = hdb[:].rearrange("w (p a) -> w a p", a=32)
        st = s1_state(P, sb, ps, K, "fs1")
        fstg = [sb(f"fstg{i}", [128, 2, 2, D], BF16) for i in range(2)]; fstg_b = P.bufs(2)
        ad_b = P.bufs(32)
        rn2 = sb("rn2", [128, 2, 256], F32); rn2_b = P.buf()
        gbt = [sb(f"gbt{i}", [128, 4, 4, 128], BF16) for i in range(2)]; gbt_b = P.bufs(2)
        ain = [sb(f"ain{i}", [128, 2, 2, 4, 256], BF16) for i in range(2)]; ain_b = [P.bufs(4) for _ in range(2)]
        ains = [sb(f"ains{i}", [128, 2, 2, 4, 256], BF16) for i in range(2)]; ains_b = [P.bufs(2) for _ in range(2)]
        p_k = [ps(f"p_k{i}", [128, 2, 256], F32) for i in range(2)]; pk_b = P.bufs(2)
        p_n = ps("p_n", [128, 2, 256], F32); pn_b = P.buf()
        kst = [sb(f"kst{i}", [128, 4, 2, 256], BF16) for i in range(2)]; kst_b = P.bufs(2)
        it = 0
        for o in range(2):
            P.op("pool", lambda e: e.memset(asum[:], 0.0), [], [as_b])
            sp4 = st["p"] + [p_k[0][:].rearrange("p r g -> p (r g)"), p_k[1][:].rearrange("p r g -> p (r g)")]
            sp4 = [t if not hasattr(t, "ap") or True else t for t in sp4]
            sp4_b = st["p_b"] + pk_b

            def gen(a):
                nonlocal it
                di = a % 2
                P.op("act", lambda e: e.activation(out=dec[di][:], in_=dl[:], func=AF.Exp, scale=ntl[:, a:a + 1]), [dl_b, ntl_b], [dec_b[di]])
                for dr in range(2):
                    for chh in range(2):
                        col = (o * 2 + dr) * D + chh * 512
                        k = it % 2
                        it += 1
                        P.op("pe", lambda e, k=k, col=col: e.matmul(p_f[k][:], lhsT=hdv[:, a, :], rhs=w4[:, col:col + 512], start=True, stop=True), [hdb_b, w4_b], [pf_b[k]])
                        lc = dr * D + chh * 512
                        P.op("dve", lambda e, k=k, lc=lc, chh=chh: e.tensor_tensor(out=fa[di][:, lc:lc + 512], in0=p_f[k][:], in1=dec[di][:, chh * 512:(chh + 1) * 512], op=ALU.mult),
                             [pf_b[k], dec_b[di]], [fa_b[di]])
                P.op("act", lambda e: e.activation(out=absf[di][:], in_=fa[di][:], func=AF.Abs), [fa_b[di]], [absf_b[di]])
                P.op("pool", lambda e: e.tensor_tensor(out=asum[:], in0=asum[:], in1=absf[di][:], op=ALU.add), [absf_b[di], as_b], [as_b])
                if a == 0:
                    P.op("pool", lambda e: e.memset(fa[di][0:1, D:2 * D], 0.0), [as_b, fa_b[di]], [fa_b[di]])

            def s1(a):
                di = a % 2
                g = a % 2
                for dr in range(2):
                    for chh in range(2):
                        lc = dr * D + chh * 512
                        for ri in range(2):
                            k = st["it"] % 4
                            st["it"] += 1
                            pk = sp4[k]
                            pkv = pk if k >= 2 else pk[:]
                            P.op("pe", lambda e, pkv=pkv, ri=ri, lc=lc: e.matmul(pkv, lhsT=st["F1"][:, ri, :], rhs=fa[di][:, lc:lc + 512], start=True, stop=True),
                                 [fa_b[di], st["F1_b"]], [sp4_b[k]])
                            if ri == 0:
                                P.op("act", lambda e, pkv=pkv, dr=dr, ri=ri, chh=chh: e.copy(out=fstg[g][:, dr, ri, chh * 512:(chh + 1) * 512], in_=pkv), [sp4_b[k]], [fstg_b[g]])
                            else:
                                P.op("dve", lambda e, pkv=pkv, dr=dr, ri=ri, chh=chh: e.tensor_copy(out=fstg[g][:, dr, ri, chh * 512:(chh + 1) * 512], in_=pkv), [sp4_b[k]], [fstg_b[g]])
                P.dma("sp", K.Ad[:, :, :, a * D:(a + 1) * D].rearrange("s r k n -> k s r n"), fstg[g][:], reads=[fstg_b[g]], writes=[ad_b[a]])

            gen(0)
            for a in range(32):
                if a + 1 < 32:
                    gen(a + 1)
                s1(a)
            for dr in range(2):
                for c4 in range(4):
                    P.op("pe", lambda e, dr=dr, c4=c4: e.matmul(p_n[:, dr, :], lhsT=msk[:, c4, :], rhs=asum[:, dr * D + c4 * 256:dr * D + (c4 + 1) * 256], start=(c4 == 0), stop=(c4 == 3)),
                         [msk_b, as_b], [pn_b])
            P.op("dve", lambda e: e.tensor_scalar(out=rn2[:], in0=p_n[:], scalar1=EPS, scalar2=None, op0=ALU.add), [pn_b], [rn2_b])
            P.op("dve", lambda e: e.reciprocal(out=rn2[:], in_=rn2[:]), [rn2_b], [rn2_b])
            if "rn2" in K.dbg and o == 0:
                P.dma("sp", K.dbg["rn2"], rn2[:], reads=[rn2_b])
            re_terms = [(0, 0, 0), (2, 0, 1), (0, 1, 0), (2, 1, 1)]
            im_terms = [(1, 0, 0), (0, 0, 1), (2, 1, 0), (3, 1, 1)]
            KB = 4
            for grp in range(128 // KB):
                gi = grp % 2
                k0 = grp * KB
                P.dma("sp", gbt[gi][:], K.I["GB"][:, k0:k0 + KB, 0:4, :], writes=[gbt_b[gi]])
                for s_ in range(2):
                    for r_ in range(2):
                        P.dma("sp", ain[gi][:, s_, r_, :, :], K.Ad[s_, r_, k0:k0 + KB, :].rearrange("k (q g) -> q k g", g=256), reads=ad_b, writes=[ain_b[gi][s_ * 2 + r_]])
                for s_ in range(2):
                    P.op("dve", lambda e, gi=gi, s_=s_: e.tensor_tensor(out=ains[gi][:, s_].rearrange("p r k g -> p (r k) g"), in0=ain[gi][:, s_].rearrange("p r k g -> p (r k) g"),
                                                                       in1=rn2[:, s_:s_ + 1, :].to_broadcast([128, 2 * KB, 256]), op=ALU.mult),
                         ain_b[gi][s_ * 2:s_ * 2 + 2] + [rn2_b], [ains_b[gi][s_]])
                for kl in range(KB):
                    k1 = k0 + kl
                    kk = k1 % 2
                    for ri, terms in ((0, re_terms), (1, im_terms)):
                        for n, (mi, s_, r_) in enumerate(terms):
                            P.op("pe", lambda e, kk=kk, ri=ri, mi=mi, s_=s_, r_=r_, gi=gi, n=n, kl=kl: e.matmul(p_k[kk][:, ri, :], lhsT=gbt[gi][:, kl, mi, :], rhs=ains[gi][:, s_, r_, kl, :], start=(n == 0), stop=(n == 3)),
                                 [gbt_b[gi], ains_b[gi][s_]], [pk_b[kk]])
                    ks = (k1 // 4) % 2
                    P.op("act", lambda e, kk=kk, ks=ks, k1=k1: e.copy(out=kst[ks][:, k1 % 4, :, :], in_=p_k[kk][:]), [pk_b[kk]], [kst_b[ks]])
                    if k1 % 4 == 3:
                        P.dma("sp", K.Kd[o, :, k1 - 3:k1 + 1], kst[ks][:], reads=[kst_b[ks]])
    run_phase(K, body)


def phase_hyena_conv(K):
    I = K.I

    def body(P, sb, ps):
        st = s1_state(P, sb, ps, K, "ds1")
        F1T = sb("F1T", [128, 2, 128], BF16); f1t_b = P.buf()
        P.dma("sp", F1T[:], I["F1T"], writes=[f1t_b])
        fbias, fbias_b = None, None
        fbias = sb("fbias", [128, 2, D], F32); fbias_b = P.buf()
        for o in range(2):
            P.dma("sp", fbias[:, o, :], I["hy_filt_bias"][o:o + 1, :].partition_broadcast(128), writes=[fbias_b])
        xin = [sb(f"xin{i}", [128, 4, D], BF16) for i in range(2)]; xin_b = P.bufs(2)
        stg = [sb(f"s1stg{i}", [128, 2, 2048], BF16) for i in range(2)]; stg_b = P.bufs(2)
        ad_b = P.bufs(16)
        bd_b = P.bufs(16)
        z0_b = P.bufs(8)
        gb = [sb(f"gb{i}", [128, 4, 7, 128], BF16) for i in range(3)]; gb_b = P.bufs(3)
        ain = [sb(f"cain{i}", [128, 2, 4, 256], BF16) for i in range(3)]; ain_b = [P.bufs(2) for _ in range(3)]
        kk_ = [sb(f"ckk{i}", [128, 4, 2, 256], BF16) for i in range(3)]; kk_b = P.bufs(3)
        ksw = [sb(f"cksw{i}", [128, 4, 2, 256], BF16) for i in range(3)]; ksw_b = [P.bufs(2) for _ in range(3)]
        p_y = [ps(f"p_y{i}", [128, 2, 256], F32) for i in range(3)]; py_b = P.bufs(3)
        p_b = [ps(f"p_b{i}", [128, 2, 256], F32) for i in range(3)]; pb_b = P.bufs(3)
        m1 = [sb(f"m1{i}", [128, 2, 256], BF16) for i in range(2)]; m1_b = P.bufs(2)
        m2 = [sb(f"m2{i}", [128, 2, 256], BF16) for i in range(2)]; m2_b = P.bufs(2)
        pp = [sb(f"pp{i}", [128, 2, 256], BF16) for i in range(2)]; pp_b = P.bufs(2)
        bst = [sb(f"bst{i}", [128, 8, 2, 256], BF16) for i in range(2)]; bst_b = P.bufs(2)
        p_i = st["p"]; pi_b = st["p_b"]
        bin_ = stg; bin_b = stg_b
        gt = [sb(f"gt{i}", [128, 2, D], BF16) for i in range(2)]; gt_b = P.bufs(2)
        zi = [sb(f"zi{i}", [128, 2, D], F32) for i in range(2)]; zi_b = P.bufs(2)
        zib = [sb(f"zib{i}", [128, 2, D], BF16) for i in range(2)]; zib_b = P.bufs(2)
        t1 = [sb(f"t1{i}", [128, 512], F32) for i in range(2)]; t1_b = P.bufs(2)
        zo = [sb(f"zo{i}", [128, 2, D], BF16) for i in range(2)]; zo_b = P.bufs(2)
        UCv = K.UC.rearrange("(p a) c -> p a c", a=32)
        Zv = [K.Z[o].rearrange("(p a) c -> p a c", a=32) for o in range(2)]
        for o in range(2):
            for grp in range(8):
                xi = grp % 2
                if o == 0:
                    P.dma("pool", xin[xi][:], UCv[:, grp * 4:(grp + 1) * 4, 2 * D:3 * D], writes=[xin_b[xi]])
                else:
                    P.dma("sp", xin[xi][:], Zv[0][:, grp * 4:(grp + 1) * 4, :], reads=z0_b, writes=[xin_b[xi]])
                for j in range(8):
                    nch = grp * 8 + j
                    rhs = xin[xi][:, j // 2, (j % 2) * 512:(j % 2 + 1) * 512]
                    g = (nch // 4) % 2
                    for ri in range(2):
                        k = st["it"] % 2
                        st["it"] += 1
                        P.op("pe", lambda e, k=k, ri=ri, rhs=rhs: e.matmul(st["p"][k][:], lhsT=st["F1"][:, ri, :], rhs=rhs, start=True, stop=True), [xin_b[xi], st["F1_b"]], [st["p_b"][k]])
                        if ri == 0:
                            P.op("act", lambda e, k=k, g=g, ri=ri, nch=nch: e.copy(out=stg[g][:, ri, (nch % 4) * 512:(nch % 4 + 1) * 512], in_=st["p"][k][:]), [st["p_b"][k]], [stg_b[g]])
                        else:
                            P.op("dve", lambda e, k=k, g=g, ri=ri, nch=nch: e.tensor_copy(out=stg[g][:, ri, (nch % 4) * 512:(nch % 4 + 1) * 512], in_=st["p"][k][:]), [st["p_b"][k]], [stg_b[g]])
                    if nch % 4 == 3:
                        c0 = (nch // 4) * 2048
                        P.dma("sp", K.Ad[0, :, :, c0:c0 + 2048].rearrange("r k n -> k r n"), stg[g][:], reads=[stg_b[g]], writes=[ad_b[nch // 4]])
            KB = 4

            def load_grp(grp):
                bi = grp % 3
                k0 = grp * KB
                P.dma("sp", gb[bi][:], I["GB"][:, k0:k0 + KB], writes=[gb_b[bi]])
                for r_ in range(2):
                    P.dma("sp", ain[bi][:, r_, :, :], K.Ad[0, r_, k0:k0 + KB, :].rearrange("k (q g) -> q k g", g=256), reads=ad_b, writes=[ain_b[bi][r_]])
                P.dma("sp", kk_[bi][:], K.Kd[o, :, k0:k0 + KB], writes=[kk_b[bi]])

            def emit_s2(k1):
                bi = (k1 // KB) % 3
                kl = k1 % KB
                yi = k1 % 3
                if kl == 0:
                    load_grp(k1 // KB)
                for ri, terms in ((0, [(0, 0), (2, 1)]), (1, [(1, 0), (0, 1)])):
                    for n, (mi, r_) in enumerate(terms):
                        P.op("pe", lambda e, ri=ri, mi=mi, r_=r_, n=n: e.matmul(p_y[yi][:, ri, :], lhsT=gb[bi][:, kl, mi, :], rhs=ain[bi][:, r_, kl, :], start=(n == 0), stop=(n == 1)),
                             [gb_b[bi], ain_b[bi][r_]], [py_b[yi]])

            def emit_rest(k1):
                bi = (k1 // KB) % 3
                kl = k1 % KB
                yi = k1 % 3
                gi = k1 % 2
                P.op("dve", lambda e: e.tensor_tensor(out=m1[gi][:], in0=p_y[yi][:], in1=kk_[bi][:, kl], op=ALU.mult), [py_b[yi], kk_b[bi]], [m1_b[gi]])
                P.op("dve", lambda e: e.tensor_tensor(out=m2[gi][:, 0, :], in0=p_y[yi][:, 0, :], in1=kk_[bi][:, kl, 1, :], op=ALU.mult), [py_b[yi], kk_b[bi]], [m2_b[gi]])
                P.op("dve", lambda e: e.tensor_tensor(out=m2[gi][:, 1, :], in0=p_y[yi][:, 1, :], in1=kk_[bi][:, kl, 0, :], op=ALU.mult), [py_b[yi], kk_b[bi]], [m2_b[gi]])
                P.op("pool", lambda e: e.tensor_tensor(out=pp[gi][:, 0, :], in0=m1[gi][:, 0, :], in1=m1[gi][:, 1, :], op=ALU.subtract), [m1_b[gi]], [pp_b[gi]])
                P.op("pool", lambda e: e.tensor_tensor(out=pp[gi][:, 1, :], in0=m2[gi][:, 0, :], in1=m2[gi][:, 1, :], op=ALU.add), [m2_b[gi]], [pp_b[gi]])
                for ri, terms in ((0, [(4, 0), (5, 1)]), (1, [(4, 1), (6, 0)])):
                    for n, (mi, r_) in enumerate(terms):
                        P.op("pe", lambda e, ri=ri, mi=mi, r_=r_, n=n: e.matmul(p_b[yi][:, ri, :], lhsT=gb[bi][:, kl, mi, :], rhs=pp[gi][:, r_, :], start=(n == 0), stop=(n == 1)),
                             [gb_b[bi], pp_b[gi]], [pb_b[yi]])
                ks = (k1 // 8) % 2
                P.op("act", lambda e: e.copy(out=bst[ks][:, k1 % 8, :, :], in_=p_b[yi][:]), [pb_b[yi]], [bst_b[ks]])
                if k1 % 8 == 7:
                    P.dma("sp", K.Bd[0, k1 - 7:k1 + 1, :].rearrange("k (q g) -> q k g", g=256), bst[ks][:, :, 0, :], reads=[bst_b[ks]], writes=[bd_b[k1 // 8]])
                    P.dma("sp", K.Bd[1, k1 - 7:k1 + 1, :].rearrange("k (q g) -> q k g", g=256), bst[ks][:, :, 1, :], reads=[bst_b[ks]], writes=[bd_b[k1 // 8]])

            LOOK = 2
            for k1 in range(LOOK):
                emit_s2(k1)
            for k1 in range(128):
                if k1 + LOOK < 128:
                    emit_s2(k1 + LOOK)
                emit_rest(k1)
            for grp in range(16):
                bi = grp % 2
                P.dma("sp", bin_[bi][:], K.Bd[:, :, grp * 2048:(grp + 1) * 2048].rearrange("r k n -> k r n"), reads=bd_b, writes=[bin_b[bi]])
                P.dma("pool", gt[bi][:], UCv[:, grp * 2:(grp + 1) * 2, o * D:(o + 1) * D], writes=[gt_b[bi]])
                if o == 0:
                    P.dma("sp", zib[bi][:], UCv[:, grp * 2:(grp + 1) * 2, 2 * D:3 * D], writes=[zib_b[bi]])
                else:
                    P.dma("sp", zib[bi][:], Zv[0][:, grp * 2:(grp + 1) * 2, :], reads=z0_b, writes=[zib_b[bi]])
                P.op("dve", lambda e, bi=bi, o=o: e.tensor_tensor(out=zi[bi][:], in0=zib[bi][:], in1=fbias[:, o:o + 1, :].to_broadcast([128, 2, D]), op=ALU.mult), [zib_b[bi], fbias_b], [zi_b[bi]])
                for j in range(4):
                    k = (grp * 4 + j) % 2
                    for ri in range(2):
                        P.op("pe", lambda e, k=k, ri=ri, bi=bi, j=j: e.matmul(p_i[k][:], lhsT=F1T[:, ri, :], rhs=bin_[bi][:, ri, j * 512:(j + 1) * 512], start=(ri == 0), stop=(ri == 1)),
                             [f1t_b, bin_b[bi]], [pi_b[k]])
                    aa, hh = j // 2, j % 2
                    P.op("dve", lambda e, k=k, bi=bi, aa=aa, hh=hh: e.tensor_tensor(out=t1[k][:], in0=p_i[k][:], in1=zi[bi][:, aa, hh * 512:(hh + 1) * 512], op=ALU.add), [pi_b[k], zi_b[bi]], [t1_b[k]])
                    P.op("dve", lambda e, k=k, bi=bi, aa=aa, hh=hh: e.tensor_tensor(out=zo[bi][:, aa, hh * 512:(hh + 1) * 512], in0=t1[k][:], in1=gt[bi][:, aa, hh * 512:(hh + 1) * 512], op=ALU.mult), [t1_b[k], gt_b[bi]], [zo_b[bi]])
                P.dma("sp", Zv[o][:, grp * 2:(grp + 1) * 2, :], zo[bi][:], reads=[zo_b[bi]], writes=([z0_b[grp // 2]] if o == 0 else []))
    run_phase(K, body)


_PROG = {}


def _layout_inputs(inp, b):
    f = lambda a: np.ascontiguousarray(np.asarray(a, dtype=np.float32))
    m = {}
    m["x"] = f(inp["x"][b])
    m["c_t"] = f(np.asarray(inp["c"][b]).reshape(8, 128).T)
    m["pos_t"] = np.ascontiguousarray(np.asarray(inp["positions"][b]).astype(np.int32).reshape(NT, 128).T)
    for k in ("ada_w", "ada_b", "norm_mix_g", "norm_ffn_g"):
        m[k] = f(inp[k])
    m["mla_w_down"] = f(inp["mla_w_down"][0])
    m["mla_q_a_g"] = f(inp["mla_q_a_g"])
    m["mla_kv_a_g"] = f(inp["mla_kv_a_g"])
    m["mla_w_uq"] = f(inp["mla_w_uq"][0])
    m["mla_w_ukv"] = f(inp["mla_w_ukv"][0])
    m["mla_q_norm_g"] = f(inp["mla_q_norm_g"])
    m["mla_k_norm_g"] = f(inp["mla_k_norm_g"])
    m["mla_w_o"] = f(inp["mla_w_o"][0])
    m["hy_w_in"] = f(inp["hy_w_in"][0])
    m["hy_b_in_t"] = f(np.asarray(inp["hy_b_in"][0]).reshape(24, 128).T)
    m["hy_conv_w_t"] = f(np.asarray(inp["hy_conv_w"][0]).reshape(3, 24, 128).transpose(2, 0, 1))
    m["hy_conv_b_t"] = f(np.asarray(inp["hy_conv_b"][0]).reshape(24, 128).T)
    m["hy_f_w1"] = f(inp["hy_f_w1"][0])
    m["hy_f_w2"] = f(inp["hy_f_w2"][0])
    m["hy_f_w3"] = f(inp["hy_f_w3"][0])
    m["hy_f_bf_t"] = f(np.stack([np.asarray(inp[k][0]) for k in ("hy_f_b1", "hy_f_freq1", "hy_f_b2", "hy_f_freq2", "hy_f_b3", "hy_f_freq3")], axis=1))
    m["hy_f_w4"] = f(inp["hy_f_w4"][0])
    m["hy_filt_bias"] = f(inp["hy_filt_bias"][0])
    m["hy_w_out"] = f(inp["hy_w_out"][0])
    m["moe_wr"] = f(np.concatenate([np.asarray(inp["moe_wg"]), np.asarray(inp["moe_we"])], axis=-1))
    m["moe_br"] = f(np.concatenate([np.asarray(inp["moe_bg"]), np.asarray(inp["moe_be"])], axis=-1))
    m["moe_w_gate"] = f(inp["moe_w_gate"])
    m["moe_w_up"] = f(inp["moe_w_up"])
    m["moe_w_down"] = f(inp["moe_w_down"])
    return m


def kernel(**inputs):
    if "nc" not in _PROG:
        _PROG["nc"] = build_program()
    nc = _PROG["nc"]
    const = host_constants()
    shared = None
    in_maps = []
    for b in range(8):
        m = _layout_inputs(inputs, b)
        if shared is None:
            shared = {k: v for k, v in m.items() if k not in ("x", "c_t", "pos_t")}
        else:
            for k in shared:
                m[k] = shared[k]
        m.update(const)
        in_maps.append(m)
    res = run_bass_kernel_spmd(nc, in_maps, core_ids=list(range(8)))
    return np.stack([np.asarray(r["out"], dtype=np.float32) for r in res.results], axis=0)
```

```python
import math
from contextlib import ExitStack
import numpy as np
import ml_dtypes
import concourse.bass as bass
import concourse.mybir as mybir
from concourse.bass_utils import run_bass_kernel_spmd

F32 = mybir.dt.float32
BF16 = mybir.dt.bfloat16
I32 = mybir.dt.int32
AF = mybir.ActivationFunctionType
ALU = mybir.AluOpType
AX = mybir.AxisListType

D = 1024
S = 4096
NT = S // 128
EPS = 1e-6
NE = 16
DE = 256
NFFT = 8192

ENGS = ("pe", "act", "dve", "pool", "sp")
DMA_RING = {"sp": 12, "act": 4, "pool": 12}
SEM_CAP = 30000


_UID = [0]


def _uid():
    _UID[0] += 1
    return _UID[0]


class Buf:
    __slots__ = ("last_w", "readers", "excl")

    def __init__(self):
        self.last_w = None
        self.readers = []
        self.excl = True


class Op:
    __slots__ = ("eng", "fn", "is_dma", "lidx", "deps", "signal", "sig_idx", "waits", "dma_slot",
                 "dma_val", "ring_wait")

    def __init__(self, eng, fn, is_dma):
        self.eng = eng
        self.fn = fn
        self.is_dma = is_dma
        self.deps = []
        self.signal = False
        self.sig_idx = None
        self.waits = []
        self.dma_slot = None
        self.dma_val = None
        self.ring_wait = None


class Prog:
    def __init__(self):
        self.ops = []
        self.per_eng = {e: [] for e in ENGS}
        self.dma_count = {e: 0 for e in DMA_RING}

    def buf(self):
        return Buf()

    def bufs(self, n):
        return [Buf() for _ in range(n)]

    def op(self, eng, fn, reads=(), writes=(), dma=False):
        o = Op(eng, fn, dma)
        o.lidx = len(self.per_eng[eng])
        deps = set()
        for b in reads:
            if b.last_w is not None:
                deps.add(b.last_w)
            if b.excl:
                for r in b.readers:
                    if r.eng != eng:
                        deps.add(r)
        for b in writes:
            if b.last_w is not None:
                deps.add(b.last_w)
            for r in b.readers:
                deps.add(r)
        deps.discard(o)
        o.deps = list(deps)
        for b in reads:
            b.readers.append(o)
        for b in writes:
            b.last_w = o
            b.readers = []
        if dma:
            n = self.dma_count[eng]
            self.dma_count[eng] = n + 1
            R = DMA_RING[eng]
            o.dma_slot = (eng, n % R)
            o.dma_val = 16 * (n // R + 1)
            if n >= R:
                o.ring_wait = 16 * (n // R)
        self.ops.append(o)
        self.per_eng[eng].append(o)
        return o

    def dma(self, q, out, in_, reads=(), writes=(), **kw):
        if q != "act":
            is_store = "DRAM" in str(out.space)
            cast = out.dtype != in_.dtype
            q = "pool" if (is_store or cast) else "sp"
        return self.op(q, lambda e: e.dma_start(out=out, in_=in_, **kw), reads, writes, dma=True)

    def resolve(self):
        known = {c: {p: -1 for p in ENGS} for c in ENGS}
        known_dma = {c: {} for c in ENGS}
        for o in self.ops:
            c = o.eng
            latest = {}
            latest_dma = {}
            for d in o.deps:
                if d.is_dma:
                    if d.dma_slot not in latest_dma or d.dma_val > latest_dma[d.dma_slot].dma_val:
                        latest_dma[d.dma_slot] = d
                else:
                    if d.eng not in latest or d.lidx > latest[d.eng].lidx:
                        latest[d.eng] = d
            for slot in sorted(latest_dma):
                d = latest_dma[slot]
                k = known_dma[c].get(d.dma_slot, 0)
                if d.dma_val > k:
                    known_dma[c][d.dma_slot] = d.dma_val
                    o.waits.append(("dma", d.dma_slot, d.dma_val))
            for p in ENGS:
                if p not in latest:
                    continue
                d = latest[p]
                if p == "pe" and c == "pe":
                    continue
                if d.lidx > known[c][p]:
                    known[c][p] = d.lidx
                    d.signal = True
                    o.waits.append(("cmp", d))
            if o.is_dma and o.ring_wait is not None:
                k = known_dma[c].get(o.dma_slot, 0)
                if o.ring_wait > k:
                    known_dma[c][o.dma_slot] = o.ring_wait
                    o.waits.append(("dma", o.dma_slot, o.ring_wait))
        sig_count = {}
        for e in ENGS:
            n = 0
            for o in self.per_eng[e]:
                if o.signal and not o.is_dma:
                    o.sig_idx = n
                    n += 1
            sig_count[e] = n
        return sig_count

    def emit(self, nc):
        sig_count = self.resolve()
        handles = []

        def newsem(pfx):
            h = nc.alloc_semaphore(name=f"{pfx}{_uid()}")
            handles.append(h)
            return h
        with ExitStack() as es:
            csem = {}
            for e in ENGS:
                ns = max(1, (sig_count[e] + SEM_CAP - 1) // SEM_CAP)
                csem[e] = [newsem("cs") for i in range(ns)]
            dsem = {}
            for q, R in DMA_RING.items():
                for i in range(min(R, self.dma_count[q])):
                    dsem[(q, i)] = newsem("ds")
            block = es.enter_context(nc.Block())

            def run(engname):
                def body(eng):
                    for o in self.per_eng[engname]:
                        for w in o.waits:
                            if w[0] == "dma":
                                eng.wait_ge(dsem[w[1]], w[2])
                            else:
                                d = w[1]
                                eng.wait_ge(csem[d.eng][d.sig_idx // SEM_CAP], d.sig_idx % SEM_CAP + 1)
                        ins = o.fn(eng)
                        if o.is_dma:
                            ins.then_inc(dsem[o.dma_slot], 16)
                        elif o.signal:
                            ins.then_inc(csem[engname][o.sig_idx // SEM_CAP], 1)
                    if engname in DMA_RING:
                        n = self.dma_count[engname]
                        R = DMA_RING[engname]
                        for s in range(min(R, n)):
                            cnt = (n - 1 - s) // R + 1
                            eng.wait_ge(dsem[(engname, s)], 16 * cnt)
                return body

            block.tensor(run("pe"))
            block.scalar(run("act"))
            block.vector(run("dve"))
            block.gpsimd(run("pool"))
            block.sync(run("sp"))
        nc.clear_and_free_semaphores(handles)


_CONST = {}


def _bf(a):
    return np.ascontiguousarray(a.astype(np.float32)).astype(ml_dtypes.bfloat16)


def host_constants():
    if _CONST:
        return _CONST
    c = {}
    c["ident32"] = np.eye(128, dtype=np.float32)
    c["identb"] = _bf(np.eye(128))
    c["inv_freq"] = (1.0 / (10000.0 ** (np.arange(0, 64, 2, dtype=np.float32) / 64))).astype(np.float32)[None, :]
    sel = np.zeros((16, 16, 128), np.float32)
    for e in range(16):
        sel[e, e, :] = 1.0
    c["sel"] = _bf(sel.reshape(16, 16 * 128))
    L = S
    t = np.linspace(0.0, 1.0, L, dtype=np.float32)[:, None]
    w = (2.0 * math.pi * np.arange(L, dtype=np.float32)[:, None] / L).astype(np.float32)
    fr = np.linspace(1e-4, 15, 16, dtype=np.float32)[None, :]
    z = np.concatenate([t, np.cos(fr * w), -np.sin(fr * w)], axis=-1).astype(np.float32)
    c["zT"] = np.ascontiguousarray(z.T)
    tt = (32 * np.arange(128)[:, None] + np.arange(32)[None, :])
    c["ntl"] = (-t[:, 0][tt]).astype(np.float32)
    deltas = np.abs(np.linspace(math.log(0.3) / 1e-2, math.log(1.5) / 1e-2, D, dtype=np.float32))
    c["deltas"] = deltas.astype(np.float32)[None, :]
    p = np.arange(128)[:, None].astype(np.float64)
    k1 = np.arange(128)[None, :].astype(np.float64)
    F1 = np.exp(-2j * np.pi * p * (k1 + 0.5) / 256.0)
    c["F1"] = _bf(np.stack([F1.real, F1.imag], axis=1))
    sc = 2.0 / NFFT
    c["F1T"] = _bf(np.stack([F1.real.T * sc, F1.imag.T * sc], axis=1))
    a = np.arange(32)[:, None].astype(np.float64)
    k2 = np.arange(32)[None, :].astype(np.float64)
    GB = np.zeros((128, 128, 7, 128), np.float32)
    for kk in range(128):
        G = np.exp(-2j * np.pi * a * (kk + 256.0 * k2 + 0.5) / NFFT)
        mats = [G.real, G.imag, -G.imag, -G.real, G.real.T, G.imag.T, -G.imag.T]
        for mi, M in enumerate(mats):
            for c4 in range(4):
                GB[c4::4, kk, mi, c4::4] = M
    c["GB"] = _bf(GB)
    m4 = np.zeros((128, 4, 128), np.float32)
    for c4 in range(4):
        m4[:, c4, c4::4] = 1.0
    c["mask4"] = m4
    _CONST.update(c)
    return _CONST


class Ctx:
    pass


def build_program(debug=None, stop=None, dump=()):
    nc = bass.Bass("TRN2", target_bir_lowering=False)
    K = Ctx()
    K.nc = nc

    def din(name, shape, dt=F32):
        return nc.dram_tensor(name, list(shape), dt, kind="ExternalInput").ap()

    def dscr(name, shape, dt=F32):
        return nc.dram_tensor(name, list(shape), dt, kind="Internal").ap()

    I = {}
    I["x"] = din("x", [S, D])
    I["c_t"] = din("c_t", [128, 8])
    I["pos_t"] = din("pos_t", [128, NT], I32)
    I["ada_w"] = din("ada_w", [2, D, 6 * D])
    I["ada_b"] = din("ada_b", [2, 6 * D])
    I["norm_mix_g"] = din("norm_mix_g", [2, D])
    I["norm_ffn_g"] = din("norm_ffn_g", [2, D])
    I["mla_w_down"] = din("mla_w_down", [D, 448])
    I["mla_q_a_g"] = din("mla_q_a_g", [1, 256])
    I["mla_kv_a_g"] = din("mla_kv_a_g", [1, 128])
    I["mla_w_uq"] = din("mla_w_uq", [256, 1536])
    I["mla_w_ukv"] = din("mla_w_ukv", [128, 2048])
    I["mla_q_norm_g"] = din("mla_q_norm_g", [1, 192])
    I["mla_k_norm_g"] = din("mla_k_norm_g", [1, 192])
    I["mla_w_o"] = din("mla_w_o", [D, D])
    I["hy_w_in"] = din("hy_w_in", [D, 3 * D])
    I["hy_b_in_t"] = din("hy_b_in_t", [128, 24])
    I["hy_conv_w_t"] = din("hy_conv_w_t", [128, 3, 24])
    I["hy_conv_b_t"] = din("hy_conv_b_t", [128, 24])
    I["hy_f_w1"] = din("hy_f_w1", [33, 64])
    I["hy_f_w2"] = din("hy_f_w2", [64, 64])
    I["hy_f_w3"] = din("hy_f_w3", [64, 64])
    I["hy_f_bf_t"] = din("hy_f_bf_t", [64, 6])
    I["hy_f_w4"] = din("hy_f_w4", [64, 4 * D])
    I["hy_filt_bias"] = din("hy_filt_bias", [2, D])
    I["hy_w_out"] = din("hy_w_out", [D, D])
    I["moe_wr"] = din("moe_wr", [2, D, 20])
    I["moe_br"] = din("moe_br", [2, 20])
    I["moe_w_gate"] = din("moe_w_gate", [2, NE, D, DE])
    I["moe_w_up"] = din("moe_w_up", [2, NE, D, DE])
    I["moe_w_down"] = din("moe_w_down", [2, NE, DE, D])
    I["ident32"] = din("ident32", [128, 128])
    I["identb"] = din("identb", [128, 128], BF16)
    I["inv_freq"] = din("inv_freq", [1, 32])
    I["sel"] = din("sel", [16, 16 * 128], BF16)
    I["zT"] = din("zT", [33, S])
    I["ntl"] = din("ntl", [128, 32])
    I["deltas"] = din("deltas", [1, D])
    I["F1"] = din("F1", [128, 2, 128], BF16)
    I["F1T"] = din("F1T", [128, 2, 128], BF16)
    I["GB"] = din("GB", [128, 128, 7, 128], BF16)
    I["mask4"] = din("mask4", [128, 4, 128])
    K.I = I
    out = nc.dram_tensor("out", [S, D], F32, kind="ExternalOutput").ap()
    K.out = out

    K.xs = dscr("xs", [S, D])
    K.QN = dscr("QN", [8, 128, S], BF16)
    K.KN = dscr("KN", [8, 128, S], BF16)
    K.QP = dscr("QP", [4, 128, S], BF16)
    K.KP = dscr("KP", [64, S], BF16)
    K.V = dscr("V", [8, S, 130], BF16)
    K.OT = dscr("OT", [8, 128, S], BF16)
    K.AT = dscr("AT", [NE, 2, 128, S], BF16)
    K.UC = dscr("UC", [S, 3 * D], BF16)
    K.Ad = dscr("Ad", [2, 2, 128, 32 * D], BF16)
    K.Bd = dscr("Bd", [2, 128, 32 * D], BF16)
    K.Kd = dscr("Kd", [2, 128, 128, 2, 256], BF16)
    K.Z = dscr("Z", [2, S, D], BF16)
    K.HT = dscr("HT", [128, 8, S], BF16)
    K.CT = dscr("CT", [16, S], BF16)
    K.dbg = {}
    if debug:
        for name, (shape, dt) in debug.items():
            K.dbg[name] = nc.dram_tensor("dbg_" + name, list(shape), dt, kind="ExternalOutput").ap()

    with ExitStack() as pes:
        def psb(name, shape, dt):
            return pes.enter_context(nc.sbuf_tensor(name, list(shape), dt))
        K.ident32 = psb("ident32_sb", [128, 128], F32)
        K.identb = psb("identb_sb", [128, 128], BF16)
        K.modb = psb("modb", [128, 6 * D], F32)
        K.cbc = psb("cbc", [128, 8, 128], F32)

        seq = [("setup", lambda: phase_setup(K))]
        for layer in range(2):
            seq.append((f"adaln{layer}", lambda layer=layer: phase_adaln(K, layer)))
            if layer == 0:
                seq.append(("mla_proj", lambda: phase_mla_proj(K)))
                seq.append(("attn", lambda: phase_attention(K)))
                seq.append(("outproj0", lambda: phase_outproj(K, None, I["mla_w_o"], 0, src_fm=K.OT, x_src=I["x"])))
            else:
                seq.append(("hy_norm", lambda: phase_norm_T(K, 0)))
                seq.append(("hy_in", lambda: phase_hyena_in(K)))
                seq.append(("hy_filter", lambda: phase_hyena_filter(K)))
                seq.append(("hy_conv", lambda: phase_hyena_conv(K)))
                seq.append(("outproj1", lambda: phase_outproj(K, K.Z[1], I["hy_w_out"], 1)))
            seq.append((f"moe_norm{layer}", lambda layer=layer: phase_norm_T(K, 3, router_layer=layer)))
            seq.append((f"moe_a{layer}", lambda layer=layer: phase_moe_a(K, layer)))
            seq.append((f"moe_b{layer}", lambda layer=layer: phase_moe_b(K, layer)))
        K.skip = set()
        for name, fn in seq:
            if name not in getattr(build_program, "SKIP", set()):
                fn()
            if stop == name:
                break
        if dump:
            def body(P, sb, ps):
                for nm in dump:
                    src = getattr(K, nm) if hasattr(K, nm) else None
                    if nm == "modb":
                        P.dma("sp", K.dbg[nm], K.modb[:])
                    else:
                        P.dma("sp", K.dbg[nm], src)
            run_phase(K, body)
    return nc


def run_phase(K, body):
    nc = K.nc
    with ExitStack() as es:
        P = Prog()

        pre = f"ph{_uid()}_"

        def sb(name, shape, dt):
            return es.enter_context(nc.sbuf_tensor(pre + name, list(shape), dt))

        def ps(name, shape, dt):
            return es.enter_context(nc.psum_tensor(pre + name, list(shape), dt))
        body(P, sb, ps)
        P.emit(nc)
    nc.all_engine_barrier()


def phase_setup(K):
    I = K.I

    def body(P, sb, ps):
        ct = sb("ct", [128, 8], F32)
        ca = sb("ca", [128, 8], F32)
        b1, b2, b3, b4 = P.bufs(4)
        P.dma("sp", K.ident32[:], I["ident32"], writes=[b1])
        P.dma("sp", K.identb[:], I["identb"], writes=[b2])
        P.dma("sp", ct[:], I["c_t"], writes=[b3])
        P.op("act", lambda e: e.activation(out=ca[:], in_=ct[:], func=AF.Silu), [b3], [b4])
        for kc in range(8):
            P.op("dve", lambda e, kc=kc: e.tensor_copy(out=K.cbc[:, kc, :], in_=ca[:, kc:kc + 1].to_broadcast([128, 128])), [b4], [b1])
    run_phase(K, body)


def phase_adaln(K, layer):
    I = K.I

    def body(P, sb, ps):
        wt = [sb(f"adaw{i}", [128, 8, 512], F32) for i in range(2)]
        wb = P.bufs(2)
        bt = sb("adab", [128, 6 * D], F32)
        bb = P.buf()
        gm = sb("gm", [128, 2, D], F32)
        gb_ = P.buf()
        pp = [ps(f"adaps{i}", [128, 512], F32) for i in range(2)]
        pb = P.bufs(2)
        mb = P.bufs(12)
        P.dma("sp", bt[:], I["ada_b"][layer:layer + 1, :].partition_broadcast(128), writes=[bb])
        P.dma("sp", gm[:, 0, :], I["norm_mix_g"][layer:layer + 1, :].partition_broadcast(128), writes=[gb_])
        P.dma("sp", gm[:, 1, :], I["norm_ffn_g"][layer:layer + 1, :].partition_broadcast(128), writes=[gb_])
        wv = I["ada_w"][layer].rearrange("(kc p) n -> p kc n", p=128)
        for j in range(12):
            q = "sp" if j % 2 == 0 else "pool"
            P.dma(q, wt[j % 2][:], wv[:, :, j * 512:(j + 1) * 512], writes=[wb[j % 2]])
            for kc in range(8):
                P.op("pe", lambda e, j=j, kc=kc: e.matmul(pp[j % 2][:], lhsT=K.cbc[:, kc, :], rhs=wt[j % 2][:, kc, :], start=(kc == 0), stop=(kc == 7)),
                     [wb[j % 2]], [pb[j % 2]])
            P.op("dve", lambda e, j=j: e.tensor_tensor(out=K.modb[:, j * 512:(j + 1) * 512], in0=pp[j % 2][:], in1=bt[:, j * 512:(j + 1) * 512], op=ALU.add),
                 [pb[j % 2], bb], [mb[j]])
        for si, gi in ((1, 0), (4, 1)):
            sec = K.modb[:, si * D:(si + 1) * D]
            P.op("dve", lambda e, sec=sec, gi=gi: e.scalar_tensor_tensor(out=sec, in0=sec, scalar=1.0, in1=gm[:, gi, :], op0=ALU.add, op1=ALU.mult),
                 [mb[2 * si], mb[2 * si + 1], gb_], [mb[2 * si], mb[2 * si + 1]])
    run_phase(K, body)


class NormCtx:
    def __init__(self, P, sb, ps, K, want_f32=False, tag="n"):
        self.P, self.K = P, K
        self.want_f32 = want_f32
        self.xt = [sb(f"{tag}_xt{i}", [128, D], F32) for i in range(2)]
        self.xb = P.bufs(2)
        self.junk = sb(f"{tag}_junk", [128, D], F32)
        self.jb = P.buf()
        self.ss = [sb(f"{tag}_ss{i}", [128, 2], F32) for i in range(2)]
        self.sb_ = P.bufs(2)
        self.hn = [sb(f"{tag}_hn{i}", [128, D], F32 if want_f32 else BF16) for i in range(2)]
        self.hb = P.bufs(2)
        self.h32 = [sb(f"{tag}_h32{i}", [128, D], F32) for i in range(2)]
        self.h32b = P.bufs(2)
        if want_f32:
            self.pt = [ps(f"{tag}_pt{i}", [128, 512], F32) for i in range(2)]
            self.ptb = P.bufs(2)
        else:
            self.pt = [ps(f"{tag}_pt", [128, D], BF16)]
            self.ptb = P.bufs(1)
        self.n = 0

    def run(self, x_ap, sec, out_bf, out_bf_buf, out_f32=None, out_f32_buf=None, q="sp"):
        st = self.run_a1(x_ap, sec, q)
        self.run_a2(st, out_bf, out_bf_buf, out_f32, out_f32_buf)

    def run_a1(self, x_ap, sec, q="sp"):
        P, K = self.P, self.K
        i = self.n % 2
        self.n += 1
        xt, ss, hn, h32 = self.xt[i], self.ss[i], self.hn[i], self.h32[i]
        SH = K.modb[:, sec * D:(sec + 1) * D]
        G = K.modb[:, (sec + 1) * D:(sec + 2) * D]
        P.dma(q, xt[:], x_ap, writes=[self.xb[i]])
        P.op("act", lambda e: e.activation(out=self.junk[:], in_=xt[:], func=AF.Square, accum_out=ss[:, 0:1]), [self.xb[i]], [self.jb, self.sb_[i]])
        P.op("act", lambda e: e.activation(out=ss[:, 1:2], in_=ss[:, 0:1], func=AF.Sqrt, scale=1.0 / D, bias=EPS), [self.sb_[i]], [self.sb_[i]])
        P.op("dve", lambda e: e.reciprocal(out=ss[:, 1:2], in_=ss[:, 1:2]), [self.sb_[i]], [self.sb_[i]])
        P.op("dve", lambda e: e.scalar_tensor_tensor(out=h32[:], in0=xt[:], scalar=ss[:, 1:2], in1=G, op0=ALU.mult, op1=ALU.mult),
             [self.xb[i], self.sb_[i]], [self.h32b[i]])
        P.op("pool", lambda e: e.tensor_tensor(out=hn[:], in0=h32[:], in1=SH, op=ALU.add), [self.h32b[i]], [self.hb[i]])
        return i

    def run_a2(self, i, out_bf, out_bf_buf, out_f32=None, out_f32_buf=None):
        P, K = self.P, self.K
        hn = self.hn[i]
        if self.want_f32:
            for half in range(2):
                for k in range(4):
                    kc = half * 4 + k
                    P.op("pe", lambda e, kc=kc, k=k, half=half: e.transpose(self.pt[half][:, k * 128:(k + 1) * 128], hn[:, kc * 128:(kc + 1) * 128], K.ident32[:]),
                         [self.hb[i]], [self.ptb[half]])
                P.op("act", lambda e, half=half: e.copy(out=out_bf[:, half * 4:(half + 1) * 4, :], in_=self.pt[half][:].rearrange("p (k t) -> p k t", k=4)),
                     [self.ptb[half]], [out_bf_buf])
                P.op("dve", lambda e, half=half: e.tensor_copy(out=out_f32[:, half * 4:(half + 1) * 4, :], in_=self.pt[half][:].rearrange("p (k t) -> p k t", k=4)),
                     [self.ptb[half]], [out_f32_buf])
        else:
            for kc in range(8):
                P.op("pe", lambda e, kc=kc: e.transpose(self.pt[0][:, kc * 128:(kc + 1) * 128], hn[:, kc * 128:(kc + 1) * 128], K.identb[:]),
                     [self.hb[i]], [self.ptb[0]])
            P.op("act", lambda e: e.copy(out=out_bf, in_=self.pt[0][:].rearrange("p (k t) -> p k t", k=8)), [self.ptb[0]], [out_bf_buf])


def load_bcast(P, sb, name, src_row_ap, n, q="sp", dt=F32):
    t = sb(name, [128, n], dt)
    b = P.buf()
    P.dma(q, t[:], src_row_ap.partition_broadcast(128), writes=[b])
    return t, b


def emit_sincos_reduce(P, t_ap, tmp_i, tmp_f, bufs_rw, shift):
    b = bufs_rw
    P.op("dve", lambda e: e.tensor_scalar(out=t_ap, in0=t_ap, scalar1=float(1.0 / (2 * math.pi)), scalar2=float(shift / (2 * math.pi)), op0=ALU.mult, op1=ALU.add), b, b)
    P.op("dve", lambda e: e.tensor_copy(out=tmp_i, in_=t_ap), b, b)
    P.op("dve", lambda e: e.tensor_copy(out=tmp_f, in_=tmp_i), b, b)
    P.op("dve", lambda e: e.tensor_tensor(out=t_ap, in0=t_ap, in1=tmp_f, op=ALU.subtract), b, b)
    P.op("dve", lambda e: e.tensor_scalar(out=tmp_f, in0=t_ap, scalar1=0.0, scalar2=None, op0=ALU.is_lt), b, b)
    P.op("dve", lambda e: e.tensor_tensor(out=t_ap, in0=t_ap, in1=tmp_f, op=ALU.add), b, b)
    P.op("dve", lambda e: e.tensor_scalar(out=t_ap, in0=t_ap, scalar1=float(2 * math.pi), scalar2=float(-math.pi), op0=ALU.mult, op1=ALU.add), b, b)
    P.op("dve", lambda e: e.tensor_scalar(out=t_ap, in0=t_ap, scalar1=3.1415925, scalar2=-3.1415925, op0=ALU.min, op1=ALU.max), b, b)


import os
CUT = float(os.environ.get("CUT", "9999"))
NTT = int(os.environ.get("NTT", "32"))


class _Cut(Exception):
    pass


def cut(n):
    if n >= CUT:
        raise _Cut()


def phase_mla_proj(K):
    I = K.I

    def body(P, sb, ps):
        try:
            body2(P, sb, ps)
        except _Cut:
            pass

    def body2(P, sb, ps):
        nctx = NormCtx(P, sb, ps, K, want_f32=False, tag="mn")
        wdn = sb("wdn", [128, 8, 448], BF16); wdn_b = P.buf()
        P.dma("pool", wdn[:], I["mla_w_down"].rearrange("(kc p) n -> p kc n", p=128), writes=[wdn_b])
        wuq = sb("wuq", [128, 2, 1536], BF16); wuq_b = P.buf()
        P.dma("pool", wuq[:], I["mla_w_uq"].rearrange("(kc p) n -> p kc n", p=128), writes=[wuq_b])
        wukv = sb("wukv", [128, 2048], BF16); wukv_b = P.buf()
        P.dma("pool", wukv[:], I["mla_w_ukv"], writes=[wukv_b])
        gqa, gqa_b = load_bcast(P, sb, "gqa", I["mla_q_a_g"], 256)
        gkva, gkva_b = load_bcast(P, sb, "gkva", I["mla_kv_a_g"], 128)
        gq, gq_b = load_bcast(P, sb, "gq", I["mla_q_norm_g"], 192)
        gk, gk_b = load_bcast(P, sb, "gk", I["mla_k_norm_g"], 192)
        invf, invf_b = load_bcast(P, sb, "invf", I["inv_freq"], 32)
        gqk = sb("gqk", [128, 128], F32); gqk_b = P.buf()
        P.op("dve", lambda e: e.tensor_tensor(out=gqk[:], in0=gq[:, 0:128], in1=gk[:, 0:128], op=ALU.mult), [gq_b, gk_b], [gqk_b])
        dv3 = sb("dv3", [128, 3], F32); dv_b = P.buf()
        for j, v in enumerate((1.0 / 256, 1.0 / 128, 1.0 / 64)):
            P.op("dve", lambda e, j=j, v=v: e.memset(dv3[:, j:j + 1], v), [], [dv_b])
        dv16 = sb("dv16", [128, 16], F32)
        P.op("dve", lambda e: e.memset(dv16[:, 0:8], 1.0 / 128), [], [dv_b])
        P.op("dve", lambda e: e.memset(dv16[:, 8:16], 1.0 / 64), [], [dv_b])
        posi = sb("posi", [128, NT], I32); posf = sb("posf", [128, NT], F32); pos_b = P.buf()
        P.dma("sp", posi[:], I["pos_t"], writes=[pos_b])
        P.op("dve", lambda e: e.tensor_copy(out=posf[:], in_=posi[:]), [pos_b], [pos_b])
        cs = sb("cs", [128, NT, 2, 32], F32); cs_b = P.buf()
        tmpi = sb("rtmpi", [128, NT, 2, 32], I32); tmpf = sb("rtmpf", [128, NT, 2, 32], F32)
        for tt in range(NT):
            for j in range(2):
                P.op("dve", lambda e, tt=tt, j=j: e.tensor_scalar(out=cs[:, tt, j, :], in0=invf[:], scalar1=posf[:, tt:tt + 1], scalar2=None, op0=ALU.mult),
                     [pos_b, invf_b], [cs_b])
        emit_sincos_reduce(P, cs[:, :, 0, :], tmpi[:, :, 0, :], tmpf[:, :, 0, :], [cs_b], 1.5 * math.pi)
        emit_sincos_reduce(P, cs[:, :, 1, :], tmpi[:, :, 1, :], tmpf[:, :, 1, :], [cs_b], math.pi)
        P.op("act", lambda e: e.activation(out=cs[:], in_=cs[:], func=AF.Sin), [cs_b], [cs_b])

        hT = [sb(f"hT{i}", [128, 8, 128], BF16) for i in range(2)]; hT_b = P.bufs(2)
        p_lat = ps("p_lat", [128, 512], F32); lat_b = P.buf()
        p_tr = ps("p_tr", [128, 1024], BF16); tr_b = P.buf()
        p_tr2 = ps("p_tr2", [128, 1024], BF16); tr2_b = P.buf()
        p_q = [ps(f"p_q{i}", [128, 512], F32) for i in range(3)]; q_b = P.bufs(3)
        p_kv = [ps("p_kv0", [128, 512], F32), p_lat]; kv_b = [P.buf(), lat_b]; kvser_b = P.buf()
        ss3_l = [sb(f"ss3{i}", [128, 3], F32) for i in range(2)]; ss3_bl = P.bufs(2)
        junk_l = [sb("junk448", [128, 448], F32)] * 2; junk_bl = [P.buf()] * 2
        cqn_l = [sb(f"cqn{i}", [128, 384], BF16) for i in range(2)]; cqn_bl = P.bufs(2)
        kpn_l = [sb(f"kpn{i}", [128, 64], F32) for i in range(2)]; kpn_bl = P.bufs(2)
        cT_l = [sb(f"cT{i}", [128, 3, 128], BF16) for i in range(2)]; cT_bl = P.bufs(2)
        sqq_l = [sb("sqq", [128, 1536], F32)] * 2; sqq_bl = [P.buf()] * 2
        qsb_l = [sb(f"qsb{i}", [128, 1536], F32) for i in range(2)]; qsb_bl = P.bufs(2)
        rq_l = [sb(f"rq{i}", [128, 16], F32) for i in range(2)]; rq_bl = P.bufs(2)
        qn_l = [sb(f"qn{i}", [128, 8, 128], BF16) for i in range(2)]; qn_bl = P.bufs(2)
        qr_l = [sb(f"qr{i}", [128, 8, 64], F32) for i in range(2)]; qr_bl = P.bufs(2)
        qr2_l = [sb(f"qr2{i}", [128, 8, 64], F32) for i in range(2)]; qr2_bl = P.bufs(2)
        qpe_l = [sb(f"qpe{i}", [128, 8, 64], BF16) for i in range(2)]; qpe_bl = P.bufs(2)
        kraw_l = [sb(f"kraw{i}", [128, 8, 128], F32) for i in range(2)]; kraw_bl = P.bufs(2)
        sqk_l = [sb("sqk", [128, 8, 128], F32)] * 2; sqk_bl = [P.buf()] * 2
        rk_l = [sb(f"rk{i}", [128, 8], F32) for i in range(2)]; rk_bl = P.bufs(2)
        kn_l = [sb(f"kn{i}", [128, 8, 128], BF16) for i in range(2)]; kn_bl = P.bufs(2)
        kpe_l = [sb(f"kpe{i}", [128, 64], BF16) for i in range(2)]; kpe_bl = P.bufs(2)
        kp2_l = [sb(f"kp2{i}", [128, 64], F32) for i in range(2)]; kp2_bl = P.bufs(2)
        vext = [sb(f"vext{i}", [128, 8, 130], BF16) for i in range(2)]; vext_b = P.bufs(2)
        for i in range(2):
            P.op("pool", lambda e, i=i: e.memset(vext[i][:], 1.0), [], [vext_b[i]])
        sQN = [sb(f"sQN{i}", [128, 8, 512], BF16) for i in range(2)]; sQN_b = P.bufs(2)
        sKN = [sb(f"sKN{i}", [128, 8, 512], BF16) for i in range(2)]; sKN_b = P.bufs(2)
        sQP = [sb(f"sQP{i}", [128, 4, 512], BF16) for i in range(2)]; sQP_b = P.bufs(2)
        sKP = [sb(f"sKP{i}", [64, 512], BF16) for i in range(2)]; sKP_b = P.bufs(2)

        def rope(src, dst, cosv, sinv, nh, tmp, rb, wb):
            x1 = src[:, :, 0:32] if nh else src[:, 0:32]
            x2 = src[:, :, 32:64] if nh else src[:, 32:64]
            t1 = tmp[:, :, 0:32] if nh else tmp[:, 0:32]
            t2 = tmp[:, :, 32:64] if nh else tmp[:, 32:64]
            d1 = dst[:, :, 0:32] if nh else dst[:, 0:32]
            d2 = dst[:, :, 32:64] if nh else dst[:, 32:64]
            if nh:
                cb = cosv.unsqueeze(1).to_broadcast([128, nh, 32])
                sn = sinv.unsqueeze(1).to_broadcast([128, nh, 32])
            else:
                cb, sn = cosv, sinv
            P.op("dve", lambda e: e.tensor_tensor(out=t1, in0=x2, in1=sn, op=ALU.mult), rb, wb)
            P.op("dve", lambda e: e.tensor_tensor(out=t2, in0=x1, in1=sn, op=ALU.mult), rb, wb)
            P.op("dve", lambda e: e.tensor_tensor(out=x1, in0=x1, in1=cb, op=ALU.mult), rb, rb)
            P.op("dve", lambda e: e.tensor_tensor(out=x2, in0=x2, in1=cb, op=ALU.mult), rb, rb)
            P.op("dve", lambda e: e.tensor_tensor(out=d1, in0=x1, in1=t1, op=ALU.subtract), rb + wb, wb + [P.buf()] if False else wb)
            P.op("dve", lambda e: e.tensor_tensor(out=d2, in0=x2, in1=t2, op=ALU.add), rb + wb, wb)

        cut(1)

        def do_tile(tt):
            ss3, ss3_b = ss3_l[tt % 2], ss3_bl[tt % 2]
            junk, junk_b = junk_l[tt % 2], junk_bl[tt % 2]
            cqn, cqn_b = cqn_l[tt % 2], cqn_bl[tt % 2]
            kpn, kpn_b = kpn_l[tt % 2], kpn_bl[tt % 2]
            cT, cT_b = cT_l[tt % 2], cT_bl[tt % 2]
            sqq, sqq_b = sqq_l[tt % 2], sqq_bl[tt % 2]
            qsb, qsb_b = qsb_l[tt % 2], qsb_bl[tt % 2]
            rq, rq_b = rq_l[tt % 2], rq_bl[tt % 2]
            qn, qn_b = qn_l[tt % 2], qn_bl[tt % 2]
            qr, qr_b = qr_l[tt % 2], qr_bl[tt % 2]
            qr2, qr2_b = qr2_l[tt % 2], qr2_bl[tt % 2]
            qpe, qpe_b = qpe_l[tt % 2], qpe_bl[tt % 2]
            kraw, kraw_b = kraw_l[tt % 2], kraw_bl[tt % 2]
            sqk, sqk_b = sqk_l[tt % 2], sqk_bl[tt % 2]
            rk, rk_b = rk_l[tt % 2], rk_bl[tt % 2]
            kn, kn_b = kn_l[tt % 2], kn_bl[tt % 2]
            kpe, kpe_b = kpe_l[tt % 2], kpe_bl[tt % 2]
            kp2, kp2_b = kp2_l[tt % 2], kp2_bl[tt % 2]
            hi = tt % 2
            g = (tt // 4) % 2
            j4 = tt % 4
            nctx.run(K.I["x"][tt * 128:(tt + 1) * 128, :], 0, hT[hi][:], hT_b[hi])
            yield
            for kc in range(8):
                P.op("pe", lambda e, kc=kc, hi=hi: e.matmul(p_lat[:, 0:448], lhsT=hT[hi][:, kc, :], rhs=wdn[:, kc, :], start=(kc == 0), stop=(kc == 7)),
                     [hT_b[hi], wdn_b], [lat_b])
            for j, (a, b) in enumerate(((0, 256), (256, 384), (384, 448))):
                P.op("act", lambda e, j=j, a=a, b=b: e.activation(out=junk[:, a:b], in_=p_lat[:, a:b], func=AF.Square, accum_out=ss3[:, j:j + 1]),
                     [lat_b], [junk_b, ss3_b])
            P.op("dve", lambda e: e.tensor_tensor(out=ss3[:], in0=ss3[:], in1=dv3[:], op=ALU.mult), [ss3_b, dv_b], [ss3_b])
            P.op("act", lambda e: e.activation(out=ss3[:], in_=ss3[:], func=AF.Sqrt, bias=EPS), [ss3_b], [ss3_b])
            P.op("dve", lambda e: e.reciprocal(out=ss3[:], in_=ss3[:]), [ss3_b], [ss3_b])
            P.op("dve", lambda e: e.scalar_tensor_tensor(out=cqn[:, 0:256], in0=p_lat[:, 0:256], scalar=ss3[:, 0:1], in1=gqa[:], op0=ALU.mult, op1=ALU.mult),
                 [lat_b, ss3_b, gqa_b], [cqn_b])
            P.op("dve", lambda e: e.scalar_tensor_tensor(out=cqn[:, 256:384], in0=p_lat[:, 256:384], scalar=ss3[:, 1:2], in1=gkva[:], op0=ALU.mult, op1=ALU.mult),
                 [lat_b, ss3_b, gkva_b], [cqn_b])
            P.op("dve", lambda e: e.scalar_tensor_tensor(out=kpn[:], in0=p_lat[:, 384:448], scalar=ss3[:, 2:3], in1=gk[:, 128:192], op0=ALU.mult, op1=ALU.mult),
                 [lat_b, ss3_b, gk_b], [kpn_b])
            cut(3)
            rope(kpn, kpe, cs[:, tt, 0, :], cs[:, tt, 1, :], 0, kp2, [kpn_b, cs_b], [kpe_b, kp2_b])
            cut(4)
            for j in range(3):
                P.op("pe", lambda e, j=j: e.transpose(p_tr2[:, j * 128:(j + 1) * 128], cqn[:, j * 128:(j + 1) * 128], K.identb[:]), [cqn_b], [tr2_b])
            P.op("act", lambda e: e.copy(out=cT[:], in_=p_tr2[:, 0:384].rearrange("p (k t) -> p k t", k=3)), [tr2_b], [cT_b])
            yield
            for n3 in range(3):
                for kc in range(2):
                    P.op("pe", lambda e, n3=n3, kc=kc: e.matmul(p_q[n3][:], lhsT=cT[:, kc, :], rhs=wuq[:, kc, n3 * 512:(n3 + 1) * 512], start=(kc == 0), stop=(kc == 1)),
                         [cT_b, wuq_b], [q_b[n3]])
            cut(5)
            for n3 in range(3):
                P.op("act", lambda e, n3=n3: e.copy(out=qsb[:, n3 * 512:(n3 + 1) * 512], in_=p_q[n3][:]), [q_b[n3]], [qsb_b])
            P.op("act", lambda e: e.activation(out=sqq[:], in_=qsb[:], func=AF.Square), [qsb_b], [sqq_b])
            sq3 = sqq[:].rearrange("p (h d) -> p h d", h=8)
            q3 = qsb[:].rearrange("p (h d) -> p h d", h=8)
            P.op("dve", lambda e: e.tensor_reduce(out=rq[:, 0:8], in_=sq3[:, :, 0:128], axis=AX.X, op=ALU.add), [sqq_b], [rq_b])
            P.op("dve", lambda e: e.tensor_reduce(out=rq[:, 8:16], in_=sq3[:, :, 128:192], axis=AX.X, op=ALU.add), [sqq_b], [rq_b])
            P.op("dve", lambda e: e.tensor_tensor(out=rq[:], in0=rq[:], in1=dv16[:], op=ALU.mult), [rq_b, dv_b], [rq_b])
            P.op("act", lambda e: e.activation(out=rq[:], in_=rq[:], func=AF.Sqrt, bias=EPS), [rq_b], [rq_b])
            P.op("dve", lambda e: e.reciprocal(out=rq[:], in_=rq[:]), [rq_b], [rq_b])
            P.op("dve", lambda e: e.tensor_tensor(out=qn[:], in0=q3[:, :, 0:128], in1=rq[:, 0:8].unsqueeze(2).to_broadcast([128, 8, 128]), op=ALU.mult), [qsb_b, rq_b], [qn_b])
            P.op("dve", lambda e: e.tensor_tensor(out=qr[:], in0=q3[:, :, 128:192], in1=rq[:, 8:16].unsqueeze(2).to_broadcast([128, 8, 64]), op=ALU.mult), [qsb_b, rq_b], [qr_b])
            P.op("dve", lambda e: e.tensor_tensor(out=qr[:], in0=qr[:], in1=gq[:, 128:192].unsqueeze(1).to_broadcast([128, 8, 64]), op=ALU.mult), [qr_b, gq_b], [qr_b])
            cut(6)
            rope(qr, qpe, cs[:, tt, 0, :], cs[:, tt, 1, :], 8, qr2, [qr_b, cs_b], [qpe_b, qr2_b])
            cut(7)
            vi = tt % 2
            for j in range(4):
                pb_ = p_kv[j % 2]
                P.op("pe", lambda e, j=j, pb_=pb_: e.matmul(pb_[:], lhsT=cT[:, 2, :], rhs=wukv[:, j * 512:(j + 1) * 512], start=True, stop=True),
                     [cT_b, wukv_b], [kv_b[j % 2]])
                kvv = pb_[:].rearrange("p (h d) -> p h d", h=2)
                cut(7.1)
                P.op("act", lambda e, j=j, kvv=kvv: e.activation(out=sqk[:, 2 * j:2 * j + 2, :], in_=kvv[:, :, 0:128], func=AF.Square), [kv_b[j % 2]], [sqk_b, kvser_b])
                cut(7.2)
                P.op("dve", lambda e, j=j, kvv=kvv: e.tensor_copy(out=kraw[:, 2 * j:2 * j + 2, :], in_=kvv[:, :, 0:128]), [kv_b[j % 2]], [kraw_b, kvser_b])
                cut(7.3)
                P.op("act", lambda e, j=j, kvv=kvv, vi=vi: e.copy(out=vext[vi][:, 2 * j:2 * j + 2, 0:128], in_=kvv[:, :, 128:256]), [kv_b[j % 2]], [vext_b[vi], kvser_b])
                cut(7.4 + 0.01 * j)
            cut(7.5)
            P.op("dve", lambda e: e.tensor_reduce(out=rk[:], in_=sqk[:], axis=AX.X, op=ALU.add), [sqk_b], [rk_b])
            cut(7.6)
            P.op("act", lambda e: e.activation(out=rk[:], in_=rk[:], func=AF.Sqrt, scale=1.0 / 128, bias=EPS), [rk_b], [rk_b])
            P.op("dve", lambda e: e.reciprocal(out=rk[:], in_=rk[:]), [rk_b], [rk_b])
            cut(7.7)
            P.op("dve", lambda e: e.tensor_tensor(out=kraw[:], in0=kraw[:], in1=rk[:].unsqueeze(2).to_broadcast([128, 8, 128]), op=ALU.mult), [kraw_b, rk_b], [kraw_b])
            cut(7.8)
            P.op("pool", lambda e: e.tensor_tensor(out=kn[:], in0=kraw[:], in1=gqk[:].unsqueeze(1).to_broadcast([128, 8, 128]), op=ALU.mult), [kraw_b, gqk_b], [kn_b])
            cut(8)
            P.dma("sp", K.V[:, tt * 128:(tt + 1) * 128, :].rearrange("h p e -> p h e"), vext[vi][:], reads=[vext_b[vi]])
            yield
            for h in range(8):
                P.op("pe", lambda e, h=h: e.transpose(p_tr[:, h * 128:(h + 1) * 128], qn[:, h, :], K.identb[:]), [qn_b], [tr_b])
            P.op("act", lambda e, g=g, j4=j4: e.copy(out=sQN[g][:, :, j4 * 128:(j4 + 1) * 128], in_=p_tr[:].rearrange("p (h t) -> p h t", h=8)), [tr_b], [sQN_b[g]])
            for h in range(8):
                P.op("pe", lambda e, h=h: e.transpose(p_tr[:, h * 128:(h + 1) * 128], kn[:, h, :], K.identb[:]), [kn_b], [tr_b])
            P.op("dve", lambda e, g=g, j4=j4: e.tensor_copy(out=sKN[g][:, :, j4 * 128:(j4 + 1) * 128], in_=p_tr[:].rearrange("p (h t) -> p h t", h=8)), [tr_b], [sKN_b[g]])
            qpe2 = qpe[:].rearrange("p (a b) d -> p a (b d)", b=2)
            for a in range(4):
                P.op("pe", lambda e, a=a: e.transpose(p_tr[:, a * 128:(a + 1) * 128], qpe2[:, a, :], K.identb[:]), [qpe_b], [tr_b])
            P.op("pe", lambda e: e.transpose(p_tr[0:64, 512:640], kpe[:], K.identb[:]), [kpe_b], [tr_b])
            P.op("act", lambda e, g=g, j4=j4: e.copy(out=sQP[g][:, :, j4 * 128:(j4 + 1) * 128], in_=p_tr[:, 0:512].rearrange("p (h t) -> p h t", h=4)), [tr_b], [sQP_b[g]])
            P.op("dve", lambda e, g=g, j4=j4: e.tensor_copy(out=sKP[g][:, j4 * 128:(j4 + 1) * 128], in_=p_tr[0:64, 512:640]), [tr_b], [sKP_b[g]])
            cut(10)
            if j4 == 3:
                t0 = (tt // 4) * 512
                P.dma("sp", K.QN[:, :, t0:t0 + 512].rearrange("h d t -> d h t"), sQN[g][:], reads=[sQN_b[g]])
                P.dma("sp", K.KN[:, :, t0:t0 + 512].rearrange("h d t -> d h t"), sKN[g][:], reads=[sKN_b[g]])
                P.dma("sp", K.QP[:, :, t0:t0 + 512].rearrange("h d t -> d h t"), sQP[g][:], reads=[sQP_b[g]])
                P.dma("sp", K.KP[:, t0:t0 + 512], sKP[g][:], reads=[sKP_b[g]])
        gens = [do_tile(tt) for tt in range(NTT)]
        NST = 4
        for step in range(NTT + NST - 1):
            for s_ in range(NST - 1, -1, -1):
                tt = step - s_
                if 0 <= tt < NTT:
                    try:
                        next(gens[tt])
                    except StopIteration:
                        pass
    run_phase(K, body)


def phase_attention(K):
    scale = 192 ** -0.5

    def body(P, sb, ps):
        kp = sb("kp", [64, S], BF16); kp_b = P.buf()
        P.dma("sp", kp[:], K.KP, writes=[kp_b])
        ones = sb("ones_b", [128, 128], BF16); ones_b = P.buf()
        P.op("dve", lambda e: e.memset(ones[:], 1.0), [], [ones_b])
        qn = [sb(f"aqn{i}", [128, S], BF16) for i in range(2)]; qn_b = P.bufs(2)
        kn = [sb(f"akn{i}", [128, S], BF16) for i in range(2)]; kn_b = P.bufs(2)
        qp = [sb(f"aqp{i}", [64, S], BF16) for i in range(2)]; qp_b = P.bufs(2)
        vv = [sb(f"av{i}", [128, NT, 130], BF16) for i in range(2)]; vv_b = P.bufs(2)
        NS = 4
        p_s = [ps(f"p_s{i}", [128, 512], F32) for i in range(NS)]; s_b = P.bufs(NS)
        p_o = [ps(f"p_o{i}", [128, 512], F32) for i in range(2)]; o_b = P.bufs(2)
        p_m = [ps(f"p_m{i}", [128, 512], F32) for i in range(2)]; m_b = P.bufs(2)
        NP = 5
        pt = [sb(f"pt{i}", [128, 512], BF16) for i in range(NP)]; pt_b = P.bufs(NP)
        rinv = [sb(f"rinv{i}", [128, 512], F32) for i in range(2)]; rinv_b = P.bufs(2)
        ost = [sb(f"ost{i}", [128, 512], BF16) for i in range(2)]; ost_b = P.bufs(2)

        def load_head(h):
            hi = h % 2
            P.dma("sp", qn[hi][:], K.QN[h], writes=[qn_b[hi]])
            P.dma("pool", kn[hi][:], K.KN[h], writes=[kn_b[hi]])
            P.dma("sp", qp[hi][:], K.QP[h // 2, (h % 2) * 64:(h % 2) * 64 + 64, :], writes=[qp_b[hi]])
            P.dma("pool", vv[hi][:], K.V[h].rearrange("(kt p) e -> p kt e", p=128), writes=[vv_b[hi]])

        iters = [(h, qg, kt) for h in range(8) for qg in range(8) for kt in range(NT)]

        def emit_s(i):
            h, qg, kt = iters[i]
            hi = h % 2
            si = i % NS
            P.op("pe", lambda e: e.matmul(p_s[si][:], lhsT=kn[hi][:, kt * 128:(kt + 1) * 128], rhs=qn[hi][:, qg * 512:(qg + 1) * 512], start=True, stop=False),
                 [kn_b[hi], qn_b[hi]], [s_b[si]])
            P.op("pe", lambda e: e.matmul(p_s[si][:], lhsT=kp[:, kt * 128:(kt + 1) * 128], rhs=qp[hi][:, qg * 512:(qg + 1) * 512], start=False, stop=True),
                 [kp_b, qp_b[hi]], [s_b[si]])

        def emit_rest(i):
            h, qg, kt = iters[i]
            hi = h % 2
            si = i % NS
            pi = i % NP
            oi = (h * 8 + qg) % 2
            P.op("act", lambda e: e.activation(out=pt[pi][:], in_=p_s[si][:], func=AF.Exp, scale=scale), [s_b[si]], [pt_b[pi]])
            P.op("pe", lambda e: e.matmul(p_o[oi][:], lhsT=vv[hi][:, kt, 0:128], rhs=pt[pi][:], start=(kt == 0), stop=(kt == NT - 1)),
                 [pt_b[pi], vv_b[hi]], [o_b[oi]])
            P.op("pe", lambda e: e.matmul(p_m[oi][:], lhsT=ones[:], rhs=pt[pi][:], start=(kt == 0), stop=(kt == NT - 1)),
                 [pt_b[pi], ones_b], [m_b[oi]])
            if kt == NT - 1:
                P.op("dve", lambda e: e.reciprocal(out=rinv[oi][:], in_=p_m[oi][:]), [m_b[oi]], [rinv_b[oi]])
                P.op("dve", lambda e: e.tensor_tensor(out=ost[oi][:], in0=p_o[oi][:], in1=rinv[oi][:], op=ALU.mult), [o_b[oi], rinv_b[oi]], [ost_b[oi]])
                P.dma("sp", K.OT[h, :, qg * 512:(qg + 1) * 512], ost[oi][:], reads=[ost_b[oi]])

        LOOK = 3
        load_head(0)
        load_head(1)
        n = len(iters)
        for i in range(min(LOOK, n)):
            emit_s(i)
        for i in range(n):
            if i + LOOK < n:
                emit_s(i + LOOK)
            emit_rest(i)
            h, qg, kt = iters[i]
            if qg == 0 and kt == 8 and 1 <= h and h + 1 < 8:
                load_head(h + 1)
    run_phase(K, body)


def phase_outproj(K, src_bf, w_ap, layer, src_fm=None, x_src=None):
    def body(P, sb, ps):
        wo = sb("wo", [128, 8, D], BF16); wo_b = P.bufs(8)
        wv = w_ap.rearrange("(kc p) n -> p kc n", p=128)
        for kc in range(8):
            P.dma("pool", wo[:, kc, :], wv[:, kc, :], writes=[wo_b[kc]])
        xt = [sb(f"oxt{i}", [128, D], F32) for i in range(2)]; xt_b = P.bufs(2)
        p_y = [ps(f"p_y{i}", [128, 512], F32) for i in range(4)]; y_b = P.bufs(4)
        tmp = [sb(f"otmp{i}", [128, D], F32) for i in range(2)]; tmp_b = P.bufs(2)
        GATE = K.modb[:, 2 * D:3 * D]
        if src_fm is None:
            ot = [sb(f"ot{i}", [128, D], BF16) for i in range(2)]; ot_b = P.bufs(2)
            oT = [sb(f"oT{i}", [128, 8, 128], BF16) for i in range(2)]; oT_b = P.bufs(2)
            p_tr = ps("p_tr", [128, D], BF16); tr_b = P.buf()
        else:
            og = [sb(f"og{i}", [128, 8, 512], BF16) for i in range(2)]; og_b = P.bufs(2)
        for tt in range(NT):
            i = tt % 2
            P.dma("pool", xt[i][:], (K.xs if x_src is None else x_src)[tt * 128:(tt + 1) * 128, :], writes=[xt_b[i]])
            if src_fm is None:
                P.dma("sp", ot[i][:], src_bf[tt * 128:(tt + 1) * 128, :], writes=[ot_b[i]])
                for kc in range(8):
                    P.op("pe", lambda e, kc=kc, i=i: e.transpose(p_tr[:, kc * 128:(kc + 1) * 128], ot[i][:, kc * 128:(kc + 1) * 128], K.identb[:]), [ot_b[i]], [tr_b])
                P.op("act", lambda e, i=i: e.copy(out=oT[i][:], in_=p_tr[:].rearrange("p (k t) -> p k t", k=8)), [tr_b], [oT_b[i]])
                lhs = lambda kc, i=i: oT[i][:, kc, :]
                lb = oT_b[i]
            else:
                gi = (tt // 4) % 2
                if tt % 4 == 0:
                    t0 = tt * 128
                    P.dma("sp", og[gi][:], src_fm[:, :, t0:t0 + 512].rearrange("h d t -> d h t"), writes=[og_b[gi]])
                lhs = lambda kc, gi=gi, j=tt % 4: og[gi][:, kc, j * 128:(j + 1) * 128]
                lb = og_b[gi]
            for dh in range(2):
                yb = i * 2 + dh
                for kc in range(8):
                    P.op("pe", lambda e, kc=kc, dh=dh, yb=yb, lhs=lhs: e.matmul(p_y[yb][:], lhsT=lhs(kc), rhs=wo[:, kc, dh * 512:(dh + 1) * 512], start=(kc == 0), stop=(kc == 7)),
                         [lb, wo_b[kc]], [y_b[yb]])
                P.op("dve", lambda e, i=i, dh=dh, yb=yb: e.tensor_tensor(out=tmp[i][:, dh * 512:(dh + 1) * 512], in0=p_y[yb][:], in1=GATE[:, dh * 512:(dh + 1) * 512], op=ALU.mult),
                     [y_b[yb]], [tmp_b[i]])
            P.op("dve", lambda e, i=i: e.tensor_tensor(out=xt[i][:], in0=xt[i][:], in1=tmp[i][:], op=ALU.add), [xt_b[i], tmp_b[i]], [xt_b[i]])
            P.dma("sp", K.xs[tt * 128:(tt + 1) * 128, :], xt[i][:], reads=[xt_b[i]])
    run_phase(K, body)


def phase_norm_T(K, sec, router_layer=None):
    I = K.I
    router = router_layer is not None

    def body(P, sb, ps):
        nctx = NormCtx(P, sb, ps, K, want_f32=router, tag="fn")
        hst = [sb(f"hst{i}", [128, 8, 512], BF16) for i in range(2)]; hst_b = P.bufs(2)
        if router:
            layer = router_layer
            h32 = [sb(f"h32T{i}", [128, 8, 128], F32) for i in range(2)]; h32_b = P.bufs(2)
            wr = sb("wr", [128, 8, 20], F32); wr_b = P.buf()
            P.dma("sp", wr[:], I["moe_wr"][layer].rearrange("(kc p) n -> p kc n", p=128), writes=[wr_b])
            br, br_b = load_bcast(P, sb, "br", I["moe_br"][layer:layer + 1, :], 20)
            combT = sb("combT", [16, S], BF16); combT_b = P.buf()
            p_r = ps("p_r", [128, 512], F32); r_b = P.buf()
            lg_l = [sb(f"lg{i}", [128, 20], F32) for i in range(2)]; lg_bl = P.bufs(2)
            m8_l = [sb(f"m8{i}", [128, 8], F32) for i in range(2)]; m8_bl = P.bufs(2)
            gm_l = [sb(f"gmask{i}", [128, 4], F32) for i in range(2)]; gm_bl = P.bufs(2)
            gw_l = [sb(f"gw{i}", [128, 4], F32) for i in range(2)]; gw_bl = P.bufs(2)
            em_l = [sb(f"em{i}", [128, 4, 4], F32) for i in range(2)]; em_bl = P.bufs(2)
            ex_l = [sb(f"ex{i}", [128, 16], F32) for i in range(2)]; ex_bl = P.bufs(2)
            cmb_l = [sb(f"cmb{i}", [128, 16], F32) for i in range(2)]; cmb_bl = P.bufs(2)
            den_l = [sb(f"den{i}", [128, 2], F32) for i in range(2)]; den_bl = P.bufs(2)
            BIG = 1.0e4
        a1 = {}

        def stage_a1(tt):
            a1[tt] = nctx.run_a1(K.xs[tt * 128:(tt + 1) * 128, :], sec)

        def stage_a2(tt):
            i = tt % 2
            g = (tt // 4) % 2
            j4 = tt % 4
            if router:
                nctx.run_a2(a1[tt], hst[g][:, :, j4 * 128:(j4 + 1) * 128], hst_b[g], h32[i][:], h32_b[i])
            else:
                nctx.run_a2(a1[tt], hst[g][:, :, j4 * 128:(j4 + 1) * 128], hst_b[g])
            if j4 == 3:
                t0 = (tt // 4) * 512
                P.dma("sp", K.HT[:, :, t0:t0 + 512], hst[g][:], reads=[hst_b[g]])

        def do_tile(tt):
            if not router:
                return
            lg, lg_b = lg_l[tt % 2], lg_bl[tt % 2]
            m8, m8_b = m8_l[tt % 2], m8_bl[tt % 2]
            gm, gm_b = gm_l[tt % 2], gm_bl[tt % 2]
            gw, gw_b = gw_l[tt % 2], gw_bl[tt % 2]
            em, em_b = em_l[tt % 2], em_bl[tt % 2]
            ex, ex_b = ex_l[tt % 2], ex_bl[tt % 2]
            cmb, cmb_b = cmb_l[tt % 2], cmb_bl[tt % 2]
            den, den_b = den_l[tt % 2], den_bl[tt % 2]
            i = tt % 2
            for kc in range(8):
                P.op("pe", lambda e, kc=kc, i=i: e.matmul(p_r[:, 0:20], lhsT=h32[i][:, kc, :], rhs=wr[:, kc, :], start=(kc == 0), stop=(kc == 7)),
                     [h32_b[i], wr_b], [r_b])
            P.op("dve", lambda e: e.tensor_tensor(out=lg[:], in0=p_r[:, 0:20], in1=br[:], op=ALU.add), [r_b, br_b], [lg_b])
            P.op("dve", lambda e: e.tensor_reduce(out=m8[:, 0:1], in_=lg[:, 0:4], axis=AX.X, op=ALU.max), [lg_b], [m8_b])
            P.op("dve", lambda e: e.tensor_scalar(out=gm[:], in0=lg[:, 0:4], scalar1=m8[:, 0:1], scalar2=None, op0=ALU.is_ge), [lg_b, m8_b], [gm_b])
            P.op("dve", lambda e: e.tensor_scalar(out=gw[:], in0=lg[:, 0:4], scalar1=m8[:, 0:1], scalar2=None, op0=ALU.subtract), [lg_b, m8_b], [gw_b])
            P.op("act", lambda e: e.activation(out=gw[:], in_=gw[:], func=AF.Exp, accum_out=den[:, 0:1]), [gw_b], [gw_b, den_b])
            P.op("dve", lambda e: e.tensor_scalar(out=gm[:], in0=gm[:], scalar1=BIG, scalar2=-BIG, op0=ALU.mult, op1=ALU.add), [gm_b], [gm_b])
            P.op("dve", lambda e: e.tensor_tensor(out=em[:], in0=lg[:, 4:20].rearrange("p (g k) -> p g k", g=4), in1=gm[:].unsqueeze(2).to_broadcast([128, 4, 4]), op=ALU.add),
                 [lg_b, gm_b], [em_b])
            emf = em[:].rearrange("p g k -> p (g k)")
            P.op("dve", lambda e: e.max(out=m8[:], in_=emf), [em_b], [m8_b])
            P.op("dve", lambda e: e.tensor_scalar(out=ex[:], in0=emf, scalar1=m8[:, 0:1], scalar2=None, op0=ALU.subtract), [em_b, m8_b], [ex_b])
            P.op("act", lambda e: e.activation(out=ex[:], in_=ex[:], func=AF.Exp), [ex_b], [ex_b])
            P.op("dve", lambda e: e.tensor_scalar(out=cmb[:], in0=emf, scalar1=m8[:, 1:2], scalar2=None, op0=ALU.is_ge), [em_b, m8_b], [cmb_b])
            P.op("dve", lambda e: e.tensor_tensor(out=cmb[:], in0=cmb[:], in1=ex[:], op=ALU.mult), [cmb_b, ex_b], [cmb_b])
            P.op("dve", lambda e: e.tensor_reduce(out=den[:, 1:2], in_=cmb[:], axis=AX.X, op=ALU.add), [cmb_b], [den_b])
            P.op("dve", lambda e: e.tensor_tensor(out=den[:, 0:1], in0=den[:, 0:1], in1=den[:, 1:2], op=ALU.mult), [den_b], [den_b])
            P.op("dve", lambda e: e.reciprocal(out=den[:, 0:1], in_=den[:, 0:1]), [den_b], [den_b])
            P.op("dve", lambda e: e.tensor_scalar(out=cmb[:], in0=cmb[:], scalar1=den[:, 0:1], scalar2=None, op0=ALU.mult), [cmb_b, den_b], [cmb_b])
            P.op("pe", lambda e: e.transpose(p_r[0:16, 128:256], cmb[:], K.ident32[:]), [cmb_b], [r_b])
            P.op("act", lambda e, tt=tt: e.copy(out=combT[:, tt * 128:(tt + 1) * 128], in_=p_r[0:16, 128:256]), [r_b], [combT_b])
        stage_a1(0)
        stage_a1(1)
        stage_a2(0)
        for tt in range(NT):
            if tt + 2 < NT:
                stage_a1(tt + 2)
            if tt + 1 < NT:
                stage_a2(tt + 1)
            do_tile(tt)
        if router:
            P.dma("sp", K.CT, combT[:], reads=[combT_b])
    run_phase(K, body)


def phase_moe_a(K, layer):
    I = K.I

    def body(P, sb, ps):
        hT = sb("hT_all", [128, 8, S], BF16); hT_b = P.bufs(8)
        for kc in range(8):
            P.dma("sp" if kc % 2 == 0 else "pool", hT[:, kc, :], K.HT[:, kc, :], writes=[hT_b[kc]])
        sel = sb("sel", [16, 16 * 128], BF16); sel_b = P.buf()
        P.dma("sp", sel[:], I["sel"], writes=[sel_b])
        combT = sb("combT", [16, S], BF16); combT_b = P.buf()
        P.dma("sp", combT[:], K.CT, writes=[combT_b])
        wg = [sb(f"wg{i}", [128, 8, DE], BF16) for i in range(2)]; wg_b = P.bufs(2)
        wu = [sb(f"wu{i}", [128, 8, DE], BF16) for i in range(2)]; wu_b = P.bufs(2)
        p_g = [ps(f"p_g{i}", [128, 512], F32) for i in range(2)]; g_b = P.bufs(2)
        p_u = [ps(f"p_u{i}", [128, 512], F32) for i in range(2)]; u_b = P.bufs(2)
        p_c = [ps(f"p_c{i}", [128, 512], F32) for i in range(2)]; c_b = P.bufs(2)
        cb = [sb(f"cb{i}", [128, 512], F32) for i in range(2)]; cb_b = P.bufs(2)
        sg = [sb(f"sg{i}", [128, 512], F32) for i in range(2)]; sg_b = P.bufs(2)
        tu = [sb(f"tu{i}", [128, 512], F32) for i in range(2)]; tu_b = P.bufs(2)
        ast = [sb(f"ast{i}", [128, 2, 512], BF16) for i in range(2)]; ast_b = P.bufs(2)
        it = 0
        ci = 0
        for ex_i in range(NE):
            wi = ex_i % 2
            P.dma("pool", wg[wi][:], I["moe_w_gate"][layer, ex_i].rearrange("(kc p) n -> p kc n", p=128), writes=[wg_b[wi]])
            P.dma("pool", wu[wi][:], I["moe_w_up"][layer, ex_i].rearrange("(kc p) n -> p kc n", p=128), writes=[wu_b[wi]])
            for tq in range(8):
                c_i = ci % 2
                ci += 1
                P.op("pe", lambda e, ex_i=ex_i, tq=tq, c_i=c_i: e.matmul(p_c[c_i][:], lhsT=sel[:, ex_i * 128:(ex_i + 1) * 128], rhs=combT[:, tq * 512:(tq + 1) * 512], start=True, stop=True),
                     [sel_b, combT_b], [c_b[c_i]])
                P.op("act", lambda e, c_i=c_i: e.copy(out=cb[c_i][:], in_=p_c[c_i][:]), [c_b[c_i]], [cb_b[c_i]])
                for hc in range(2):
                    k = it % 2
                    it += 1
                    for kc in range(8):
                        P.op("pe", lambda e, k=k, kc=kc, wi=wi, hc=hc, tq=tq: e.matmul(p_g[k][:], lhsT=wg[wi][:, kc, hc * 128:(hc + 1) * 128], rhs=hT[:, kc, tq * 512:(tq + 1) * 512], start=(kc == 0), stop=(kc == 7)),
                             [wg_b[wi], hT_b[kc]], [g_b[k]])
                    for kc in range(8):
                        P.op("pe", lambda e, k=k, kc=kc, wi=wi, hc=hc, tq=tq: e.matmul(p_u[k][:], lhsT=wu[wi][:, kc, hc * 128:(hc + 1) * 128], rhs=hT[:, kc, tq * 512:(tq + 1) * 512], start=(kc == 0), stop=(kc == 7)),
                             [wu_b[wi], hT_b[kc]], [u_b[k]])
                    P.op("act", lambda e, k=k: e.activation(out=sg[k][:], in_=p_g[k][:], func=AF.Silu), [g_b[k]], [sg_b[k]])
                    P.op("dve", lambda e, k=k: e.tensor_tensor(out=tu[k][:], in0=sg[k][:], in1=p_u[k][:], op=ALU.mult), [sg_b[k], u_b[k]], [tu_b[k]])
                    P.op("pool", lambda e, k=k, c_i=c_i, hc=hc: e.tensor_tensor(out=ast[c_i][:, hc, :], in0=tu[k][:], in1=cb[c_i][:], op=ALU.mult), [tu_b[k], cb_b[c_i]], [ast_b[c_i]])
                P.dma("sp", K.AT[ex_i, :, :, tq * 512:(tq + 1) * 512].rearrange("c p t -> p c t"), ast[c_i][:], reads=[ast_b[c_i]])
    run_phase(K, body)


def phase_moe_b(K, layer):
    I = K.I
    final = (layer == 1)

    def body(P, sb, ps):
        wd = sb("wd", [128, NE, 2, D], BF16); wd_b = P.bufs(NE)
        for e_ in range(NE):
            P.dma("pool", wd[:, e_, :, :], I["moe_w_down"][layer, e_].rearrange("(c p) n -> p c n", p=128), writes=[wd_b[e_]])
        at = [sb(f"at{i}", [128, NE, 2, 512], BF16) for i in range(2)]; at_b = P.bufs(2)
        xt = [sb(f"mxt{i}", [128, D], F32) for i in range(2)]; xt_b = P.bufs(2)
        tmp = [sb(f"mtmp{i}", [128, D], F32) for i in range(2)]; tmp_b = P.bufs(2)
        p_y = [ps(f"p_y{i}", [128, 512], F32) for i in range(4)]; y_b = P.bufs(4)
        GATE = K.modb[:, 5 * D:6 * D]
        dst = K.out if final else K.xs
        for tq in range(8):
            ai = tq % 2
            P.dma("sp", at[ai][:], K.AT[:, :, :, tq * 512:(tq + 1) * 512].rearrange("e c p t -> p e c t"), writes=[at_b[ai]])
            for j in range(4):
                tt = tq * 4 + j
                i = tt % 2
                P.dma("sp", xt[i][:], K.xs[tt * 128:(tt + 1) * 128, :], writes=[xt_b[i]])
                for dh in range(2):
                    yb = i * 2 + dh
                    n = 0
                    for e_ in range(NE):
                        for c in range(2):
                            P.op("pe", lambda e, ai=ai, e_=e_, c=c, j=j, dh=dh, yb=yb, n=n: e.matmul(p_y[yb][:], lhsT=at[ai][:, e_, c, j * 128:(j + 1) * 128], rhs=wd[:, e_, c, dh * 512:(dh + 1) * 512], start=(n == 0), stop=(n == 2 * NE - 1)),
                                 [at_b[ai], wd_b[e_]], [y_b[yb]])
                            n += 1
                    P.op("dve", lambda e, i=i, dh=dh, yb=yb: e.tensor_tensor(out=tmp[i][:, dh * 512:(dh + 1) * 512], in0=p_y[yb][:], in1=GATE[:, dh * 512:(dh + 1) * 512], op=ALU.mult),
                         [y_b[yb]], [tmp_b[i]])
                P.op("pool", lambda e, i=i: e.tensor_tensor(out=xt[i][:], in0=xt[i][:], in1=tmp[i][:], op=ALU.add), [xt_b[i], tmp_b[i]], [xt_b[i]])
                P.dma("sp", dst[tt * 128:(tt + 1) * 128, :], xt[i][:], reads=[xt_b[i]])
    run_phase(K, body)


def phase_hyena_in(K):
    I = K.I

    def body(P, sb, ps):
        hT = sb("hT_all", [128, 8, S], BF16); hT_b = P.bufs(8)
        for kc in range(8):
            P.dma("sp", hT[:, kc, :], K.HT[:, kc, :], writes=[hT_b[kc]])
        bin_ = sb("bin", [128, 24], F32); cw = sb("cw", [128, 3, 24], F32); cbv = sb("cbv", [128, 24], F32); par_b = P.buf()
        P.dma("sp", bin_[:], I["hy_b_in_t"], writes=[par_b])
        P.dma("sp", cw[:], I["hy_conv_w_t"], writes=[par_b])
        P.dma("sp", cbv[:], I["hy_conv_b_t"], writes=[par_b])
        wv = I["hy_w_in"].rearrange("(kc p) n -> p kc n", p=128)
        wi = [sb(f"wi{i}", [128, 8, 128], BF16) for i in range(2)]; wi_b = P.bufs(2)
        p_u = [ps(f"p_u{i}", [128, 512], F32) for i in range(3)]; pu_b = P.bufs(3)
        urow = [sb(f"urow{i}", [128, S + 2], F32) for i in range(2)]; ur_b = P.bufs(2)
        for i in range(2):
            P.op("pool", lambda e, i=i: e.memset(urow[i][:, 0:1], 0.0), [], [ur_b[i]])
            P.op("pool", lambda e, i=i: e.memset(urow[i][:, S + 1:S + 2], 0.0), [], [ur_b[i]])
        ucT = sb("ucT0", [128, S], F32); uc_b = P.buf()
        p_t = [ps(f"p_t{i}", [128, 1024], BF16) for i in range(2)]; pt_b = P.bufs(2)
        stg = [sb(f"ustg{i}", [128, NT, 128], BF16) for i in range(2)]; stg_b = P.bufs(2)
        ucB = [sb(f"ucB{i}", [128, S], BF16) for i in range(2)]; ucB_b = P.bufs(2)
        st = {"it": 0}

        def proj(cc):
            i = cc % 2
            P.dma("pool", wi[i][:], wv[:, :, cc * 128:(cc + 1) * 128], writes=[wi_b[i]])
            for tq in range(8):
                k = st["it"] % 3
                st["it"] += 1
                for kc in range(8):
                    P.op("pe", lambda e, k=k, kc=kc, tq=tq: e.matmul(p_u[k][:], lhsT=wi[i][:, kc, :], rhs=hT[:, kc, tq * 512:(tq + 1) * 512], start=(kc == 0), stop=(kc == 7)),
                         [wi_b[i], hT_b[kc]], [pu_b[k]])
                P.op("act", lambda e, k=k, tq=tq: e.activation(out=urow[i][:, 1 + tq * 512:1 + (tq + 1) * 512], in_=p_u[k][:], func=AF.Identity, bias=bin_[:, cc:cc + 1], scale=1.0),
                     [pu_b[k], par_b], [ur_b[i]])
            P.op("dve", lambda e: e.tensor_scalar(out=ucT[:], in0=urow[i][:, 1:S + 1], scalar1=cw[:, 1, cc:cc + 1], scalar2=cbv[:, cc:cc + 1], op0=ALU.mult, op1=ALU.add),
                 [ur_b[i], par_b], [uc_b])
            P.op("dve", lambda e: e.scalar_tensor_tensor(out=ucT[:], in0=urow[i][:, 0:S], scalar=cw[:, 0, cc:cc + 1], in1=ucT[:], op0=ALU.mult, op1=ALU.add),
                 [ur_b[i], par_b, uc_b], [uc_b])
            P.op("dve", lambda e: e.scalar_tensor_tensor(out=ucB[i][:], in0=urow[i][:, 2:S + 2], scalar=cw[:, 2, cc:cc + 1], in1=ucT[:], op0=ALU.mult, op1=ALU.add),
                 [ur_b[i], par_b, uc_b], [ucB_b[i]])

        def back(cc):
            i = cc % 2
            for t4 in range(8):
                k = t4 % 2
                for j in range(4):
                    tt = t4 * 4 + j
                    P.op("pe", lambda e, k=k, j=j, tt=tt: e.transpose(p_t[k][:, j * 128:(j + 1) * 128], ucB[i][:, tt * 128:(tt + 1) * 128], K.identb[:]), [ucB_b[i]], [pt_b[k]])
                P.op("act", lambda e, k=k, t4=t4: e.copy(out=stg[i][:, t4 * 4:(t4 + 1) * 4, :], in_=p_t[k][:, 0:512].rearrange("p (j c) -> p j c", j=4)), [pt_b[k]], [stg_b[i]])
            P.dma("act", K.UC[:, cc * 128:(cc + 1) * 128].rearrange("(tt p) c -> p tt c", p=128), stg[i][:], reads=[stg_b[i]])

        proj(0)
        for cc in range(24):
            if cc + 1 < 24:
                proj(cc + 1)
            back(cc)
    run_phase(K, body)


def s1_state(P, sb, ps, K, tag):
    st = {"it": 0}
    st["F1"] = sb(tag + "F1", [128, 2, 128], BF16); st["F1_b"] = P.buf()
    P.dma("sp", st["F1"][:], K.I["F1"], writes=[st["F1_b"]])
    st["p"] = [ps(f"{tag}p{i}", [128, 512], F32) for i in range(2)]; st["p_b"] = P.bufs(2)
    return st


def phase_hyena_filter(K):
    I = K.I

    def body(P, sb, ps):
        zT = sb("zT", [33, S], F32); z_b = P.buf()
        P.dma("sp", zT[:], I["zT"], writes=[z_b])
        w1 = sb("fw1", [33, 64], F32); w2 = sb("fw2", [64, 64], F32); w3 = sb("fw3", [64, 64], F32); bf_ = sb("fbf", [64, 6], F32); w_b = P.buf()
        P.dma("sp", w1[:], I["hy_f_w1"], writes=[w_b])
        P.dma("sp", w2[:], I["hy_f_w2"], writes=[w_b])
        P.dma("sp", w3[:], I["hy_f_w3"], writes=[w_b])
        P.dma("sp", bf_[:], I["hy_f_bf_t"], writes=[w_b])
        fb = sb("fb", [64, 3], F32)
        for l in range(3):
            P.op("dve", lambda e, l=l: e.tensor_tensor(out=fb[:, l:l + 1], in0=bf_[:, 2 * l:2 * l + 1], in1=bf_[:, 2 * l + 1:2 * l + 2], op=ALU.mult), [w_b], [w_b])
        hd = [sb(f"hd{i}", [64, S], F32) for i in range(2)]; hd_b = P.bufs(2)
        hdb = sb("hdb", [64, S], BF16); hdb_b = P.buf()
        ti = sb("fti", [64, 512], I32); tf = sb("ftf", [64, 512], F32); tmp_b = P.buf()
        p_h = ps("p_h", [128, 512], F32); ph_b = P.buf()
        srcs = [(zT, z_b, w1, 33), (hd[0], hd_b[0], w2, 64), (hd[1], hd_b[1], w3, 64)]
        for l in range(3):
            src, src_b, wl, kdim = srcs[l]
            dst, dst_b = (hd[l % 2], hd_b[l % 2])
            for ch in range(8):
                sl = slice(ch * 512, (ch + 1) * 512)
                P.op("pe", lambda e, src=src, wl=wl, kdim=kdim, sl=sl: e.matmul(p_h[0:64, :], lhsT=wl[0:kdim, :], rhs=src[0:kdim, sl], start=True, stop=True), [src_b, w_b], [ph_b])
                P.op("dve", lambda e, dst=dst, sl=sl, l=l: e.tensor_scalar(out=dst[:, sl], in0=p_h[0:64, :], scalar1=bf_[:, 2 * l + 1:2 * l + 2], scalar2=fb[:, l:l + 1], op0=ALU.mult, op1=ALU.add),
                     [ph_b, w_b], [dst_b])
                emit_sincos_reduce(P, dst[:, sl], ti[:], tf[:], [dst_b, tmp_b], math.pi + 64 * math.pi)
            P.op("act", lambda e, dst=dst: e.activation(out=dst[:], in_=dst[:], func=AF.Sin), [dst_b], [dst_b])
        P.op("dve", lambda e: e.tensor_copy(out=hdb[:], in_=hd[0][:]), [hd_b[0]], [hdb_b])
        if "hdn" in K.dbg:
            P.dma("sp", K.dbg["hdn"], hd[0][:], reads=[hd_b[0]])
        w4 = sb("fw4", [64, 4 * D], BF16); w4_b = P.buf()
        P.dma("pool", w4[:], I["hy_f_w4"], writes=[w4_b])
        dl, dl_b = load_bcast(P, sb, "deltas", I["deltas"], D)
        ntl = sb("ntl", [128, 32], F32); ntl_b = P.buf()
        P.dma("sp", ntl[:], I["ntl"], writes=[ntl_b])
        msk = sb("msk", [128, 4, 128], F32); msk_b = P.buf()
        P.dma("sp", msk[:], I["mask4"], writes=[msk_b])
        dec = [sb(f"dec{i}", [128, D], F32) for i in range(2)]; dec_b = P.bufs(2)
        asum = sb("asum", [128, 2 * D], F32); as_b = P.buf()
        fa = [sb(f"fa{i}", [128, 2 * D], BF16) for i in range(2)]; fa_b = P.bufs(2)
        absf = [sb("absf0", [128, 2 * D], F32)] * 2; absf_b = [P.buf()] * 2
        p_f = [ps(f"p_f{i}", [128, 512], F32) for i in range(2)]; pf_b = P.bufs(2)
        hdv = hdb[:].rearrange("w (p a) -> w a p", a=32)
        st = s1_state(P, sb, ps, K, "fs1")
        fstg = [sb(f"fstg{i}", [128, 2, 2, D], BF16) for i in range(2)]; fstg_b = P.bufs(2)
        ad_b = P.bufs(32)
        rn2 = sb("rn2", [128, 2, 256], F32); rn2_b = P.buf()
        gbt = [sb(f"gbt{i}", [128, 4, 4, 128], BF16) for i in range(2)]; gbt_b = P.bufs(2)
        ain = [sb(f"ain{i}", [128, 2, 2, 4, 256], BF16) for i in range(2)]; ain_b = [P.bufs(4) for _ in range(2)]
        ains = [sb(f"ains{i}", [128, 2, 2, 4, 256], BF16) for i in range(2)]; ains_b = [P.bufs(2) for _ in range(2)]
        p_k = [ps(f"p_k{i}", [128, 2, 256], F32) for i in range(2)]; pk_b = P.bufs(2)
        p_n = ps("p_n", [128, 2, 256], F32); pn_b = P.buf()
        kst = [sb(f"kst{i}", [128, 4, 2, 256], BF16) for i in range(2)]; kst_b = P.bufs(2)
        it = 0
        for o in range(2):
            P.op("pool", lambda e: e.memset(asum[:], 0.0), [], [as_b])
            sp4 = st["p"] + [p_k[0][:].rearrange("p r g -> p (r g)"), p_k[1][:].rearrange("p r g -> p (r g)")]
            sp4 = [t if not hasattr(t, "ap") or True else t for t in sp4]
            sp4_b = st["p_b"] + pk_b

            def gen(a):
                nonlocal it
                di = a % 2
                P.op("act", lambda e: e.activation(out=dec[di][:], in_=dl[:], func=AF.Exp, scale=ntl[:, a:a + 1]), [dl_b, ntl_b], [dec_b[di]])
                for dr in range(2):
                    for chh in range(2):
                        col = (o * 2 + dr) * D + chh * 512
                        k = it % 2
                        it += 1
                        P.op("pe", lambda e, k=k, col=col: e.matmul(p_f[k][:], lhsT=hdv[:, a, :], rhs=w4[:, col:col + 512], start=True, stop=True), [hdb_b, w4_b], [pf_b[k]])
                        lc = dr * D + chh * 512
                        P.op("dve", lambda e, k=k, lc=lc, chh=chh: e.tensor_tensor(out=fa[di][:, lc:lc + 512], in0=p_f[k][:], in1=dec[di][:, chh * 512:(chh + 1) * 512], op=ALU.mult),
                             [pf_b[k], dec_b[di]], [fa_b[di]])
                P.op("act", lambda e: e.activation(out=absf[di][:], in_=fa[di][:], func=AF.Abs), [fa_b[di]], [absf_b[di]])
                P.op("pool", lambda e: e.tensor_tensor(out=asum[:], in0=asum[:], in1=absf[di][:], op=ALU.add), [absf_b[di], as_b], [as_b])
                if a == 0:
                    P.op("pool", lambda e: e.memset(fa[di][0:1, D:2 * D], 0.0), [as_b, fa_b[di]], [fa_b[di]])

            def s1(a):
                di = a % 2
                g = a % 2
                for dr in range(2):
                    for chh in range(2):
                        lc = dr * D + chh * 512
                        for ri in range(2):
                            k = st["it"] % 4
                            st["it"] += 1
                            pk = sp4[k]
                            pkv = pk if k >= 2 else pk[:]
                            P.op("pe", lambda e, pkv=pkv, ri=ri, lc=lc: e.matmul(pkv, lhsT=st["F1"][:, ri, :], rhs=fa[di][:, lc:lc + 512], start=True, stop=True),
                                 [fa_b[di], st["F1_b"]], [sp4_b[k]])
                            if ri == 0:
                                P.op("act", lambda e, pkv=pkv, dr=dr, ri=ri, chh=chh: e.copy(out=fstg[g][:, dr, ri, chh * 512:(chh + 1) * 512], in_=pkv), [sp4_b[k]], [fstg_b[g]])
                            else:
                                P.op("dve", lambda e, pkv=pkv, dr=dr, ri=ri, chh=chh: e.tensor_copy(out=fstg[g][:, dr, ri, chh * 512:(chh + 1) * 512], in_=pkv), [sp4_b[k]], [fstg_b[g]])
                P.dma("sp", K.Ad[:, :, :, a * D:(a + 1) * D].rearrange("s r k n -> k s r n"), fstg[g][:], reads=[fstg_b[g]], writes=[ad_b[a]])

            gen(0)
            for a in range(32):
                if a + 1 < 32:
                    gen(a + 1)
                s1(a)
            for dr in range(2):
                for c4 in range(4):
                    P.op("pe", lambda e, dr=dr, c4=c4: e.matmul(p_n[:, dr, :], lhsT=msk[:, c4, :], rhs=asum[:, dr * D + c4 * 256:dr * D + (c4 + 1) * 256], start=(c4 == 0), stop=(c4 == 3)),
                         [msk_b, as_b], [pn_b])
            P.op("dve", lambda e: e.tensor_scalar(out=rn2[:], in0=p_n[:], scalar1=EPS, scalar2=None, op0=ALU.add), [pn_b], [rn2_b])
            P.op("dve", lambda e: e.reciprocal(out=rn2[:], in_=rn2[:]), [rn2_b], [rn2_b])
            if "rn2" in K.dbg and o == 0:
                P.dma("sp", K.dbg["rn2"], rn2[:], reads=[rn2_b])
            re_terms = [(0, 0, 0), (2, 0, 1), (0, 1, 0), (2, 1, 1)]
            im_terms = [(1, 0, 0), (0, 0, 1), (2, 1, 0), (3, 1, 1)]
            KB = 4
            for grp in range(128 // KB):
                gi = grp % 2
                k0 = grp * KB
                P.dma("sp", gbt[gi][:], K.I["GB"][:, k0:k0 + KB, 0:4, :], writes=[gbt_b[gi]])
                for s_ in range(2):
                    for r_ in range(2):
                        P.dma("sp", ain[gi][:, s_, r_, :, :], K.Ad[s_, r_, k0:k0 + KB, :].rearrange("k (q g) -> q k g", g=256), reads=ad_b, writes=[ain_b[gi][s_ * 2 + r_]])
                for s_ in range(2):
                    P.op("dve", lambda e, gi=gi, s_=s_: e.tensor_tensor(out=ains[gi][:, s_].rearrange("p r k g -> p (r k) g"), in0=ain[gi][:, s_].rearrange("p r k g -> p (r k) g"),
                                                                       in1=rn2[:, s_:s_ + 1, :].to_broadcast([128, 2 * KB, 256]), op=ALU.mult),
                         ain_b[gi][s_ * 2:s_ * 2 + 2] + [rn2_b], [ains_b[gi][s_]])
                for kl in range(KB):
                    k1 = k0 + kl
                    kk = k1 % 2
                    for ri, terms in ((0, re_terms), (1, im_terms)):
                        for n, (mi, s_, r_) in enumerate(terms):
                            P.op("pe", lambda e, kk=kk, ri=ri, mi=mi, s_=s_, r_=r_, gi=gi, n=n, kl=kl: e.matmul(p_k[kk][:, ri, :], lhsT=gbt[gi][:, kl, mi, :], rhs=ains[gi][:, s_, r_, kl, :], start=(n == 0), stop=(n == 3)),
                                 [gbt_b[gi], ains_b[gi][s_]], [pk_b[kk]])
                    ks = (k1 // 4) % 2
                    P.op("act", lambda e, kk=kk, ks=ks, k1=k1: e.copy(out=kst[ks][:, k1 % 4, :, :], in_=p_k[kk][:]), [pk_b[kk]], [kst_b[ks]])
                    if k1 % 4 == 3:
                        P.dma("sp", K.Kd[o, :, k1 - 3:k1 + 1], kst[ks][:], reads=[kst_b[ks]])
    run_phase(K, body)


def phase_hyena_conv(K):
    I = K.I

    def body(P, sb, ps):
        st = s1_state(P, sb, ps, K, "ds1")
        F1T = sb("F1T", [128, 2, 128], BF16); f1t_b = P.buf()
        P.dma("sp", F1T[:], I["F1T"], writes=[f1t_b])
        fbias, fbias_b = None, None
        fbias = sb("fbias", [128, 2, D], F32); fbias_b = P.buf()
        for o in range(2):
            P.dma("sp", fbias[:, o, :], I["hy_filt_bias"][o:o + 1, :].partition_broadcast(128), writes=[fbias_b])
        xin = [sb(f"xin{i}", [128, 4, D], BF16) for i in range(2)]; xin_b = P.bufs(2)
        stg = [sb(f"s1stg{i}", [128, 2, 2048], BF16) for i in range(2)]; stg_b = P.bufs(2)
        ad_b = P.bufs(16)
        bd_b = P.bufs(16)
        z0_b = P.bufs(8)
        gb = [sb(f"gb{i}", [128, 4, 7, 128], BF16) for i in range(3)]; gb_b = P.bufs(3)
        ain = [sb(f"cain{i}", [128, 2, 4, 256], BF16) for i in range(3)]; ain_b = [P.bufs(2) for _ in range(3)]
        kk_ = [sb(f"ckk{i}", [128, 4, 2, 256], BF16) for i in range(3)]; kk_b = P.bufs(3)
        ksw = [sb(f"cksw{i}", [128, 4, 2, 256], BF16) for i in range(3)]; ksw_b = [P.bufs(2) for _ in range(3)]
        p_y = [ps(f"p_y{i}", [128, 2, 256], F32) for i in range(3)]; py_b = P.bufs(3)
        p_b = [ps(f"p_b{i}", [128, 2, 256], F32) for i in range(3)]; pb_b = P.bufs(3)
        m1 = [sb(f"m1{i}", [128, 2, 256], BF16) for i in range(2)]; m1_b = P.bufs(2)
        m2 = [sb(f"m2{i}", [128, 2, 256], BF16) for i in range(2)]; m2_b = P.bufs(2)
        pp = [sb(f"pp{i}", [128, 2, 256], BF16) for i in range(2)]; pp_b = P.bufs(2)
        bst = [sb(f"bst{i}", [128, 8, 2, 256], BF16) for i in range(2)]; bst_b = P.bufs(2)
        p_i = st["p"]; pi_b = st["p_b"]
        bin_ = stg; bin_b = stg_b
        gt = [sb(f"gt{i}", [128, 2, D], BF16) for i in range(2)]; gt_b = P.bufs(2)
        zi = [sb(f"zi{i}", [128, 2, D], F32) for i in range(2)]; zi_b = P.bufs(2)
        zib = [sb(f"zib{i}", [128, 2, D], BF16) for i in range(2)]; zib_b = P.bufs(2)
        t1 = [sb(f"t1{i}", [128, 512], F32) for i in range(2)]; t1_b = P.bufs(2)
        zo = [sb(f"zo{i}", [128, 2, D], BF16) for i in range(2)]; zo_b = P.bufs(2)
        UCv = K.UC.rearrange("(p a) c -> p a c", a=32)
        Zv = [K.Z[o].rearrange("(p a) c -> p a c", a=32) for o in range(2)]
        for o in range(2):
            for grp in range(8):
                xi = grp % 2
                if o == 0:
                    P.dma("pool", xin[xi][:], UCv[:, grp * 4:(grp + 1) * 4, 2 * D:3 * D], writes=[xin_b[xi]])
                else:
                    P.dma("sp", xin[xi][:], Zv[0][:, grp * 4:(grp + 1) * 4, :], reads=z0_b, writes=[xin_b[xi]])
                for j in range(8):
                    nch = grp * 8 + j
                    rhs = xin[xi][:, j // 2, (j % 2) * 512:(j % 2 + 1) * 512]
                    g = (nch // 4) % 2
                    for ri in range(2):
                        k = st["it"] % 2
                        st["it"] += 1
                        P.op("pe", lambda e, k=k, ri=ri, rhs=rhs: e.matmul(st["p"][k][:], lhsT=st["F1"][:, ri, :], rhs=rhs, start=True, stop=True), [xin_b[xi], st["F1_b"]], [st["p_b"][k]])
                        if ri == 0:
                            P.op("act", lambda e, k=k, g=g, ri=ri, nch=nch: e.copy(out=stg[g][:, ri, (nch % 4) * 512:(nch % 4 + 1) * 512], in_=st["p"][k][:]), [st["p_b"][k]], [stg_b[g]])
                        else:
                            P.op("dve", lambda e, k=k, g=g, ri=ri, nch=nch: e.tensor_copy(out=stg[g][:, ri, (nch % 4) * 512:(nch % 4 + 1) * 512], in_=st["p"][k][:]), [st["p_b"][k]], [stg_b[g]])
                    if nch % 4 == 3:
                        c0 = (nch // 4) * 2048
                        P.dma("sp", K.Ad[0, :, :, c0:c0 + 2048].rearrange("r k n -> k r n"), stg[g][:], reads=[stg_b[g]], writes=[ad_b[nch // 4]])
            KB = 4

            def load_grp(grp):
                bi = grp % 3
                k0 = grp * KB
                P.dma("sp", gb[bi][:], I["GB"][:, k0:k0 + KB], writes=[gb_b[bi]])
                for r_ in range(2):
                    P.dma("sp", ain[bi][:, r_, :, :], K.Ad[0, r_, k0:k0 + KB, :].rearrange("k (q g) -> q k g", g=256), reads=ad_b, writes=[ain_b[bi][r_]])
                P.dma("sp", kk_[bi][:], K.Kd[o, :, k0:k0 + KB], writes=[kk_b[bi]])

            def emit_s2(k1):
                bi = (k1 // KB) % 3
                kl = k1 % KB
                yi = k1 % 3
                if kl == 0:
                    load_grp(k1 // KB)
                for ri, terms in ((0, [(0, 0), (2, 1)]), (1, [(1, 0), (0, 1)])):
                    for n, (mi, r_) in enumerate(terms):
                        P.op("pe", lambda e, ri=ri, mi=mi, r_=r_, n=n: e.matmul(p_y[yi][:, ri, :], lhsT=gb[bi][:, kl, mi, :], rhs=ain[bi][:, r_, kl, :], start=(n == 0), stop=(n == 1)),
                             [gb_b[bi], ain_b[bi][r_]], [py_b[yi]])

            def emit_rest(k1):
                bi = (k1 // KB) % 3
                kl = k1 % KB
                yi = k1 % 3
                gi = k1 % 2
                P.op("dve", lambda e: e.tensor_tensor(out=m1[gi][:], in0=p_y[yi][:], in1=kk_[bi][:, kl], op=ALU.mult), [py_b[yi], kk_b[bi]], [m1_b[gi]])
                P.op("dve", lambda e: e.tensor_tensor(out=m2[gi][:, 0, :], in0=p_y[yi][:, 0, :], in1=kk_[bi][:, kl, 1, :], op=ALU.mult), [py_b[yi], kk_b[bi]], [m2_b[gi]])
                P.op("dve", lambda e: e.tensor_tensor(out=m2[gi][:, 1, :], in0=p_y[yi][:, 1, :], in1=kk_[bi][:, kl, 0, :], op=ALU.mult), [py_b[yi], kk_b[bi]], [m2_b[gi]])
                P.op("pool", lambda e: e.tensor_tensor(out=pp[gi][:, 0, :], in0=m1[gi][:, 0, :], in1=m1[gi][:, 1, :], op=ALU.subtract), [m1_b[gi]], [pp_b[gi]])
                P.op("pool", lambda e: e.tensor_tensor(out=pp[gi][:, 1, :], in0=m2[gi][:, 0, :], in1=m2[gi][:, 1, :], op=ALU.add), [m2_b[gi]], [pp_b[gi]])
                for ri, terms in ((0, [(4, 0), (5, 1)]), (1, [(4, 1), (6, 0)])):
                    for n, (mi, r_) in enumerate(terms):
                        P.op("pe", lambda e, ri=ri, mi=mi, r_=r_, n=n: e.matmul(p_b[yi][:, ri, :], lhsT=gb[bi][:, kl, mi, :], rhs=pp[gi][:, r_, :], start=(n == 0), stop=(n == 1)),
                             [gb_b[bi], pp_b[gi]], [pb_b[yi]])
                ks = (k1 // 8) % 2
                P.op("act", lambda e: e.copy(out=bst[ks][:, k1 % 8, :, :], in_=p_b[yi][:]), [pb_b[yi]], [bst_b[ks]])
                if k1 % 8 == 7:
                    P.dma("sp", K.Bd[0, k1 - 7:k1 + 1, :].rearrange("k (q g) -> q k g", g=256), bst[ks][:, :, 0, :], reads=[bst_b[ks]], writes=[bd_b[k1 // 8]])
                    P.dma("sp", K.Bd[1, k1 - 7:k1 + 1, :].rearrange("k (q g) -> q k g", g=256), bst[ks][:, :, 1, :], reads=[bst_b[ks]], writes=[bd_b[k1 // 8]])

            LOOK = 2
            for k1 in range(LOOK):
                emit_s2(k1)
            for k1 in range(128):
                if k1 + LOOK < 128:
                    emit_s2(k1 + LOOK)
                emit_rest(k1)
            for grp in range(16):
                bi = grp % 2
                P.dma("sp", bin_[bi][:], K.Bd[:, :, grp * 2048:(grp + 1) * 2048].rearrange("r k n -> k r n"), reads=bd_b, writes=[bin_b[bi]])
                P.dma("pool", gt[bi][:], UCv[:, grp * 2:(grp + 1) * 2, o * D:(o + 1) * D], writes=[gt_b[bi]])
                if o == 0:
                    P.dma("sp", zib[bi][:], UCv[:, grp * 2:(grp + 1) * 2, 2 * D:3 * D], writes=[zib_b[bi]])
                else:
                    P.dma("sp", zib[bi][:], Zv[0][:, grp * 2:(grp + 1) * 2, :], reads=z0_b, writes=[zib_b[bi]])
                P.op("dve", lambda e, bi=bi, o=o: e.tensor_tensor(out=zi[bi][:], in0=zib[bi][:], in1=fbias[:, o:o + 1, :].to_broadcast([128, 2, D]), op=ALU.mult), [zib_b[bi], fbias_b], [zi_b[bi]])
                for j in range(4):
                    k = (grp * 4 + j) % 2
                    for ri in range(2):
                        P.op("pe", lambda e, k=k, ri=ri, bi=bi, j=j: e.matmul(p_i[k][:], lhsT=F1T[:, ri, :], rhs=bin_[bi][:, ri, j * 512:(j + 1) * 512], start=(ri == 0), stop=(ri == 1)),
                             [f1t_b, bin_b[bi]], [pi_b[k]])
                    aa, hh = j // 2, j % 2
                    P.op("dve", lambda e, k=k, bi=bi, aa=aa, hh=hh: e.tensor_tensor(out=t1[k][:], in0=p_i[k][:], in1=zi[bi][:, aa, hh * 512:(hh + 1) * 512], op=ALU.add), [pi_b[k], zi_b[bi]], [t1_b[k]])
                    P.op("dve", lambda e, k=k, bi=bi, aa=aa, hh=hh: e.tensor_tensor(out=zo[bi][:, aa, hh * 512:(hh + 1) * 512], in0=t1[k][:], in1=gt[bi][:, aa, hh * 512:(hh + 1) * 512], op=ALU.mult), [t1_b[k], gt_b[bi]], [zo_b[bi]])
                P.dma("sp", Zv[o][:, grp * 2:(grp + 1) * 2, :], zo[bi][:], reads=[zo_b[bi]], writes=([z0_b[grp // 2]] if o == 0 else []))
    run_phase(K, body)


_PROG = {}


def _layout_inputs(inp, b):
    f = lambda a: np.ascontiguousarray(np.asarray(a, dtype=np.float32))
    m = {}
    m["x"] = f(inp["x"][b])
    m["c_t"] = f(np.asarray(inp["c"][b]).reshape(8, 128).T)
    m["pos_t"] = np.ascontiguousarray(np.asarray(inp["positions"][b]).astype(np.int32).reshape(NT, 128).T)
    for k in ("ada_w", "ada_b", "norm_mix_g", "norm_ffn_g"):
        m[k] = f(inp[k])
    m["mla_w_down"] = f(inp["mla_w_down"][0])
    m["mla_q_a_g"] = f(inp["mla_q_a_g"])
    m["mla_kv_a_g"] = f(inp["mla_kv_a_g"])
    m["mla_w_uq"] = f(inp["mla_w_uq"][0])
    m["mla_w_ukv"] = f(inp["mla_w_ukv"][0])
    m["mla_q_norm_g"] = f(inp["mla_q_norm_g"])
    m["mla_k_norm_g"] = f(inp["mla_k_norm_g"])
    m["mla_w_o"] = f(inp["mla_w_o"][0])
    m["hy_w_in"] = f(inp["hy_w_in"][0])
    m["hy_b_in_t"] = f(np.asarray(inp["hy_b_in"][0]).reshape(24, 128).T)
    m["hy_conv_w_t"] = f(np.asarray(inp["hy_conv_w"][0]).reshape(3, 24, 128).transpose(2, 0, 1))
    m["hy_conv_b_t"] = f(np.asarray(inp["hy_conv_b"][0]).reshape(24, 128).T)
    m["hy_f_w1"] = f(inp["hy_f_w1"][0])
    m["hy_f_w2"] = f(inp["hy_f_w2"][0])
    m["hy_f_w3"] = f(inp["hy_f_w3"][0])
    m["hy_f_bf_t"] = f(np.stack([np.asarray(inp[k][0]) for k in ("hy_f_b1", "hy_f_freq1", "hy_f_b2", "hy_f_freq2", "hy_f_b3", "hy_f_freq3")], axis=1))
    m["hy_f_w4"] = f(inp["hy_f_w4"][0])
    m["hy_filt_bias"] = f(inp["hy_filt_bias"][0])
    m["hy_w_out"] = f(inp["hy_w_out"][0])
    m["moe_wr"] = f(np.concatenate([np.asarray(inp["moe_wg"]), np.asarray(inp["moe_we"])], axis=-1))
    m["moe_br"] = f(np.concatenate([np.asarray(inp["moe_bg"]), np.asarray(inp["moe_be"])], axis=-1))
    m["moe_w_gate"] = f(inp["moe_w_gate"])
    m["moe_w_up"] = f(inp["moe_w_up"])
    m["moe_w_down"] = f(inp["moe_w_down"])
    return m


def kernel(**inputs):
    if "nc" not in _PROG:
        _PROG["nc"] = build_program()
    nc = _PROG["nc"]
    const = host_constants()
    shared = None
    in_maps = []
    for b in range(8):
        m = _layout_inputs(inputs, b)
        if shared is None:
            shared = {k: v for k, v in m.items() if k not in ("x", "c_t", "pos_t")}
        else:
            for k in shared:
                m[k] = shared[k]
        m.update(const)
        in_maps.append(m)
    res = run_bass_kernel_spmd(nc, in_maps, core_ids=list(range(8)))
    return np.stack([np.asarray(r["out"], dtype=np.float32) for r in res.results], axis=0)
```
